# Optimizing a Trainium2 kernel written in Bass

```python
import math
import jax, jax.numpy as jnp
from jax import lax
import numpy as np

D_MODEL = 1024
BATCH = 8
SEQ = 2048
DEPTH = 1

MLA_HEADS = 8
MLA_QK_NOPE = 64
MLA_QK_ROPE = 32
MLA_V_DIM = 64
MLA_Q_RANK = 384
MLA_KV_RANK = 256
DIFF_HEADS = 4
DIFF_HALF = 64
DIFF_V_DIM = 2 * DIFF_HALF
MIX_WIDTH = MLA_HEADS * MLA_V_DIM + DIFF_HEADS * DIFF_V_DIM
SPLIT_SIZES = (MLA_Q_RANK, MLA_KV_RANK, MLA_QK_ROPE,
               DIFF_HEADS * DIFF_V_DIM, DIFF_HEADS * DIFF_V_DIM, DIFF_HEADS * DIFF_V_DIM)
SPLIT_POINTS = tuple(int(v) for v in np.cumsum(SPLIT_SIZES)[:-1])
IN_COLS = int(sum(SPLIT_SIZES))
N_GROUPS = 4
EXPERTS_PER_GROUP = 8
N_EXPERTS = N_GROUPS * EXPERTS_PER_GROUP
TOP_K_INNER = 2
D_FF_EXPERT = 256

ROPE_THETA = 10000.0
Q_BLOCK = 128
EPS = 1e-6

kernel_name = "hybrid_mla_diffattn_hier_moe"


def rmsnorm(x, g):
    xf = x.astype(jnp.float32)
    y = xf * lax.rsqrt(jnp.mean(xf * xf, axis=-1, keepdims=True) + EPS)
    return (y * g.astype(jnp.float32)).astype(x.dtype)


def rope(x, positions):
    half = x.shape[-1] // 2
    inv_freq = ROPE_THETA ** (-jnp.arange(half, dtype=jnp.float32) / half)
    ang = positions.astype(jnp.float32)[:, :, None, None] * inv_freq
    cos, sin = jnp.cos(ang), jnp.sin(ang)
    x1 = x[..., :half].astype(jnp.float32)
    x2 = x[..., half:].astype(jnp.float32)
    out = jnp.concatenate([x1 * cos - x2 * sin, x2 * cos + x1 * sin], axis=-1)
    return out.astype(x.dtype)


def causal_attention(q, k, v, scale):
    B, S, H, Dk = q.shape
    Dv = v.shape[-1]
    nb = S // Q_BLOCK
    qb = q.reshape(B, nb, Q_BLOCK, H, Dk).transpose(1, 0, 2, 3, 4)
    kf = k.astype(jnp.float32)
    vf = v.astype(jnp.float32)
    kpos = jnp.arange(S)

    def one_block(args):
        q_blk, i = args
        s = jnp.einsum('bqhd,bkhd->bhqk', q_blk.astype(jnp.float32), kf) * scale
        qpos = i * Q_BLOCK + jnp.arange(Q_BLOCK)
        mask = kpos[None, :] <= qpos[:, None]
        s = jnp.where(mask[None, None], s, jnp.finfo(jnp.float32).min)
        p = jax.nn.softmax(s, axis=-1)
        return jnp.einsum('bhqk,bkhd->bqhd', p, vf)

    o = lax.map(one_block, (qb, jnp.arange(nb)))
    return o.transpose(1, 0, 2, 3, 4).reshape(B, S, H, Dv).astype(v.dtype)


def hybrid_mixer(h, positions, w_in, q_norm_g, w_uq, kv_norm_g, w_ukv,
                 lam_q1, lam_k1, lam_q2, lam_k2, subln_g, w_o, lam_init):
    B, S, _ = h.shape
    proj = h @ w_in
    c_q, c_kv, k_pe, dq, dk, dv = jnp.split(proj, SPLIT_POINTS, axis=-1)

    q = (rmsnorm(c_q, q_norm_g) @ w_uq).reshape(B, S, MLA_HEADS, MLA_QK_NOPE + MLA_QK_ROPE)
    q_nope, q_pe = q[..., :MLA_QK_NOPE], q[..., MLA_QK_NOPE:]
    kv = (rmsnorm(c_kv, kv_norm_g) @ w_ukv).reshape(B, S, MLA_HEADS, MLA_QK_NOPE + MLA_V_DIM)
    k_nope, v_mla = kv[..., :MLA_QK_NOPE], kv[..., MLA_QK_NOPE:]
    k_pe = rope(k_pe[:, :, None, :], positions)
    q_m = jnp.concatenate([q_nope, rope(q_pe, positions)], axis=-1)
    k_m = jnp.concatenate([k_nope, jnp.broadcast_to(k_pe, (B, S, MLA_HEADS, MLA_QK_ROPE))], axis=-1)
    o_mla = causal_attention(q_m, k_m, v_mla, (MLA_QK_NOPE + MLA_QK_ROPE) ** -0.5)
    o_mla = o_mla.reshape(B, S, MLA_HEADS * MLA_V_DIM)

    dq = rope(dq.reshape(B, S, 2 * DIFF_HEADS, DIFF_HALF), positions).reshape(B, S, DIFF_HEADS, 2, DIFF_HALF)
    dk = rope(dk.reshape(B, S, 2 * DIFF_HEADS, DIFF_HALF), positions).reshape(B, S, DIFF_HEADS, 2, DIFF_HALF)
    dv = dv.reshape(B, S, DIFF_HEADS, DIFF_V_DIM)
    lam = (jnp.exp(jnp.sum(lam_q1.astype(jnp.float32) * lam_k1.astype(jnp.float32)))
           - jnp.exp(jnp.sum(lam_q2.astype(jnp.float32) * lam_k2.astype(jnp.float32)))
           + lam_init)
    scale_d = DIFF_HALF ** -0.5
    a1 = causal_attention(dq[..., 0, :], dk[..., 0, :], dv, scale_d).astype(jnp.float32)
    a2 = causal_attention(dq[..., 1, :], dk[..., 1, :], dv, scale_d).astype(jnp.float32)
    o_diff = rmsnorm(a1 - lam * a2, subln_g) * (1.0 - lam_init)
    o_diff = o_diff.astype(h.dtype).reshape(B, S, DIFF_HEADS * DIFF_V_DIM)

    return jnp.concatenate([o_mla, o_diff], axis=-1) @ w_o


def hier_moe(h, w_rg, b_rg, w_re, b_re, w_gate, w_up, w_down):
    B, S, D = h.shape
    t = h.reshape(B * S, D)
    g_logits = (t @ w_rg).astype(jnp.float32) + b_rg.astype(jnp.float32)
    p_group = jax.nn.softmax(g_logits, axis=-1)
    g_idx = jnp.argmax(g_logits, axis=-1)
    g_onehot = jax.nn.one_hot(g_idx, N_GROUPS, dtype=jnp.float32)
    p_g_sel = jnp.sum(p_group * g_onehot, axis=-1, keepdims=True)
    e_logits = (jnp.einsum('td,dge->tge', t, w_re).astype(jnp.float32)
                + b_re.astype(jnp.float32))
    e_logits = jnp.einsum('tge,tg->te', e_logits, g_onehot)
    p_exp = jax.nn.softmax(e_logits, axis=-1)
    top_p, top_i = lax.top_k(p_exp, TOP_K_INNER)
    top_p = top_p / jnp.sum(top_p, axis=-1, keepdims=True)
    expert_id = g_idx[:, None] * EXPERTS_PER_GROUP + top_i
    gates = jnp.einsum('tk,tkn->tn', p_g_sel * top_p,
                       jax.nn.one_hot(expert_id, N_EXPERTS, dtype=jnp.float32))
    a = jnp.einsum('td,ndf->tnf', t, w_gate)
    u = jnp.einsum('td,ndf->tnf', t, w_up)
    hdn = jax.nn.silu(a) * u * gates[:, :, None].astype(t.dtype)
    y = jnp.einsum('tnf,nfd->td', hdn, w_down)
    return y.reshape(B, S, D)


def setup_inputs(seed: int = 0) -> dict:
    key = jax.random.key(seed)
    ks = jax.random.split(key, 24)
    f32 = jnp.float32

    def nrm(k, shape, fan_in):
        return jax.random.normal(k, shape, f32) * (fan_in ** -0.5)

    def gain(k, shape):
        return 1.0 + 0.05 * jax.random.normal(k, shape, f32)

    L = DEPTH
    return {
        "x": jax.random.normal(ks[0], (BATCH, SEQ, D_MODEL), f32),
        "positions": jnp.broadcast_to(jnp.arange(SEQ, dtype=jnp.int32)[None, :], (BATCH, SEQ)),
        "attn_norm_g": gain(ks[1], (L, D_MODEL)),
        "w_in": nrm(ks[2], (L, D_MODEL, IN_COLS), D_MODEL),
        "q_norm_g": gain(ks[3], (L, MLA_Q_RANK)),
        "w_uq": nrm(ks[4], (L, MLA_Q_RANK, MLA_HEADS * (MLA_QK_NOPE + MLA_QK_ROPE)), MLA_Q_RANK),
        "kv_norm_g": gain(ks[5], (L, MLA_KV_RANK)),
        "w_ukv": nrm(ks[6], (L, MLA_KV_RANK, MLA_HEADS * (MLA_QK_NOPE + MLA_V_DIM)), MLA_KV_RANK),
        "lambda_q1": 0.1 * jax.random.normal(ks[7], (L, DIFF_HALF), f32),
        "lambda_k1": 0.1 * jax.random.normal(ks[8], (L, DIFF_HALF), f32),
        "lambda_q2": 0.1 * jax.random.normal(ks[9], (L, DIFF_HALF), f32),
        "lambda_k2": 0.1 * jax.random.normal(ks[10], (L, DIFF_HALF), f32),
        "subln_g": gain(ks[11], (L, DIFF_V_DIM)),
        "w_o": nrm(ks[12], (L, MIX_WIDTH, D_MODEL), MIX_WIDTH),
        "ffn_norm_g": gain(ks[13], (L, D_MODEL)),
        "w_router_group": nrm(ks[14], (L, D_MODEL, N_GROUPS), D_MODEL),
        "b_router_group": 0.01 * jax.random.normal(ks[15], (L, N_GROUPS), f32),
        "w_router_expert": nrm(ks[16], (L, D_MODEL, N_GROUPS, EXPERTS_PER_GROUP), D_MODEL),
        "b_router_expert": 0.01 * jax.random.normal(ks[17], (L, N_GROUPS, EXPERTS_PER_GROUP), f32),
        "w_gate": nrm(ks[18], (L, N_EXPERTS, D_MODEL, D_FF_EXPERT), D_MODEL),
        "w_up": nrm(ks[19], (L, N_EXPERTS, D_MODEL, D_FF_EXPERT), D_MODEL),
        "w_down": nrm(ks[20], (L, N_EXPERTS, D_FF_EXPERT, D_MODEL), D_FF_EXPERT),
        "final_norm_g": gain(ks[21], (D_MODEL,)),
    }


def reference(x, positions, attn_norm_g, w_in, q_norm_g, w_uq, kv_norm_g, w_ukv,
              lambda_q1, lambda_k1, lambda_q2, lambda_k2, subln_g, w_o,
              ffn_norm_g, w_router_group, b_router_group, w_router_expert, b_router_expert,
              w_gate, w_up, w_down, final_norm_g):
    h = x
    for l in range(DEPTH):
        lam_init = 0.8 - 0.6 * math.exp(-0.3 * l)
        mixed = hybrid_mixer(rmsnorm(h, attn_norm_g[l]), positions, w_in[l], q_norm_g[l], w_uq[l],
                             kv_norm_g[l], w_ukv[l], lambda_q1[l], lambda_k1[l], lambda_q2[l],
                             lambda_k2[l], subln_g[l], w_o[l], lam_init)
        h = h + mixed.astype(h.dtype)
        moe = hier_moe(rmsnorm(h, ffn_norm_g[l]), w_router_group[l], b_router_group[l],
                       w_router_expert[l], b_router_expert[l], w_gate[l], w_up[l], w_down[l])
        h = h + moe.astype(h.dtype)
    return rmsnorm(h, final_norm_g)
```

```python
import numpy as np
import ml_dtypes
import concourse.bass as bass
import concourse.mybir as mybir
from concourse.bass_utils import run_bass_kernel_spmd

F32 = mybir.dt.float32
BF16 = mybir.dt.bfloat16
I32 = mybir.dt.int32
AF = mybir.ActivationFunctionType
ALU = mybir.AluOpType
AX = mybir.AxisListType


class Buf:
    __slots__ = ("name", "writer", "readers")

    def __init__(self, name=""):
        self.name = name
        self.writer = None
        self.readers = []


class Op:
    __slots__ = ("eng", "fn", "deps", "signal", "val", "sem", "is_dma", "idx")

    def __init__(self, eng, fn, is_dma=False):
        self.eng = eng
        self.fn = fn
        self.deps = []
        self.signal = False
        self.val = None
        self.sem = None
        self.is_dma = is_dma
        self.idx = None


ENGS = ("pe", "act", "dve", "pool", "sp")


class Plan:
    def __init__(self, n_dma_sems=24):
        self.ops = {e: [] for e in ENGS}
        self.n_dma_sems = n_dma_sems
        self.dma_count = 0
        self.dma_counts = [0, 0]
        self.dma_last = [None] * n_dma_sems
        self.nops = 0

    def _add(self, op, reads, writes, deps):
        op.idx = self.nops
        self.nops += 1
        dl = []
        for b in reads:
            if b.writer is not None:
                dl.append((b.writer, "raw"))
        for b in writes:
            if b.writer is not None:
                dl.append((b.writer, "waw"))
            for r in b.readers:
                dl.append((r, "war"))
        for d in deps:
            if d is not None:
                dl.append((d, "raw"))
        for b in reads:
            b.readers.append(op)
        for b in writes:
            b.writer = op
            b.readers = []
        seen = set()
        for d, kind in dl:
            if d is op or id(d) in seen:
                continue
            if (not d.is_dma) and d.eng == op.eng and not op.is_dma:
                if d.eng == "pe":
                    continue
            seen.add(id(d))
            d.signal = True
            op.deps.append(d)
        self.ops[op.eng].append(op)
        return op

    def op(self, eng, fn, reads=(), writes=(), deps=()):
        return self._add(Op(eng, fn), list(reads), list(writes), list(deps))

    def dma(self, eng, fn, reads=(), writes=(), deps=()):
        op = Op(eng, fn, is_dma=True)
        half = self.n_dma_sems // 2
        grp = 1 if eng == "pool" else 0
        cnt = self.dma_counts[grp]
        s = grp * half + cnt % half
        op.sem = s
        op.val = 16 * (cnt // half + 1)
        self.dma_counts[grp] += 1
        self.dma_count += 1
        deps = list(deps)
        if self.dma_last[s] is not None:
            deps.append(self.dma_last[s])
        self.dma_last[s] = op
        op.signal = True
        return self._add(op, list(reads), list(writes), deps)

    def emit(self, nc, block, sems, dma_sems, final_waits):
        for e in ENGS:
            c = 0
            for op in self.ops[e]:
                if op.is_dma:
                    continue
                if op.signal:
                    c += 1
                    op.val = c
        plan = self

        def run(eng_name, eng):
            waited = {}
            for op in plan.ops[eng_name]:
                need = {}
                for d in op.deps:
                    key = ("dma", d.sem) if d.is_dma else ("eng", d.eng)
                    if d.val > need.get(key, 0):
                        need[key] = d.val
                for key, v in need.items():
                    if waited.get(key, 0) >= v:
                        continue
                    waited[key] = v
                    sem = dma_sems[key[1]] if key[0] == "dma" else sems[key[1]]
                    eng.wait_ge(sem, v)
                if op.fn is None:
                    continue
                ins = op.fn(eng)
                if op.is_dma:
                    ins.then_inc(dma_sems[op.sem], 16)
                elif op.signal:
                    ins.then_inc(sems[eng_name], 1)
            if eng_name == "sp":
                for d in final_waits:
                    sem = dma_sems[d.sem] if d.is_dma else sems[d.eng]
                    eng.wait_ge(sem, d.val)

        @block.tensor
        def _(pe):
            run("pe", pe)

        @block.scalar
        def _(act):
            run("act", act)

        @block.vector
        def _(dve):
            run("dve", dve)

        @block.gpsimd
        def _(pool):
            run("pool", pool)

        @block.sync
        def _(sp):
            run("sp", sp)


S = 2048
D = 1024
NT = 16
EPS = 1e-6
THETA = 10000.0
LAM_INIT = 0.8 - 0.6 * 1.0
NEG = -30000.0
N_EXP = 32
DFF = 256

C_GA, C_GQKV, C_GF, C_INVD, C_INVM, C_SGND, C_SGNM, C_DIVQ, C_PCOL, C_PMB, NCST = 0, 8, 13, 21, 22, 23, 24, 25, 27, 28, 29
R_FING, R_SUBG, R_LAM, R_BR, R_GF, R_IOTA, R_C8G, NROW = 0, 1024, 1152, 1408, 1444, 2468, 2468 + 96, 2468 + 100


class Arena:
    def __init__(self, nc, es, nbytes):
        self.t = es.enter_context(nc.sbuf_tensor("arena", [128, nbytes // 2], BF16))
        self.top = 0
        self.cap = nbytes
        self.peak = 0

    def alloc(self, free_shape, dt):
        esz = 4 if dt in (F32, I32) else 2
        n = int(np.prod(free_shape))
        nb = (n * esz + 63) // 64 * 64
        off = self.top
        self.top += nb
        self.peak = max(self.peak, self.top)
        assert self.top <= self.cap, ("SBUF arena overflow", self.top, self.cap)
        v = self.t[:, off // 2: off // 2 + (n * esz) // 2]
        if dt != BF16:
            v = v.bitcast(dt)
        if len(free_shape) == 2:
            v = v.rearrange("p (a b) -> p a b", a=free_shape[0])
        elif len(free_shape) == 3:
            v = v.rearrange("p (a b c) -> p a b c", a=free_shape[0], b=free_shape[1])
        return v


def bc(ap, shape):
    return ap.to_broadcast(list(shape))


def MM(out, lhsT, rhs, start=True, stop=True, skip=False):
    if skip:
        return lambda e: e.matmul(out, lhsT, rhs, start=start, stop=stop, skip_group_check=True)
    return lambda e: e.matmul(out, lhsT, rhs, start=start, stop=stop)


def TR(out, in_, ident):
    return lambda e: e.transpose(out, in_, ident)


def ACTV(out, in_, func, bias=None, scale=1.0, accum_out=None):
    kw = {}
    if bias is not None:
        kw["bias"] = bias
    if accum_out is not None:
        kw["accum_out"] = accum_out
    return lambda e: e.activation(out=out, in_=in_, func=func, scale=scale, **kw)


def TT(out, in0, in1, op):
    return lambda e: e.tensor_tensor(out=out, in0=in0, in1=in1, op=op)


def TS(out, in0, s1, op0, s2=None, op1=None):
    if op1 is None:
        return lambda e: e.tensor_scalar(out=out, in0=in0, scalar1=s1, scalar2=None, op0=op0)
    return lambda e: e.tensor_scalar(out=out, in0=in0, scalar1=s1, scalar2=s2, op0=op0, op1=op1)


def STT(out, in0, scalar, in1, op0, op1):
    return lambda e: e.scalar_tensor_tensor(out=out, in0=in0, scalar=scalar, in1=in1, op0=op0, op1=op1)


def CP(out, in_):
    return lambda e: e.tensor_copy(out=out, in_=in_)


def MSET(out, val):
    return lambda e: e.memset(out, val)


def RECIP(out, in_):
    return lambda e: e.reciprocal(out=out, in_=in_)


def RSUM(out, in_):
    return lambda e: e.reduce_sum(out=out, in_=in_, axis=AX.X)


def DMA(out, in_):
    return lambda e: e.dma_start(out=out, in_=in_)


def _rot_cols(base, n, grp):
    idx = np.arange(n).reshape(-1, grp)
    half = grp // 2
    idx = np.concatenate([idx[:, half:], idx[:, :half]], axis=1).reshape(-1)
    return base + idx


def prep_shared(inp):
    f = np.float32
    w_in = np.asarray(inp["w_in"])[0]
    wAV = np.ascontiguousarray(np.concatenate([w_in[:, 0:640], w_in[:, 1696:2208]], axis=1))
    blocks = []
    for base in (672, 1184):
        for j in range(4):
            blocks.append(w_in[:, base + j * 128: base + (j + 1) * 128])
            blocks.append(w_in[:, _rot_cols(base + j * 128, 128, 64)])
    kpe = np.zeros((D, 128), f)
    kpe[:, 64:96] = w_in[:, 640:672]
    kper = np.zeros((D, 128), f)
    kper[:, 64:96] = w_in[:, _rot_cols(640, 32, 32)]
    blocks += [kpe, kper]
    wF = np.ascontiguousarray(np.concatenate(blocks, axis=1))
    w_uq = np.asarray(inp["w_uq"])[0]
    rot = np.zeros_like(w_uq)
    for h in range(8):
        rot[:, h * 96 + 64: h * 96 + 96] = w_uq[:, _rot_cols(h * 96 + 64, 32, 32)]
    wUQ = np.ascontiguousarray(np.concatenate([w_uq, rot], axis=1))
    w_ukv = np.asarray(inp["w_ukv"])[0]
    kcols = np.concatenate([np.arange(h * 128, h * 128 + 64) for h in range(8)])
    wUKV = np.ascontiguousarray(np.concatenate([w_ukv[:, kcols], w_ukv[:, kcols + 64]], axis=1))
    cst = np.zeros((128, NCST), f)
    p = np.arange(128)
    cst[:, C_GA:C_GA + 8] = np.asarray(inp["attn_norm_g"])[0].reshape(8, 128).T
    cst[:, C_GQKV:C_GQKV + 3] = np.asarray(inp["q_norm_g"])[0].reshape(3, 128).T
    cst[:, C_GQKV + 3:C_GQKV + 5] = np.asarray(inp["kv_norm_g"])[0].reshape(2, 128).T
    cst[:, C_GF:C_GF + 8] = np.asarray(inp["ffn_norm_g"])[0].reshape(8, 128).T
    invd = (f(THETA) ** (-(p % 32).astype(f) / f(32))).astype(f)
    invm = (f(THETA) ** (-(p % 16).astype(f) / f(16))).astype(f)
    cst[:, C_INVD] = invd / f(2 * np.pi)
    cst[:, C_INVM] = invm / f(2 * np.pi)
    cst[:, C_SGND] = np.where((p % 64) < 32, -1.0, 1.0)
    cst[:, C_SGNM] = np.where((p % 32) < 16, -1.0, 1.0)
    cst[:, C_DIVQ] = 1.0 / 384
    cst[:, C_DIVQ + 1] = 1.0 / 256
    cst[:, C_PCOL] = p
    cst[:, C_PMB] = p - float(1 << 16)
    rowc = np.zeros((1, NROW), f)
    rowc[0, R_FING:R_FING + 1024] = np.asarray(inp["final_norm_g"])
    rowc[0, R_SUBG:R_SUBG + 128] = np.asarray(inp["subln_g"])[0]
    rowc[0, R_LAM:R_LAM + 64] = np.asarray(inp["lambda_q1"])[0]
    rowc[0, R_LAM + 64:R_LAM + 128] = np.asarray(inp["lambda_q2"])[0]
    rowc[0, R_LAM + 128:R_LAM + 192] = np.asarray(inp["lambda_k1"])[0]
    rowc[0, R_LAM + 192:R_LAM + 256] = np.asarray(inp["lambda_k2"])[0]
    rowc[0, R_BR:R_BR + 4] = np.asarray(inp["b_router_group"])[0]
    rowc[0, R_BR + 4:R_BR + 36] = np.asarray(inp["b_router_expert"])[0].reshape(32)
    rowc[0, R_GF:R_GF + 1024] = np.asarray(inp["ffn_norm_g"])[0]
    rowc[0, R_IOTA:R_IOTA + 96] = np.arange(96)
    rowc[0, R_C8G:R_C8G + 4] = np.arange(4) * 8
    ident = np.eye(128, dtype=f)
    mtri = np.where(p[:, None] > p[None, :], f(NEG), f(0)).astype(f)
    wR = np.ascontiguousarray(np.concatenate([np.asarray(inp["w_router_group"])[0],
                                              np.asarray(inp["w_router_expert"])[0].reshape(D, 32)], axis=1))
    return {
        "wAV": wAV, "wF": wF, "wUQ": wUQ, "wUKV": wUKV, "wO": np.ascontiguousarray(np.asarray(inp["w_o"])[0]),
        "cst": cst, "rowc": rowc, "ident": ident, "mtri": mtri, "wR": wR,
        "wGt": np.ascontiguousarray(np.asarray(inp["w_gate"])[0].reshape(32, 8, 128, 256).transpose(0, 2, 1, 3).reshape(4096, 2048)),
        "wUt": np.ascontiguousarray(np.asarray(inp["w_up"])[0].reshape(32, 8, 128, 256).transpose(0, 2, 1, 3).reshape(4096, 2048)),
        "wDt": np.ascontiguousarray(np.asarray(inp["w_down"])[0].reshape(32, 2, 128, 1024).transpose(0, 2, 1, 3).reshape(4096, 2048)),
        "utri": np.triu(np.ones((128, 128), f)),
    }


_BOUND_REGS = {}


def IDMA(out, in_, out_off=None, in_off=None, bound=None):
    def f(e):
        key = (id(e), bound)
        if key not in _BOUND_REGS:
            _BOUND_REGS[key] = e.to_reg(bound)
        return e.indirect_dma_start(
            out=out, out_offset=(bass.IndirectOffsetOnAxis(ap=out_off, axis=0) if out_off is not None else None),
            in_=in_, in_offset=(bass.IndirectOffsetOnAxis(ap=in_off, axis=0) if in_off is not None else None),
            bounds_check=_BOUND_REGS[key], oob_is_err=False)
    return f


def TSS(out, in_, scalar, op):
    return lambda e: e.tensor_single_scalar(out=out, in_=in_, scalar=scalar, op=op)


def _fence(P):
    lasts = []
    for e in ENGS:
        comp = [o for o in P.ops[e] if (not o.is_dma) and o.fn is not None]
        if comp:
            lasts.append(comp[-1])
    dmas = [o for o in P.dma_last if o is not None]
    for e in ENGS:
        op = Op(e, None)
        op.idx = P.nops
        P.nops += 1
        for d in lasts + dmas:
            if (not d.is_dma) and d.eng == e:
                continue
            d.signal = True
            op.deps.append(d)
        P.ops[e].append(op)


Plan.fence = _fence


def build_nc(dbg=()):
    from contextlib import ExitStack
    _BOUND_REGS.clear()
    nc = bass.Bass("TRN2", target_bir_lowering=False)

    def din(name, shape, dt=F32):
        return nc.dram_tensor(name, list(shape), dt, kind="ExternalInput").ap()

    x_d = din("x", [S, D])
    pos_d = din("pos", [1, S], I32)
    wAV_d = din("wAV", [D, 1152])
    wF_d = din("wF", [D, 2304])
    wUQ_d = din("wUQ", [384, 1536])
    wUKV_d = din("wUKV", [256, 1024])
    wO_d = din("wO", [D, D])
    cst_d = din("cst", [128, NCST])
    rowc_d = din("rowc", [1, NROW])
    ident_d = din("ident", [128, 128])
    mtri_d = din("mtri", [128, 128])
    wR_d = din("wR", [D, 36])
    wGt_d = din("wGt", [4096, 2048])
    wUt_d = din("wUt", [4096, 2048])
    wDt_d = din("wDt", [4096, 2048])
    utri_d = din("utri", [128, 128])
    out_d = nc.dram_tensor("out", [S, D], F32, kind="ExternalOutput").ap()
    dbg_d = {}

    P = Plan()
    es = ExitStack()
    finals = []
    with es:
        A = Arena(nc, es, 207 * 1024)
        ps_all = es.enter_context(nc.psum_tensor("ps_all", [128, 8 * 512], F32))
        sems = {e: es.enter_context(nc.semaphore("s_" + e)) for e in ENGS}
        dsems = [es.enter_context(nc.semaphore("d%d" % i)) for i in range(P.n_dma_sems)]
        block = es.enter_context(nc.Block())

        def bank(b, n=1):
            return ps_all[:, b * 512:(b + n) * 512]

        def bank_bf(b):
            return ps_all[:, b * 512:(b + 1) * 512].bitcast(BF16)

        def dump(name, ap, shape, dt, reads=()):
            if name not in dbg:
                return
            d = nc.dram_tensor("dbg_" + name, list(shape), dt, kind="ExternalOutput").ap()
            dbg_d[name] = d
            finals.append(P.dma("sp", DMA(d, ap), reads=list(reads)))

        cst = A.alloc([NCST], F32)
        rowbc = A.alloc([NROW], F32)
        identf = A.alloc([128], F32)
        onesf = A.alloc([128], F32)
        identb = A.alloc([128], BF16)
        mtrib = A.alloc([128], BF16)
        epsb = A.alloc([1], F32)
        ssx = A.alloc([16], F32)
        sqx = A.alloc([16], F32)
        rstdx = A.alloc([16], F32)
        ssqkv = A.alloc([16, 2], F32)
        t_a = A.alloc([16, 2], F32)
        t_b = A.alloc([16, 2], F32)
        t_c = A.alloc([16, 2], F32)
        sqkv = A.alloc([16, 2], F32)
        lamt = A.alloc([128], F32)
        lam2 = A.alloc([2], F32)
        lame = A.alloc([2], F32)
        neglam = A.alloc([1], F32)
        dss = A.alloc([16, 4], F32)
        dsq = A.alloc([16, 4], F32)
        drd = A.alloc([16, 4], F32)
        ssf = A.alloc([16], F32)
        sqf = A.alloc([16], F32)
        rf = A.alloc([16], F32)
        junk = A.alloc([1024], BF16)
        Bcst, Bjunk = Buf("cst"), Buf("junk")

        P.dma("sp", DMA(cst, cst_d[:, :]), writes=[Bcst])
        P.dma("sp", DMA(rowbc, rowc_d.partition_broadcast(128)), writes=[Bcst])
        P.dma("sp", DMA(identf, ident_d[:, :]), writes=[Bcst])
        P.dma("pool", DMA(identb, ident_d[:, :]), writes=[Bcst])
        P.dma("pool", DMA(mtrib, mtri_d[:, :]), writes=[Bcst])
        Bst = Buf("stats")
        Bst_t = [Buf() for _ in range(16)]
        P.op("dve", MSET(epsb, EPS), writes=[Bst])
        P.op("dve", MSET(onesf, 1.0), writes=[Bst])
        for t_ in (ssx, ssqkv, dss, ssf):
            P.op("dve", MSET(t_, 0.0), writes=[Bst] + Bst_t)
        P.op("dve", MSET(epsb, EPS), writes=[Bst] + Bst_t)
        P.op("dve", TT(lamt, rowbc[:, R_LAM:R_LAM + 128], rowbc[:, R_LAM + 128:R_LAM + 256], ALU.mult),
             reads=[Bcst], writes=[Bst])
        P.op("dve", RSUM(lam2, lamt.rearrange("p (a b) -> p a b", a=2)), reads=[Bst], writes=[Bst])
        P.op("act", ACTV(lame, lam2, AF.Exp), reads=[Bst], writes=[Bst])
        P.op("dve", TT(neglam, lame[:, 1:2], lame[:, 0:1], ALU.subtract), reads=[Bst], writes=[Bst])
        P.op("dve", TS(neglam, neglam, -LAM_INIT, ALU.add), reads=[Bst], writes=[Bst])

        R1 = A.alloc([8, 2304], BF16)
        wF = R1
        mark_cnT = A.top
        cnT = A.alloc([5, S], BF16)
        kpeT = A.alloc([S], BF16)
        posf = A.alloc([S], F32)
        mark_VD = A.top
        VD = A.alloc([16, 4, 129], BF16)
        QTD = A.alloc([4, S], BF16)
        KTD = A.alloc([4, S], BF16)
        mark_wAV = A.top
        wAV = A.alloc([8, 1152], BF16)
        xs = [A.alloc([1024], F32)] * 2
        xb = [A.alloc([1024], BF16) for _ in range(2)]
        xT = [A.alloc([8, 512], BF16) for _ in range(2)]
        cn = [A.alloc([640], BF16) for _ in range(2)]
        CD = A.alloc([512], F32)
        SD = A.alloc([512], F32)
        CMr = A.alloc([512], F32)
        SMr = A.alloc([512], F32)
        u2 = A.alloc([2, 512], F32)
        nn = A.alloc([2, 512], F32)
        ni = nn.bitcast(I32)
        cs_set = [A.alloc([2, 512], F32) for _ in range(2)]
        t1 = [A.alloc([512], F32)] * 2
        t2 = [A.alloc([512], F32)] * 2
        diag = [A.alloc([128], F32) for _ in range(2)]
        _save = A.top
        A.top = mark_wAV
        KTz = A.alloc([8, S], BF16)
        mark_after_KTz = A.top
        A.top = _save
        BKTz = Buf("KTz")
        posi = ni.rearrange("p a b -> p (a b)")

        Bpos = Buf("pos")
        for hh in range(2):
            P.dma("sp", DMA(posi, pos_d[:, hh * 1024:(hh + 1) * 1024].partition_broadcast(128)), writes=[Bpos])
            P.op("dve", CP(posf[:, hh * 1024:(hh + 1) * 1024], posi), reads=[Bpos], writes=[Bpos])
        BVD = Buf("VD")
        P.op("pool", MSET(VD[:, :, :, 128:129], 1.0), writes=[BVD])

        BwAV = Buf("wAV")
        BwF = [Buf("wF%d" % i) for i in range(3)]
        P.dma("pool", DMA(wAV, wAV_d.rearrange("(k p) c -> p k c", p=128)), writes=[BwAV])
        for i in range(3):
            P.dma("pool", DMA(wF[:, :, i * 768:(i + 1) * 768],
                              wF_d[:, i * 768:(i + 1) * 768].rearrange("(k p) c -> p k c", p=128)), writes=[BwF[i]])

        gA3 = cst[:, C_GA:C_GA + 8].rearrange("p (a b) -> p a b", b=1)
        gQ3 = cst[:, C_GQKV:C_GQKV + 5].rearrange("p (a b) -> p a b", b=1)
        psT, psT2, psA0, psA1, psV, psB, psF0, psF1 = [bank(i) for i in range(8)]
        psT_b = bank_bf(0)
        psT2_b = bank_bf(1)
        BpsT, BpsT2, BpsA0, BpsA1, BpsV, BpsB, BpsF0, BpsF1 = [Buf("ps%d" % i) for i in range(8)]
        Bxs = [Buf()] * 2
        Bxb = [Buf(), Buf()]
        BxT = [[Buf() for _ in range(4)] for _ in range(2)]
        Bcn = [Buf(), Buf()]
        Btab = Buf("tab")
        Btmp = Buf("tabtmp")
        Bt12 = [Buf()] * 2
        Bdiag = [Buf(), Buf()]
        BcnT = Buf("cnT")
        Bqk = Buf("qkT")
        def a_load(tt):
            sl = slice(tt * 128, (tt + 1) * 128)
            i2 = tt % 2
            P.dma("sp", DMA(xs[i2], x_d[sl, :]), writes=[Bxs[i2]])
            P.dma("pool", DMA(xb[i2], x_d[sl, :]), writes=[Bxb[i2]])

        def a_step1(tc, r):
            tt = 4 * tc + r
            sl = slice(tt * 128, (tt + 1) * 128)
            rs = slice(r * 128, (r + 1) * 128)
            i2 = tt % 2
            buf = tc % 2
            rx = rstdx[:, tt:tt + 1]
            P.op("act", ACTV(junk, xs[i2], AF.Square, accum_out=ssx[:, tt:tt + 1]),
                 reads=[Bxs[i2], Bst_t[tt]], writes=[Bst_t[tt]])
            if tt + 1 < 16:
                a_load(tt + 1)
            P.op("act", ACTV(sqx[:, tt:tt + 1], ssx[:, tt:tt + 1], AF.Sqrt, bias=epsb, scale=1.0 / D),
                 reads=[Bst_t[tt]], writes=[Bst_t[tt]])
            P.op("dve", RECIP(rstdx[:, tt:tt + 1], sqx[:, tt:tt + 1]), reads=[Bst_t[tt]], writes=[Bst_t[tt]])
            for k in range(8):
                P.op("pe", TR(psT_b[:, k * 128:(k + 1) * 128], xb[i2][:, k * 128:(k + 1) * 128], identb),
                     reads=[Bxb[i2], Bcst], writes=[BpsT])
            P.op("dve", TT(xT[buf][:, :, rs], psT_b.rearrange("p (k t) -> p k t", k=8), bc(gA3, [128, 8, 128]),
                           ALU.mult), reads=[BpsT, Bcst], writes=[BxT[buf][r]])

        def a_step2(tc, r):
            tt = 4 * tc + r
            sl = slice(tt * 128, (tt + 1) * 128)
            rs = slice(r * 128, (r + 1) * 128)
            i2 = tt % 2
            buf = tc % 2
            rx = rstdx[:, tt:tt + 1]
            for (c0, c1, pb, Bp) in ((0, 384, psA0, BpsA0), (384, 640, psA1, BpsA1), (640, 1152, psV, BpsV)):
                for k in range(8):
                    P.op("pe", MM(pb[:, 0:c1 - c0], xT[buf][:, k, rs], wAV[:, k, c0:c1], k == 0, k == 7),
                         reads=[BxT[buf][r], BwAV], writes=[Bp])
            P.op("act", ACTV(junk[:, 0:384], psA0[:, 0:384], AF.Square, accum_out=ssqkv[:, tt, 0:1]),
                 reads=[BpsA0, Bst_t[tt]], writes=[Bst_t[tt]])
            P.op("act", ACTV(junk[:, 0:256], psA1[:, 0:256], AF.Square, accum_out=ssqkv[:, tt, 1:2]),
                 reads=[BpsA1, Bst_t[tt]], writes=[Bst_t[tt]])
            P.op("dve", STT(t_a[:, tt, :], ssqkv[:, tt, :], rx, cst[:, C_DIVQ:C_DIVQ + 2], ALU.mult, ALU.mult),
                 reads=[Bst_t[tt], Bcst], writes=[Bst_t[tt]])
            P.op("dve", TS(t_a[:, tt, :], t_a[:, tt, :], rx, ALU.mult), reads=[Bst_t[tt]], writes=[Bst_t[tt]])
            P.op("act", ACTV(t_b[:, tt, :], t_a[:, tt, :], AF.Sqrt, bias=epsb, scale=1.0), reads=[Bst_t[tt]], writes=[Bst_t[tt]])
            P.op("dve", RECIP(t_c[:, tt, :], t_b[:, tt, :]), reads=[Bst_t[tt]], writes=[Bst_t[tt]])
            P.op("dve", TS(sqkv[:, tt, :], t_c[:, tt, :], rx, ALU.mult), reads=[Bst_t[tt]], writes=[Bst_t[tt]])
            P.op("dve", TS(cn[i2][:, 0:384], psA0[:, 0:384], sqkv[:, tt, 0:1], ALU.mult),
                 reads=[BpsA0, Bst_t[tt]], writes=[Bcn[i2]])
            P.op("dve", TS(cn[i2][:, 384:640], psA1[:, 0:256], sqkv[:, tt, 1:2], ALU.mult),
                 reads=[BpsA1, Bst_t[tt]], writes=[Bcn[i2]])

        def a_step3(tc, r):
            tt = 4 * tc + r
            sl = slice(tt * 128, (tt + 1) * 128)
            rs = slice(r * 128, (r + 1) * 128)
            i2 = tt % 2
            buf = tc % 2
            rx = rstdx[:, tt:tt + 1]
            for j in range(5):
                P.op("pe", TR(psT2_b[:, j * 128:(j + 1) * 128], cn[i2][:, j * 128:(j + 1) * 128], identb),
                     reads=[Bcn[i2], Bcst], writes=[BpsT2])
            P.op("dve", TT(cnT[:, :, sl], psT2_b[:, 0:640].rearrange("p (k t) -> p k t", k=5),
                           bc(gQ3, [128, 5, 128]), ALU.mult), reads=[BpsT2, Bcst], writes=[BcnT])
            P.op("act", ACTV(VD[:, tt, :, 0:128], psV.rearrange("p (h d) -> p h d", h=4), AF.Copy, scale=rx),
                 reads=[BpsV, Bst_t[tt]], writes=[BVD])
            P.op("dve", TS(diag[i2], identf, rx, ALU.mult), reads=[Bst_t[tt], Bcst], writes=[Bdiag[i2]])
            P.op("pe", MM(psB[:, rs], onesf, diag[i2], True, True), reads=[Bdiag[i2], Bst_t[tt]], writes=[BpsB])

        MAGIC = 12582912.0
        Bcs = [Buf(), Buf()]

        def a_tabprep(tc, si):
            chunk = slice(tc * 512, (tc + 1) * 512)
            invc = (C_INVD, C_INVM)[si]
            iv = cst[:, invc:invc + 1]
            cs_ = cs_set[si]
            P.op("dve", TS(u2[:, 0, :], posf[:, chunk], iv, ALU.mult), reads=[Bpos, Bcst], writes=[Btmp])
            P.op("dve", TS(u2[:, 1, :], posf[:, chunk], iv, ALU.mult, 0.25, ALU.add), reads=[Bpos, Bcst], writes=[Btmp])
            P.op("dve", TS(nn, u2, MAGIC, ALU.add, MAGIC, ALU.subtract), reads=[Btmp], writes=[Btmp])
            P.op("dve", TT(cs_, u2, nn, ALU.subtract), reads=[Btmp], writes=[Bcs[si]])
            P.op("act", ACTV(cs_, cs_, AF.Sin, scale=float(2 * np.pi)), reads=[Bcs[si]], writes=[Bcs[si]])

        def a_tables(tc):
            for si, (sgnc, Cout, Sout) in enumerate(((C_SGND, CD, SD), (C_SGNM, CMr, SMr))):
                cs_ = cs_set[si]
                P.op("dve", STT(Sout, cs_[:, 0, :], cst[:, sgnc:sgnc + 1], psB, ALU.mult, ALU.mult),
                     reads=[Bcs[si], BpsB, Bcst], writes=[Btab])
                P.op("dve", TT(Cout, cs_[:, 1, :], psB, ALU.mult), reads=[Bcs[si], BpsB], writes=[Btab])

        def a_feat(tc, i):
            buf = tc % 2
            chunk = slice(tc * 512, (tc + 1) * 512)
            for hf, (pb, Bp) in enumerate(((psF0, BpsF0), (psF1, BpsF1))):
                blk = 2 * i + hf
                for k in range(8):
                    P.op("pe", MM(pb, wF[:, k, blk * 128:(blk + 1) * 128], xT[buf][:, k, :], k == 0, k == 7),
                         reads=BxT[buf] + [BwF[blk // 6]], writes=[Bp])
            if i < 8:
                rows = slice(0, 128)
                Ct, St = CD, SD
                dest = (QTD if i < 4 else KTD)[:, i % 4, chunk]
            else:
                rows = slice(64, 96)
                Ct, St = CMr, SMr
                dest = kpeT[64:96, chunk]
            j2 = i % 2
            P.op("dve", TT(t1[j2][rows], psF0[rows], Ct[rows], ALU.mult), reads=[BpsF0, Btab], writes=[Bt12[j2]])
            P.op("dve", TT(t2[j2][rows], psF1[rows], St[rows], ALU.mult), reads=[BpsF1, Btab], writes=[Bt12[j2]])
            P.op("pool", TT(dest, t1[j2][rows], t2[j2][rows], ALU.add), reads=[Bt12[j2]], writes=[Bqk])


        a_load(0)
        for tc in range(4):
            for r in range(4):
                a_step1(tc, r)
                if tc > 0:
                    a_feat(tc - 1, 2 * r)
                a_step2(tc, r)
                if r < 2:
                    a_tabprep(tc, r)
                if tc > 0:
                    a_feat(tc - 1, 2 * r + 1)
                a_step3(tc, r)
            if tc > 0:
                a_feat(tc - 1, 8)
            if tc == 3:
                for h in range(4):
                    for half in range(2):
                        zrows = slice(64 * (1 - half), 64 * (1 - half) + 64)
                        P.op("dve", MSET(KTz[zrows, 2 * h + half, :], 0.0),
                             writes=[BKTz, BwAV, Bxs[0], Bxb[0], Bxb[1]] + BxT[0])
            a_tables(tc)
        for i in range(9):
            a_feat(3, i)
            if 4 <= i < 8:
                h = i - 4
                for half in range(2):
                    rows = slice(64 * half, 64 * half + 64)
                    P.op("act", ACTV(KTz[rows, 2 * h + half, :], KTD[rows, h, :], AF.Copy), reads=[Bqk], writes=[BKTz])

        dump("cnT", cnT, [128, 5, S], BF16, [BcnT])
        dump("QTD", QTD, [128, 4, S], BF16, [Bqk])
        dump("KTD", KTD, [128, 4, S], BF16, [Bqk])
        dump("kpeT", kpeT, [128, S], BF16, [Bqk])
        dump("VD", VD, [128, 16, 4, 129], BF16, [BVD])
        dump("rstdx", rstdx, [128, 16], F32, [Bst])
        P.fence()
        if "stopA" in dbg:
            P.emit(nc, block, sems, dsems, finals)
            return nc, dbg_d
        A.top = mark_after_KTz
        PT = [A.alloc([1024], BF16) for _ in range(3)]
        o_all = R1.rearrange("p a b -> p (a b)")[:, 0:16 * 1024].rearrange("p (t c) -> p t c", t=16)
        A1 = A.alloc([4, 128], F32)
        A2 = A.alloc([4, 128], F32)
        Dd = A.alloc([4, 128], F32)
        Dsq = A.alloc([4, 128], F32)
        rec4 = [A.alloc([4], F32) for _ in range(2)]
        BS = [Buf("S0"), Buf("S1"), Buf("S2")]
        BPT = [Buf(), Buf(), Buf()]
        Bacc = [Buf("acc0"), Buf("acc1")]
        Bo = Buf("o_all")
        BA1, BA2, BDd = Buf(), Buf(), Buf()
        Brec = [Buf(), Buf()]

        def run_attention(units, nS=2):
            groups = []
            for ui, u in enumerate(units):
                c = u["c"]
                gl = []
                for pr in range(2 * c):
                    gl.append(dict(ncols=1024, ents=[(2 * pr, 0, [(r, r * 128) for r in range(4)]),
                                                     (2 * pr + 1, 512, [(r, r * 128) for r in range(4)])]))
                gl.append(dict(ncols=1024, ents=[(4 * c, 0, [(r, r * 128) for r in range(4)]),
                                                 (4 * c + 1, 512, [(r, r * 128) for r in range(1, 4)])]))
                gl.append(dict(ncols=384, ents=[(4 * c + 2, 0, [(2, 0), (3, 128)]),
                                                (4 * c + 3, 0, [(3, 256)])]))
                for gi, g in enumerate(gl):
                    g["u"] = u
                    g["ui"] = ui
                    g["last"] = gi == len(gl) - 1
                    g["second_last"] = gi == len(gl) - 2
                    groups.append(g)

            def emit_qk(gidx):
                g = groups[gidx]
                u = g["u"]
                c = u["c"]
                Sg = bank(2 * (gidx % nS), 2)
                for (kb, base, rl) in g["ents"]:
                    r0 = rl[0][0]
                    c0 = base + rl[0][1]
                    n = len(rl)
                    isdiag = kb >= 4 * c
                    P.op("pe", MM(Sg[:, c0:c0 + n * 128], u["KT"][:, kb * 128:(kb + 1) * 128],
                                  u["QT"][:, (4 * c + r0) * 128:(4 * c + 4) * 128], True, not isdiag, skip=True),
                         writes=[BS[gidx % nS]])
                    if isdiag:
                        P.op("pe", MM(Sg[:, c0:c0 + 128], identb, mtrib, False, True, skip=True),
                             writes=[BS[gidx % nS]])

            def emit_exp_pv(gidx):
                g = groups[gidx]
                u = g["u"]
                c = u["c"]
                W = u["W"]
                Sg = bank(2 * (gidx % nS), 2)
                pt = PT[gidx % 3]
                P.op("act", ACTV(pt[:, 0:g["ncols"]], Sg[:, 0:g["ncols"]], AF.Exp, scale=u["scale"]),
                     reads=[BS[gidx % nS]], writes=[BPT[gidx % 3]])
                ci = g["ui"] % 2
                for (kb, base, rl) in g["ents"]:
                    for (r, col) in rl:
                        tok = u["acctok"](ci, r) if "acctok" in u else Bacc[ci]
                        P.op("pe", MM(u["acc"](ci, r), pt[:, base + col:base + col + 128], u["V"](kb),
                                      kb == 0 and (r % u["rper"]) == 0, kb == 4 * c + r, skip=True),
                             reads=[BPT[gidx % 3]], writes=[tok])
                if "post_half" in u:
                    if g["second_last"]:
                        u["post_half"](u, 0)
                    if g["last"]:
                        u["post_half"](u, 1)
                elif g["last"]:
                    u["post"](ci, u)

            G = len(groups)
            for g0 in range(min(nS - 1, G)):
                emit_qk(g0)
            for gidx in range(G):
                if gidx + nS - 1 < G:
                    emit_qk(gidx + nS - 1)
                emit_exp_pv(gidx)

        BaccD = [Buf("accD0"), Buf("accD1")]
        BrecD = [[Buf(), Buf()], [Buf(), Buf()]]
        BA1D, BA2D, BDdD, BDsqD = [[Buf(), Buf()] for _ in range(4)]

        def acc_diff(ci, r):
            return bank(6 + r // 2)[:, (r % 2) * 129:(r % 2) * 129 + 129]

        def acctok_diff(ci, r):
            return BaccD[r // 2]

        def post_diff_half(u, hb):
            h, half, c = u["h"], u["half"], u["c"]
            q0 = 2 * hb
            accv = bank(6 + hb)[:, 0:258].rearrange("p (s w) -> p s w", w=129)
            rc = rec4[half][:, q0:q0 + 2].rearrange("p (s o) -> p s o", o=1)
            P.op("dve", RECIP(rc, accv[:, :, 128:129]), reads=[BaccD[hb]], writes=[BrecD[half][hb]])
            dst = (A1 if half == 0 else A2)[:, q0:q0 + 2, :]
            P.op("dve", TT(dst, accv[:, :, 0:128], bc(rc, [128, 2, 128]), ALU.mult),
                 reads=[BaccD[hb], BrecD[half][hb]], writes=[BA1D[hb] if half == 0 else BA2D[hb]])
            if half == 1:
                Dd_, Dsq_ = Dd[:, q0:q0 + 2, :], Dsq[:, q0:q0 + 2, :]
                P.op("dve", STT(Dd_, A2[:, q0:q0 + 2, :], neglam[:, 0:1], A1[:, q0:q0 + 2, :], ALU.mult, ALU.add),
                     reads=[BA1D[hb], BA2D[hb], Bst], writes=[BDdD[hb]])
                P.op("pool", TT(Dsq_, Dd_, Dd_, ALU.mult), reads=[BDdD[hb]], writes=[BDsqD[hb]])
                P.op("dve", RSUM(dss[:, 4 * c + q0:4 * c + q0 + 2, h], Dsq_), reads=[BDsqD[hb]], writes=[Bst])
                P.op("pool", CP(o_all[:, 4 * c + q0:4 * c + q0 + 2, 512 + h * 128:512 + (h + 1) * 128], Dd_),
                     reads=[BDdD[hb]], writes=[Bo])

        units = []
        for h in range(4):
            for c in range(4):
                for half in range(2):
                    units.append(dict(KT=KTz[:, 2 * h + half, :], QT=QTD[:, h, :], W=129, rper=2, scale=64 ** -0.5, c=c, h=h,
                                      half=half, V=(lambda kb, h=h: VD[:, kb, h, :]), acc=acc_diff, acctok=acctok_diff,
                                      post_half=post_diff_half))
        run_attention(units, nS=3)
        dump("dss", dss, [128, 16, 4], F32, [Bst])
        dump("o_all", o_all, [128, 16, 1024], BF16, [Bo])
        P.fence()
        if "stopB" in dbg:
            P.emit(nc, block, sems, dsems, finals)
            return nc, dbg_d
        A.top = mark_VD
        QTM = A.alloc([8, S], BF16)
        KTM = A.alloc([8, S], BF16)
        VM = A.alloc([16, 8, 65], BF16)
        wUQ = A.alloc([3, 1536], BF16)
        wUKV = A.alloc([2, 1024], BF16)
        CM = A.alloc([512], F32)
        SM = A.alloc([512], F32)
        u2 = A.alloc([2, 512], F32)
        nf = A.alloc([2, 512], F32)
        cs = A.alloc([2, 512], F32)
        t1c = [A.alloc([512], F32)] * 2
        t2c = [A.alloc([512], F32)] * 2
        PT = [A.alloc([1024], BF16) for _ in range(3)]
        recm = [A.alloc([4], F32) for _ in range(2)]
        BwUQ, BwUKV, BVM, BQTM, BKTM, BQTMn, BKTMn = Buf(), Buf(), Buf(), Buf(), Buf(), Buf(), Buf()
        Btab, Btmp, Bt12c = Buf(), Buf(), [Buf()] * 2
        BpsC3 = [(Buf(), Buf(), Buf()), (Buf(), Buf(), Buf())]
        P.dma("pool", DMA(wUQ, wUQ_d.rearrange("(k p) c -> p k c", p=128)), writes=[BwUQ])
        P.dma("pool", DMA(wUKV, wUKV_d.rearrange("(k p) c -> p k c", p=128)), writes=[BwUKV])
        P.op("pool", MSET(VM[:, :, :, 64:65], 1.0), writes=[BVM])
        for h in range(8):
            P.op("pool", CP(KTM[64:96, h, :], kpeT[64:96, :]), writes=[BKTM])
        psVm = bank(3)
        BpsVm = Buf()
        for tt in range(16):
            sl = slice(tt * 128, (tt + 1) * 128)
            for k in range(2):
                P.op("pe", MM(psVm, cnT[:, 3 + k, sl], wUKV[:, k, 512:1024], k == 0, k == 1),
                     reads=[BwUKV], writes=[BpsVm])
            P.op("act", ACTV(VM[:, tt, :, 0:64], psVm.rearrange("p (h d) -> p h d", h=8), AF.Copy),
                 reads=[BpsVm], writes=[BVM])
        for tc in range(4):
            chunk = slice(tc * 512, (tc + 1) * 512)
            iv = cst[:, C_INVM:C_INVM + 1]
            P.op("dve", TS(u2[:, 0, :], posf[:, chunk], iv, ALU.mult), writes=[Btmp, Bt12c[1]])
            P.op("dve", TS(u2[:, 1, :], posf[:, chunk], iv, ALU.mult, 0.25, ALU.add), writes=[Btmp, Bt12c[1]])
            P.op("dve", TS(nf, u2, MAGIC, ALU.add, MAGIC, ALU.subtract), reads=[Btmp], writes=[Btmp])
            P.op("dve", TT(cs, u2, nf, ALU.subtract), reads=[Btmp], writes=[Btmp])
            P.op("act", ACTV(cs, cs, AF.Sin, scale=float(2 * np.pi)), reads=[Btmp], writes=[Btmp])
            P.op("dve", TS(SM, cs[:, 0, :], cst[:, C_SGNM:C_SGNM + 1], ALU.mult), reads=[Btmp], writes=[Btab])
            P.op("dve", CP(CM, cs[:, 1, :]), reads=[Btmp], writes=[Btab])
            for h in range(8):
                par = h % 2
                psQ0, psQ1, psK = bank(4 * par), bank(4 * par + 1), bank(4 * par + 2)
                BpsQ0, BpsQ1, BpsK = BpsC3[par]
                for hf, (pb, Bp) in enumerate(((psQ0, BpsQ0), (psQ1, BpsQ1))):
                    for k in range(3):
                        P.op("pe", MM(pb[0:96, :], wUQ[:, k, hf * 768 + h * 96:hf * 768 + (h + 1) * 96], cnT[:, k, chunk],
                                      k == 0, k == 2), reads=[BwUQ], writes=[Bp])
                for k in range(2):
                    P.op("pe", MM(psK[0:64, :], wUKV[:, k, h * 64:(h + 1) * 64], cnT[:, 3 + k, chunk], k == 0, k == 1),
                         reads=[BwUKV], writes=[BpsK])
                P.op("act", ACTV(QTM[0:64, h, chunk], psQ0[0:64, :], AF.Copy), reads=[BpsQ0], writes=[BQTMn])
                P.op("dve", TT(t1c[par][64:96], psQ0[64:96, :], CM[64:96], ALU.mult), reads=[BpsQ0, Btab], writes=[Bt12c[par]])
                P.op("dve", TT(t2c[par][64:96], psQ1[64:96, :], SM[64:96], ALU.mult), reads=[BpsQ1, Btab], writes=[Bt12c[par]])
                P.op("dve", TT(QTM[64:96, h, chunk], t1c[par][64:96], t2c[par][64:96], ALU.add), reads=[Bt12c[par]], writes=[BQTM])
                P.op("act", ACTV(KTM[0:64, h, chunk], psK[0:64, :], AF.Copy), reads=[BpsK], writes=[BKTMn])
        dump("QTM", QTM, [128, 8, S], BF16, [BQTM, BQTMn])
        dump("KTM", KTM, [128, 8, S], BF16, [BKTM, BKTMn])
        dump("VM", VM, [128, 16, 8, 65], BF16, [BVM])
        P.fence()

        def acc_mla(ci, r):
            return bank(6 + ci)[:, r * 65:(r + 1) * 65]

        def post_mla(ci, u):
            h, c = u["h"], u["c"]
            accv = bank(6 + ci)[:, 0:260].rearrange("p (r w) -> p r w", w=65)
            rc = recm[ci].rearrange("p (r o) -> p r o", o=1)
            P.op("dve", RECIP(rc, accv[:, :, 64:65]), reads=[Bacc[ci]], writes=[Brec[ci]])
            P.op("dve", TT(o_all[:, 4 * c:4 * c + 4, h * 64:(h + 1) * 64], accv[:, :, 0:64], bc(rc, [128, 4, 64]),
                           ALU.mult), reads=[Bacc[ci], Brec[ci]], writes=[Bo])

        units = []
        for h in range(8):
            for c in range(4):
                units.append(dict(KT=KTM[0:96, h, :], QT=QTM[0:96, h, :], W=65, rper=4, scale=96 ** -0.5, c=c, h=h,
                                  V=(lambda kb, h=h: VM[:, kb, h, :]), acc=acc_mla, post=post_mla))
        run_attention(units, nS=3)
        P.fence()

        A.top = mark_cnT
        hres = A.alloc([16, 1024], F32)
        ob = [A.alloc([1024], F32) for _ in range(2)]
        mark_after_hres = A.top
        wO = A.alloc([8, 1024], BF16)
        mixT = [A.alloc([8, 128], BF16) for _ in range(2)]
        xs2 = [A.alloc([1024], F32) for _ in range(2)]
        BwO, Bmix, Bxs2, Bob, Bh = Buf(), [Buf(), Buf()], [Buf(), Buf()], [Buf(), Buf()], [Buf() for _ in range(16)]
        P.dma("pool", DMA(wO, wO_d.rearrange("(k p) c -> p k c", p=128)), writes=[BwO])
        P.op("act", ACTV(dsq, dss, AF.Sqrt, bias=epsb, scale=1.0 / 128), reads=[Bst], writes=[Bst])
        P.op("dve", RECIP(drd, dsq), reads=[Bst], writes=[Bst])
        P.op("dve", TS(drd, drd, 1.0 - LAM_INIT, ALU.mult), reads=[Bst], writes=[Bst])
        subg = rowbc[:, R_SUBG:R_SUBG + 128].rearrange("p (a b) -> p a b", a=1)
        fing = rowbc[:, R_FING:R_FING + 1024]
        BpsTe, BpsO = Buf(), [Buf(), Buf()]
        psTe = bank_bf(0)
        Bo_t = [Buf() for _ in range(16)]
        for tt in range(16):
            od = o_all[:, tt, 512:1024].rearrange("p (h d) -> p h d", h=4)
            P.op("dve", TT(od, od, bc(drd[:, tt, :].rearrange("p (h o) -> p h o", o=1), [128, 4, 128]), ALU.mult),
                 reads=[Bo, Bst], writes=[Bo_t[tt]])
            P.op("dve", TT(od, od, bc(subg, [128, 4, 128]), ALU.mult), reads=[Bo_t[tt], Bcst], writes=[Bo_t[tt]])

        def e_transposes(tt):
            i2 = tt % 2
            for c8 in range(8):
                P.op("pe", TR(psTe[:, c8 * 128:(c8 + 1) * 128], o_all[:, tt, c8 * 128:(c8 + 1) * 128], identb),
                     reads=[Bo, Bo_t[tt]], writes=[BpsTe])
            P.op("act", ACTV(mixT[i2], psTe.rearrange("p (c t) -> p c t", c=8), AF.Copy), reads=[BpsTe], writes=[Bmix[i2]])

        def e_matmuls(tt):
            i2 = tt % 2
            sl = slice(tt * 128, (tt + 1) * 128)
            psO = bank(2 + 2 * i2, 2)
            for hf in range(2):
                for c8 in range(8):
                    P.op("pe", MM(psO[:, hf * 512:(hf + 1) * 512], mixT[i2][:, c8, :], wO[:, c8, hf * 512:(hf + 1) * 512],
                                  c8 == 0, c8 == 7), reads=[Bmix[i2], BwO], writes=[BpsO[i2]])
            P.dma("sp", DMA(xs2[i2], x_d[sl, :]), writes=[Bxs2[i2]])
            P.op("dve", TT(hres[:, tt, :], psO, xs2[i2], ALU.add), reads=[BpsO[i2], Bxs2[i2]], writes=[Bh[tt]])

        e_transposes(0)
        for tt in range(16):
            if tt + 1 < 16:
                e_transposes(tt + 1)
            e_matmuls(tt)
        dump("hres", hres, [128, 16, 1024], F32, Bh)
        FINAL_DONE = [False]

        def final_tile(tt):
            i2 = tt % 2
            sl = slice(tt * 128, (tt + 1) * 128)
            P.op("act", ACTV(junk, hres[:, tt, :], AF.Square, accum_out=ssf[:, tt:tt + 1]), reads=[Bh[tt], Bst],
                 writes=[Bst])
            P.op("act", ACTV(sqf[:, tt:tt + 1], ssf[:, tt:tt + 1], AF.Sqrt, bias=epsb, scale=1.0 / D), reads=[Bst], writes=[Bst])
            P.op("dve", RECIP(rf[:, tt:tt + 1], sqf[:, tt:tt + 1]), reads=[Bst], writes=[Bst])
            P.op("dve", STT(ob[i2], hres[:, tt, :], rf[:, tt:tt + 1], fing, ALU.mult, ALU.mult),
                 reads=[Bh[tt], Bst, Bcst], writes=[Bob[i2]])
            finals.append(P.dma("sp", DMA(out_d[sl, :], ob[i2]), reads=[Bob[i2]]))

        if "noF" not in dbg and "dense" not in dbg:
            P.fence()
            A.top = mark_after_hres
            BIG = float(1 << 16)
            NTL = 48
            NSL = NTL * 256
            hn_d = nc.dram_tensor("hn_scr", [S, D], BF16).ap()
            tos_d = nc.dram_tensor("tos_scr", [NSL, 16], I32).ap()
            Y_d = nc.dram_tensor("y_scr", [NSL, D], BF16).ap()
            BhnD, BtosD, BYd = Buf(), Buf(), Buf()
            R1f = R1.rearrange("p a b -> p (a b)")
            Wg = [R1f[:, i * 6144: i * 6144 + 2048].rearrange("p (k f) -> p k f", k=8) for i in range(3)]
            Wu = [R1f[:, i * 6144 + 2048: i * 6144 + 4096].rearrange("p (k f) -> p k f", k=8) for i in range(3)]
            Wd = [R1f[:, i * 6144 + 4096: i * 6144 + 6144].rearrange("p (c d) -> p c d", c=2) for i in range(3)]
            wR32 = A.alloc([8, 36], F32)
            Whi = A.alloc([8, 36], BF16)
            Wlo = A.alloc([8, 36], BF16)
            wRt = A.alloc([8, 36], F32)
            ssh = A.alloc([16], F32)
            sqh = A.alloc([16], F32)
            rh = A.alloc([16], F32)
            lg = A.alloc([16, 36], F32)
            utri = A.alloc([128], BF16)
            onesb = A.alloc([128], BF16)

            def f16(n):
                return A.alloc([16, n], F32)
            gmax, sume, pg, v0, v1, dlt, exd, w1, w2, gi8, i1, i2_, eid1, eid2, rank1, rank2 = [A.alloc([16], F32) for _ in range(16)]
            ohg, gsh = f16(4), f16(4)
            selg = A.alloc([64, 8], F32)
            sel, m1, m2, sel2, tm8 = f16(8), f16(8), f16(8), f16(8), f16(8)
            oh1, oh2, OH, incl, t32 = f16(32), f16(32), f16(32), f16(32), f16(32)
            OHb = A.alloc([16, 32], BF16)
            g12 = A.alloc([16, 2], F32)
            posf2 = A.alloc([16, 2], F32)
            posi2 = A.alloc([16, 2], I32)
            tokf = A.alloc([16], F32)
            tokrow = A.alloc([16, 16], I32)
            ne, nt, csum, csum2, excl, sbase = [A.alloc([32], F32) for _ in range(6)]
            eot, used, widxf = [A.alloc([NTL], F32) for _ in range(3)]
            widx = A.alloc([NTL], I32)
            tosT = A.alloc([2 * NTL, 16], I32)
            tosf, yvalid, yidxf = [A.alloc([2 * NTL], F32) for _ in range(3)]
            yidx = A.alloc([2 * NTL], I32)
            m_ = A.top
            hn32 = [A.alloc([1024], F32) for _ in range(2)]
            hnb = [A.alloc([1024], BF16) for _ in range(2)]
            hnl = [A.alloc([1024], BF16) for _ in range(2)]
            hiT = [A.alloc([8, 128], BF16) for _ in range(2)]
            loT = [A.alloc([8, 128], BF16) for _ in range(2)]
            Bje = A.alloc([NTL, 32], F32)
            Aje = A.alloc([NTL, 32], F32)
            bigt = A.alloc([NSL * 16 // 128], I32)
            top_r = A.top
            A.top = m_
            Xg = [[A.alloc([1024], BF16) for _ in range(2)] for _ in range(3)]
            sa = A.alloc([512], F32)
            hdnT = A.alloc([2, 256], BF16)
            xgT = [A.alloc([8, 256], BF16) for _ in range(2)]
            ysb = [A.alloc([1024], BF16) for _ in range(2)]
            yk = [A.alloc([1024], BF16) for _ in range(4)]
            A.top = max(A.top, top_r)
            iota = rowbc[:, R_IOTA:R_IOTA + 96]
            c8g = rowbc[:, R_C8G:R_C8G + 4]
            pcol = cst[:, C_PCOL:C_PCOL + 1]
            pmB = cst[:, C_PMB:C_PMB + 1]
            BwR, Bhn32, Bhnb, Brt, Bcnt = Buf(), [Buf(), Buf()], [Buf(), Buf()], Buf(), Buf()
            Bhnl, BhiT, BloT, BpsH, BpsLo = [[Buf(), Buf()] for _ in range(5)]
            BwR0 = Buf()
            P.dma("sp", DMA(wR32, wR_d.rearrange("(k p) c -> p k c", p=128)), writes=[BwR0])
            P.op("dve", CP(Whi, wR32), reads=[BwR0], writes=[BwR])
            P.op("dve", TT(wRt, wR32, Whi, ALU.subtract), reads=[BwR0, BwR], writes=[BwR])
            P.op("dve", CP(Wlo, wRt), reads=[BwR], writes=[BwR])
            P.dma("pool", DMA(utri, utri_d[:, :]), writes=[Bcnt])
            P.op("dve", MSET(ssh, 0.0), writes=[Brt])
            P.op("pool", MSET(onesb, 1.0), writes=[Bcnt])
            P.op("pool", MSET(bigt, 1 << 16), writes=[Bcnt])
            P.dma("sp", DMA(tos_d.rearrange("(p r) w -> p (r w)", p=128), bigt), reads=[Bcnt], writes=[BtosD])
            psX = bank(6, 2)
            psL = bank(1)
            BpsX, BpsL = Buf(), Buf()
            gFbc = rowbc[:, R_GF:R_GF + 1024]
            for tt in range(16):
                P.op("act", ACTV(junk, hres[:, tt, :], AF.Square, accum_out=ssh[:, tt:tt + 1]), reads=[Bh[tt], Brt],
                     writes=[Brt])
            P.op("act", ACTV(sqh, ssh, AF.Sqrt, bias=epsb, scale=1.0 / D), reads=[Brt], writes=[Brt])
            P.op("dve", RECIP(rh, sqh), reads=[Brt], writes=[Brt])
            Blg = Buf()
            BpsXs, BpsLs = [Buf(), Buf()], [Buf(), Buf()]
            def rt_front(tt):
                i2 = tt % 2
                sl = slice(tt * 128, (tt + 1) * 128)
                P.op("dve", STT(hn32[i2], hres[:, tt, :], rh[:, tt:tt + 1], gFbc, ALU.mult, ALU.mult),
                     reads=[Bh[tt], Brt, Bcst], writes=[Bhn32[i2]])
                P.op("act", ACTV(hnb[i2], hn32[i2], AF.Copy), reads=[Bhn32[i2]], writes=[Bhnb[i2]])
                P.dma("sp", DMA(hn_d[sl, :], hnb[i2]), reads=[Bhnb[i2]], writes=[BhnD])
                P.op("dve", TT(hnl[i2], hn32[i2], hnb[i2], ALU.subtract), reads=[Bhn32[i2], Bhnb[i2]], writes=[Bhnl[i2]])
                pH, pL = bank_bf(4 + 2 * i2), bank_bf(5 + 2 * i2)
                for k in range(8):
                    P.op("pe", TR(pH[:, k * 128:(k + 1) * 128], hnb[i2][:, k * 128:(k + 1) * 128], identb),
                         reads=[Bhnb[i2]], writes=[BpsH[i2]])
                for k in range(8):
                    P.op("pe", TR(pL[:, k * 128:(k + 1) * 128], hnl[i2][:, k * 128:(k + 1) * 128], identb),
                         reads=[Bhnl[i2]], writes=[BpsLo[i2]])
                P.op("act", ACTV(hiT[i2], pH.rearrange("p (k t) -> p k t", k=8), AF.Copy), reads=[BpsH[i2]], writes=[BhiT[i2]])
                P.op("act", ACTV(loT[i2], pL.rearrange("p (k t) -> p k t", k=8), AF.Copy), reads=[BpsLo[i2]], writes=[BloT[i2]])

            def rt_back(tt):
                i2 = tt % 2
                pl = bank(1 + i2)[:, 0:36]
                n_ = 0
                for (xT_, W_, Bx) in ((hiT[i2], Whi, BhiT[i2]), (loT[i2], Whi, BloT[i2]), (hiT[i2], Wlo, BhiT[i2])):
                    for k in range(8):
                        P.op("pe", MM(pl, xT_[:, k, :], W_[:, k, :], n_ == 0, n_ == 23), reads=[Bx, BwR], writes=[BpsLs[i2]])
                        n_ += 1
                P.op("act", ACTV(lg[:, tt, :], pl, AF.Copy), reads=[BpsLs[i2]], writes=[Blg])

            rt_front(0)
            for tt in range(16):
                if tt + 1 < 16:
                    rt_front(tt + 1)
                rt_back(tt)

            def R(eng, fn):
                return P.op(eng, fn, reads=[Brt, Blg, Bcst], writes=[Brt])

            def col(t):
                return t.rearrange("p (t o) -> p t o", o=1)
            R("dve", TT(lg, lg, bc(rowbc[:, R_BR:R_BR + 36].rearrange("p (o c) -> p o c", o=1), [128, 16, 36]), ALU.add))
            gl = lg[:, :, 0:4]
            R("dve", lambda e: e.tensor_reduce(out=gmax, in_=gl, axis=AX.X, op=ALU.max))
            R("dve", TT(ohg, gl, bc(col(gmax), [128, 16, 4]), ALU.is_equal))
            R("dve", TT(gsh, gl, bc(col(gmax), [128, 16, 4]), ALU.subtract))
            R("act", ACTV(gsh, gsh, AF.Exp))
            R("dve", RSUM(sume, gsh))
            R("dve", RECIP(pg, sume))
            R("dve", CP(t32, lg[:, :, 4:36]))
            el = t32.rearrange("p t (g e) -> p (t g) e", g=4)
            R("dve", TT(selg, el, bc(ohg.rearrange("p t g -> p (t g)").rearrange("p (x o) -> p x o", o=1), [128, 64, 8]), ALU.mult))
            R("dve", RSUM(sel, selg.rearrange("p (t g) e -> p t e g", g=4)))
            R("dve", lambda e: e.tensor_reduce(out=v0, in_=sel, axis=AX.X, op=ALU.max))
            R("dve", TT(m1, sel, bc(col(v0), [128, 16, 8]), ALU.is_equal))
            R("dve", STT(sel2, m1, -1e30, sel, ALU.mult, ALU.add))
            R("dve", lambda e: e.tensor_reduce(out=v1, in_=sel2, axis=AX.X, op=ALU.max))
            R("dve", TT(m2, sel2, bc(col(v1), [128, 16, 8]), ALU.is_equal))
            R("dve", TT(dlt, v1, v0, ALU.subtract))
            R("act", ACTV(exd, dlt, AF.Exp))
            R("dve", TS(w1, exd, 1.0, ALU.add))
            R("dve", RECIP(w1, w1))
            R("dve", TT(w2, exd, w1, ALU.mult))
            R("dve", TT(g12[:, :, 0], w1, pg, ALU.mult))
            R("dve", TT(g12[:, :, 1], w2, pg, ALU.mult))
            R("dve", TT(gsh, ohg, bc(c8g.rearrange("p (o g) -> p o g", o=1), [128, 16, 4]), ALU.mult))
            R("dve", RSUM(gi8, gsh))
            io8 = bc(iota[:, 0:8].rearrange("p (o e) -> p o e", o=1), [128, 16, 8])
            R("dve", TT(tm8, m1, io8, ALU.mult))
            R("dve", RSUM(i1, tm8))
            R("dve", TT(tm8, m2, io8, ALU.mult))
            R("dve", RSUM(i2_, tm8))
            R("dve", TT(eid1, gi8, i1, ALU.add))
            R("dve", TT(eid2, gi8, i2_, ALU.add))
            io32 = bc(iota[:, 0:32].rearrange("p (o e) -> p o e", o=1), [128, 16, 32])
            R("dve", TT(oh1, io32, bc(col(eid1), [128, 16, 32]), ALU.is_equal))
            R("dve", TT(oh2, io32, bc(col(eid2), [128, 16, 32]), ALU.is_equal))
            R("dve", TT(OH, oh1, oh2, ALU.add))
            R("dve", CP(OHb, OH))
            psC = bank(0)
            psN = bank(2)
            BpsC, BpsN = Buf(), Buf()
            for tt in range(16):
                for j in range(tt):
                    P.op("pe", MM(psC[:, tt * 32:(tt + 1) * 32], onesb, OHb[:, j, :], j == 0, False, skip=True),
                         reads=[Brt, Bcnt], writes=[BpsC])
                P.op("pe", MM(psC[:, tt * 32:(tt + 1) * 32], utri, OHb[:, tt, :], tt == 0, True, skip=True),
                     reads=[Brt, Bcnt], writes=[BpsC])
            for tt in range(16):
                P.op("pe", MM(psN[:, 0:32], onesb, OHb[:, tt, :], tt == 0, tt == 15), reads=[Brt, Bcnt], writes=[BpsN])
            P.op("dve", CP(incl, psC.rearrange("p (t e) -> p t e", t=16)), reads=[BpsC], writes=[Brt])
            P.op("dve", CP(ne, psN[:, 0:32]), reads=[BpsN], writes=[Brt])
            R("dve", TT(t32, oh1, incl, ALU.mult))
            R("dve", RSUM(rank1, t32))
            R("dve", TT(t32, oh2, incl, ALU.mult))
            R("dve", RSUM(rank2, t32))
            R("dve", TSS(nt, ne, 0.0, ALU.is_gt))
            for j in range(1, 8):
                R("dve", STT(nt, ne, 256.0 * j, nt, ALU.is_gt, ALU.add))
            R("dve", CP(csum, nt))
            cur, oth = csum, csum2
            for s_ in (1, 2, 4, 8, 16):
                R("dve", CP(oth[:, 0:s_], cur[:, 0:s_]))
                R("dve", TT(oth[:, s_:32], cur[:, s_:32], cur[:, 0:32 - s_], ALU.add))
                cur, oth = oth, cur
            cfin = cur
            R("dve", TT(excl, cfin, nt, ALU.subtract))
            R("dve", TS(sbase, excl, 256.0, ALU.mult))
            sb3 = bc(sbase.rearrange("p (o e) -> p o e", o=1), [128, 16, 32])
            R("dve", TT(t32, oh1, sb3, ALU.mult))
            R("dve", RSUM(posf2[:, :, 0], t32))
            R("dve", TT(t32, oh2, sb3, ALU.mult))
            R("dve", RSUM(posf2[:, :, 1], t32))
            R("dve", TT(posf2[:, :, 0], posf2[:, :, 0], rank1, ALU.add))
            R("dve", TT(posf2[:, :, 1], posf2[:, :, 1], rank2, ALU.add))
            R("dve", TS(posf2, posf2, -1.0, ALU.add))
            R("dve", CP(posi2, posf2))
            jt = bc(iota[:, 0:NTL].rearrange("p (j o) -> p j o", o=1), [128, NTL, 32])
            R("dve", TT(Aje, jt, bc(excl.rearrange("p (o e) -> p o e", o=1), [128, NTL, 32]), ALU.is_ge))
            R("dve", TT(Bje, jt, bc(cfin.rearrange("p (o e) -> p o e", o=1), [128, NTL, 32]), ALU.is_lt))
            R("dve", TT(Aje, Aje, Bje, ALU.mult))
            R("dve", RSUM(used, Aje))
            R("dve", TT(Bje, Aje, bc(iota[:, 0:32].rearrange("p (o e) -> p o e", o=1), [128, NTL, 32]), ALU.mult))
            R("dve", RSUM(eot, Bje))
            R("dve", TS(widxf, eot, 128.0, ALU.mult, pcol, ALU.add))
            R("dve", TS(used, used, -BIG, ALU.mult, BIG, ALU.add))
            R("dve", TT(widxf, widxf, used, ALU.add))
            R("dve", CP(widx, widxf))
            R("dve", TS(tokf, iota[:, 0:16], 128.0, ALU.mult, pcol, ALU.add))
            R("dve", CP(tokrow, bc(col(tokf), [128, 16, 16])))
            dump("posi2", posi2, [128, 16, 2], I32, [Brt])
            dump("widx", widx, [128, NTL], I32, [Brt])
            dump("g12", g12, [128, 16, 2], F32, [Brt])
            Btos_list = []
            for tt in range(16):
                for k in range(2):
                    bt = Buf()
                    Btos_list.append(bt)
                    P.dma("pool", IDMA(tos_d[:, :], tokrow[:, tt, :], out_off=posi2[:, tt, k:k + 1], bound=NSL - 1),
                          reads=[Brt, BtosD], writes=[bt])
            BtosT = Buf()
            P.dma("sp", DMA(tosT, tos_d.rearrange("(js p) w -> p js w", p=128)), reads=[BtosD] + Btos_list, writes=[BtosT])
            P.op("dve", CP(tosf, tosT[:, :, 0]), reads=[BtosT], writes=[BtosT])
            P.op("dve", TSS(yvalid, tosf, 2048.0, ALU.is_lt), reads=[BtosT], writes=[BtosT])
            P.op("dve", TS(yidxf, rowbc[:, R_IOTA:R_IOTA + 2 * NTL], 128.0, ALU.mult, pmB, ALU.add),
                 reads=[BtosT, Bcst], writes=[BtosT])
            P.op("dve", TT(yidxf, yidxf, yvalid, ALU.mult), reads=[BtosT], writes=[BtosT])
            P.op("dve", TS(yidxf, yidxf, BIG, ALU.add), reads=[BtosT], writes=[BtosT])
            P.op("dve", CP(yidx, yidxf), reads=[BtosT], writes=[BtosT])
            dump("tosT", tosT, [128, 2 * NTL, 16], I32, [BtosT])
            dump("yidx", yidx, [128, 2 * NTL], I32, [BtosT])
            P.fence()
            BXg0 = Buf()
            for a_ in range(3):
                for b_ in range(2):
                    P.op("pool", MSET(Xg[a_][b_], 0.0), writes=[BXg0])
            BWg, BWu, BWd = [Buf(), Buf(), Buf()], [Buf(), Buf(), Buf()], [Buf(), Buf(), Buf()]
            BYd_list = []
            BXg = [[Buf(), Buf()], [Buf(), Buf()], [Buf(), Buf()]]
            BxgT = [Buf(), Buf()]
            Bsa, Bhd, Bpa, Bpu, Bpy, Bysb = Buf(), Buf(), Buf(), Buf(), [Buf(), Buf()], [Buf(), Buf()]
            BpsXg = [Buf(), Buf()]
            psa, psu = bank(0), bank(1)
            NT_RUN = NTL
            for nm in dbg:
                if nm.startswith("ntl"):
                    NT_RUN = int(nm[3:])
            def ffn_xgathers(j):
                wb = j % 3
                for s_ in range(2):
                    js = 2 * j + s_
                    P.dma("pool", IDMA(Xg[wb][s_], hn_d[:, :], in_off=tosT[:, js, 0:1], bound=S - 1),
                          reads=[BtosT, BhnD, BXg0], writes=[BXg[wb][s_]])

            def ffn_gathers(j):
                wb = j % 3
                P.dma("pool", IDMA(Wg[wb].rearrange("p k f -> p (k f)"), wGt_d[:, :], in_off=widx[:, j:j + 1], bound=4095),
                      reads=[Brt], writes=[BWg[wb]])
                P.dma("pool", IDMA(Wu[wb].rearrange("p k f -> p (k f)"), wUt_d[:, :], in_off=widx[:, j:j + 1], bound=4095),
                      reads=[Brt], writes=[BWu[wb]])
                P.dma("pool", IDMA(Wd[wb].rearrange("p c d -> p (c d)"), wDt_d[:, :], in_off=widx[:, j:j + 1], bound=4095),
                      reads=[Brt], writes=[BWd[wb]])

            def ffn_transposes(j):
                wb = j % 3
                xb_ = j % 2
                for s_ in range(2):
                    pX = bank_bf(6 + s_)
                    for k in range(8):
                        P.op("pe", TR(pX[:, k * 128:(k + 1) * 128], Xg[wb][s_][:, k * 128:(k + 1) * 128], identb),
                             reads=[BXg[wb][s_]], writes=[BpsXg[s_]])
                    P.op("act" if s_ == 0 else "dve",
                         (ACTV(xgT[xb_][:, :, s_ * 128:(s_ + 1) * 128], pX.rearrange("p (k t) -> p k t", k=8), AF.Copy) if s_ == 0
                          else CP(xgT[xb_][:, :, s_ * 128:(s_ + 1) * 128], pX.rearrange("p (k t) -> p k t", k=8))),
                         reads=[BpsXg[s_]], writes=[BxgT[xb_]])

            def ffn_compute(j):
                wb = j % 3
                xb_ = j % 2
                for fc in range(4):
                    pb, Bp = (psa, Bpa) if fc < 2 else (psu, Bpu)
                    Wsrc = Wg[wb] if fc < 2 else Wu[wb]
                    BWs = BWg[wb] if fc < 2 else BWu[wb]
                    for k in range(8):
                        P.op("pe", MM(pb[:, (fc % 2) * 256:(fc % 2) * 256 + 256], Wsrc[:, k, (fc % 2) * 128:(fc % 2) * 128 + 128],
                                      xgT[xb_][:, k, :], k == 0, k == 7), reads=[BWs, BxgT[xb_]], writes=[Bp])
                P.op("act", ACTV(sa, psa, AF.Silu), reads=[Bpa], writes=[Bsa])
                P.op("dve", TT(hdnT.rearrange("p c t -> p (c t)"), sa, psu, ALU.mult), reads=[Bsa, Bpu], writes=[Bhd])

            def ffn_down(j):
                wb = j % 3
                for s_ in range(2):
                    js = 2 * j + s_
                    ys = js % 2
                    py = bank(2 + 2 * ys, 2)
                    for hf in range(2):
                        for c2 in range(2):
                            P.op("pe", MM(py[:, hf * 512:(hf + 1) * 512], hdnT[:, c2, s_ * 128:(s_ + 1) * 128],
                                          Wd[wb][:, c2, hf * 512:(hf + 1) * 512], c2 == 0, c2 == 1),
                                 reads=[Bhd, BWd[wb]], writes=[Bpy[ys]])
                    P.op("act" if ys == 0 else "dve",
                         (ACTV(ysb[ys], py, AF.Copy) if ys == 0 else CP(ysb[ys], py)), reads=[Bpy[ys]], writes=[Bysb[ys]])
                    byd = Buf()
                    BYd_list.append(byd)
                    P.dma("sp", DMA(Y_d[js * 128:(js + 1) * 128, :], ysb[ys]), reads=[Bysb[ys]], writes=[byd])

            for j0 in range(min(3, NT_RUN)):
                ffn_xgathers(j0)
                ffn_gathers(j0)
            ffn_transposes(0)
            if NT_RUN > 3:
                ffn_xgathers(3)
            for j in range(NT_RUN):
                ffn_compute(j)
                if j + 1 < NT_RUN:
                    ffn_transposes(j + 1)
                    if j + 4 < NT_RUN:
                        ffn_xgathers(j + 4)
                ffn_down(j)
                if j + 3 < NT_RUN:
                    ffn_gathers(j + 3)
            P.fence()
            yk_all = list(yk) + list(ysb)
            Byk = [Buf() for _ in range(len(yk_all))]
            cnt_ = 0
            for tt in range(16):
                for k in range(2):
                    bi = cnt_ % len(yk_all)
                    cnt_ += 1
                    P.dma("pool", IDMA(yk_all[bi], Y_d[:, :], in_off=posi2[:, tt, k:k + 1], bound=NSL - 1),
                          reads=BYd_list + [Brt], writes=[Byk[bi]])
                    P.op("dve", STT(hres[:, tt, :], yk_all[bi], g12[:, tt, k:k + 1], hres[:, tt, :], ALU.mult, ALU.add),
                         reads=[Byk[bi], Brt, Bh[tt]], writes=[Bh[tt]])
                if tt >= 1:
                    final_tile(tt - 1)
            final_tile(15)
            FINAL_DONE[0] = True
        elif "noF" not in dbg:
            P.fence()
            A.top = mark_after_hres
            gF_off = R_GF
            hn32 = [A.alloc([1024], F32) for _ in range(2)]
            hnT32 = A.alloc([8, 128], F32)
            wR32 = A.alloc([8, 36], F32)
            hnT = R1.rearrange("p a b -> p (a b)")[:, 0:8 * S].rearrange("p (k t) -> p k t", k=8)
            ssh = A.alloc([16], F32)
            sqh = A.alloc([16], F32)
            rh = A.alloc([16], F32)
            lg = A.alloc([36], F32)
            g8 = A.alloc([8], F32)
            m8 = A.alloc([8], F32)
            m8b = A.alloc([8], F32)
            ohg = A.alloc([4], F32)
            negm = A.alloc([1], F32)
            ejunk = A.alloc([4], F32)
            sume = A.alloc([1], F32)
            pg = A.alloc([1], F32)
            selg = A.alloc([4, 8], F32)
            sel = A.alloc([8], F32)
            m1 = A.alloc([8], F32)
            m2 = A.alloc([8], F32)
            dlt = A.alloc([1], F32)
            exd = A.alloc([1], F32)
            w12 = A.alloc([2], F32)
            g12 = A.alloc([16, 2], F32)
            me = A.alloc([8], F32)
            gates = A.alloc([16, 32], F32)
            BwR, Bhn32, BhnT32, BhnT, Brt, Bgates = Buf(), [Buf(), Buf()], Buf(), Buf(), Buf(), Buf()
            P.dma("sp", DMA(wR32, wR_d.rearrange("(k p) c -> p k c", p=128)), writes=[BwR])
            P.op("dve", MSET(ssh, 0.0), writes=[Brt])
            P.op("dve", MSET(g8, -1e30), writes=[Brt])
            psX = bank(6, 2)
            psL = bank(1)
            BpsX, BpsL = Buf(), Buf()
            gFbc = rowbc[:, gF_off:gF_off + 1024]
            lvl = 9
            for nm in dbg:
                if nm.startswith("stoprt"):
                    lvl = int(nm[6:])
            for tt in range(16):
                i2 = tt % 2
                sl = slice(tt * 128, (tt + 1) * 128)
                P.op("act", ACTV(junk, hres[:, tt, :], AF.Square, accum_out=ssh[:, tt:tt + 1]), reads=[Bh[tt], Brt],
                     writes=[Brt])
                P.op("act", ACTV(sqh[:, tt:tt + 1], ssh[:, tt:tt + 1], AF.Sqrt, bias=epsb, scale=1.0 / D), reads=[Brt], writes=[Brt])
                P.op("dve", RECIP(rh[:, tt:tt + 1], sqh[:, tt:tt + 1]), reads=[Brt], writes=[Brt])
                P.op("dve", STT(hn32[i2], hres[:, tt, :], rh[:, tt:tt + 1], gFbc, ALU.mult, ALU.mult),
                     reads=[Bh[tt], Brt, Bcst], writes=[Bhn32[i2]])
                if lvl < 2:
                    continue
                for k in range(8):
                    P.op("pe", MM(psX[:, k * 128:(k + 1) * 128], hn32[i2][:, k * 128:(k + 1) * 128], identf, True, True),
                         reads=[Bhn32[i2]], writes=[BpsX])
                P.op("act", ACTV(hnT32, psX.rearrange("p (k t) -> p k t", k=8), AF.Copy), reads=[BpsX], writes=[BhnT32])
                P.op("pool", CP(hnT[:, :, sl], hnT32), reads=[BhnT32], writes=[BhnT])
                if lvl < 3:
                    continue
                for k in range(8):
                    P.op("pe", MM(psL[:, 0:36], hnT32[:, k, :], wR32[:, k, :], k == 0, k == 7), reads=[BhnT32, BwR], writes=[BpsL])
                P.op("dve", TT(lg, psL[:, 0:36], rowbc[:, R_BR:R_BR + 36], ALU.add), reads=[BpsL, Bcst], writes=[Brt])
                if lvl < 4:
                    continue
                P.op("dve", CP(g8[:, 0:4], lg[:, 0:4]), reads=[Brt], writes=[Brt])
                P.op("dve", lambda e: e.max(out=m8, in_=g8), reads=[Brt], writes=[Brt])
                P.op("dve", TS(ohg, lg[:, 0:4], m8[:, 0:1], ALU.is_equal), reads=[Brt], writes=[Brt])
                P.op("dve", TS(negm, m8[:, 0:1], -1.0, ALU.mult), reads=[Brt], writes=[Brt])
                P.op("act", ACTV(ejunk, lg[:, 0:4], AF.Exp, bias=negm, accum_out=sume), reads=[Brt], writes=[Brt])
                P.op("dve", RECIP(pg, sume), reads=[Brt], writes=[Brt])
                el = lg[:, 4:36].rearrange("p (g e) -> p g e", g=4)
                P.op("dve", TT(selg, el, bc(ohg.rearrange("p (g o) -> p g o", o=1), [128, 4, 8]), ALU.mult), reads=[Brt], writes=[Brt])
                P.op("dve", RSUM(sel, selg.rearrange("p g e -> p e g")), reads=[Brt], writes=[Brt])
                P.op("dve", lambda e: e.max(out=m8b, in_=sel), reads=[Brt], writes=[Brt])
                P.op("dve", TS(m1, sel, m8b[:, 0:1], ALU.is_equal), reads=[Brt], writes=[Brt])
                P.op("dve", TS(m2, sel, m8b[:, 1:2], ALU.is_equal), reads=[Brt], writes=[Brt])
                P.op("dve", TT(dlt, m8b[:, 1:2], m8b[:, 0:1], ALU.subtract), reads=[Brt], writes=[Brt])
                P.op("act", ACTV(exd, dlt, AF.Exp), reads=[Brt], writes=[Brt])
                P.op("dve", TS(w12[:, 0:1], exd, 1.0, ALU.add), reads=[Brt], writes=[Brt])
                P.op("dve", RECIP(w12[:, 0:1], w12[:, 0:1]), reads=[Brt], writes=[Brt])
                P.op("dve", TT(w12[:, 1:2], exd, w12[:, 0:1], ALU.mult), reads=[Brt], writes=[Brt])
                P.op("dve", TS(g12[:, tt, :], w12, pg[:, 0:1], ALU.mult), reads=[Brt], writes=[Brt])
                P.op("dve", TS(me, m1, g12[:, tt, 0:1], ALU.mult), reads=[Brt], writes=[Brt])
                P.op("dve", STT(me, m2, g12[:, tt, 1:2], me, ALU.mult, ALU.add), reads=[Brt], writes=[Brt])
                P.op("dve", TT(gates[:, tt, :].rearrange("p (g e) -> p g e", g=4),
                               bc(me.rearrange("p (o e) -> p o e", o=1), [128, 4, 8]),
                               bc(ohg.rearrange("p (g o) -> p g o", o=1), [128, 4, 8]), ALU.mult), reads=[Brt], writes=[Bgates])
            dump("gates", gates, [128, 16, 32], F32, [Bgates])
            dump("hnT", hnT, [128, 8, S], BF16, [BhnT])
            P.fence()
            DENSE_EXPERTS = N_EXP
            for nm in dbg:
                if nm.startswith("nexp"):
                    DENSE_EXPERTS = int(nm[4:])
            Wgu = [A.alloc([8, 512], BF16) for _ in range(2)]
            Wd = [A.alloc([2, 1024], BF16) for _ in range(2)]
            sa = A.alloc([512], F32)
            hdnT = A.alloc([2, 256], BF16)
            BW = [Buf(), Buf()]
            Bsa, Bhd, Bpa, Bpu, Bpy = Buf(), Buf(), Buf(), Buf(), [Buf(), Buf()]
            psa, psu = bank(0), bank(1)
            ycnt = 0
            for e_ in range(DENSE_EXPERTS):
                wb = e_ % 2
                er = slice(e_ * 128, (e_ + 1) * 128)
                P.dma("pool", DMA(Wgu[wb][:, :, 0:256], wGt_d[er, :].rearrange("p (k f) -> p k f", k=8)), writes=[BW[wb]])
                P.dma("pool", DMA(Wgu[wb][:, :, 256:512], wUt_d[er, :].rearrange("p (k f) -> p k f", k=8)), writes=[BW[wb]])
                P.dma("pool", DMA(Wd[wb], wDt_d[er, :].rearrange("p (c d) -> p c d", c=2)), writes=[BW[wb]])
                for pr in range(8):
                    tok = slice(pr * 256, (pr + 1) * 256)
                    for fc in range(4):
                        pb, Bp = (psa, Bpa) if fc < 2 else (psu, Bpu)
                        for k in range(8):
                            P.op("pe", MM(pb[:, (fc % 2) * 256:(fc % 2) * 256 + 256], Wgu[wb][:, k, fc * 128:(fc + 1) * 128],
                                          hnT[:, k, tok], k == 0, k == 7), reads=[BW[wb], BhnT], writes=[Bp])
                    P.op("act", ACTV(sa, psa, AF.Silu), reads=[Bpa], writes=[Bsa])
                    P.op("dve", TT(hdnT.rearrange("p c t -> p (c t)"), sa, psu, ALU.mult), reads=[Bsa, Bpu], writes=[Bhd])
                    for sub in range(2):
                        tt = 2 * pr + sub
                        ys = ycnt % 2
                        ycnt += 1
                        py = bank(2 + 2 * ys, 2)
                        for hf in range(2):
                            for c2 in range(2):
                                P.op("pe", MM(py[:, hf * 512:(hf + 1) * 512], hdnT[:, c2, sub * 128:(sub + 1) * 128],
                                              Wd[wb][:, c2, hf * 512:(hf + 1) * 512], c2 == 0, c2 == 1),
                                     reads=[Bhd, BW[wb]], writes=[Bpy[ys]])
                        P.op("dve", STT(hres[:, tt, :], py, gates[:, tt, e_:e_ + 1], hres[:, tt, :], ALU.mult, ALU.add),
                             reads=[Bpy[ys], Bgates, Bh[tt]], writes=[Bh[tt]])
        if not FINAL_DONE[0]:
            for tt in range(16):
                final_tile(tt)
        print('SBUF arena peak bytes', A.peak, 'cap', A.cap)
        P.emit(nc, block, sems, dsems, finals)
    return nc, dbg_d


_NC_CACHE = {}


def kernel(**inputs):
    inp = {k: np.asarray(v) for k, v in inputs.items()}
    if "nc" not in _NC_CACHE:
        _NC_CACHE["nc"] = build_nc()[0]
    nc = _NC_CACHE["nc"]
    sh = prep_shared(inp)
    in_maps = []
    for b in range(8):
        m = dict(sh)
        m["x"] = np.ascontiguousarray(inp["x"][b], dtype=np.float32)
        m["pos"] = np.ascontiguousarray(inp["positions"][b:b + 1]).astype(np.int32)
        in_maps.append(m)
    res = run_bass_kernel_spmd(nc, in_maps, core_ids=list(range(8)))
    out = np.stack([np.asarray(r["out"], dtype=np.float32) for r in res.results], axis=0)
    return out
```

```python
import numpy as np
import ml_dtypes
import concourse.bass as bass
import concourse.mybir as mybir
from concourse.bass_utils import run_bass_kernel_spmd

F32 = mybir.dt.float32
BF16 = mybir.dt.bfloat16
I32 = mybir.dt.int32
AF = mybir.ActivationFunctionType
ALU = mybir.AluOpType
AX = mybir.AxisListType


class Buf:
    __slots__ = ("name", "writer", "readers")

    def __init__(self, name=""):
        self.name = name
        self.writer = None
        self.readers = []


class Op:
    __slots__ = ("eng", "fn", "deps", "signal", "val", "sem", "is_dma", "idx")

    def __init__(self, eng, fn, is_dma=False):
        self.eng = eng
        self.fn = fn
        self.deps = []
        self.signal = False
        self.val = None
        self.sem = None
        self.is_dma = is_dma
        self.idx = None


ENGS = ("pe", "act", "dve", "pool", "sp")


class Plan:
    def __init__(self, n_dma_sems=24):
        self.ops = {e: [] for e in ENGS}
        self.n_dma_sems = n_dma_sems
        self.dma_count = 0
        self.dma_counts = [0, 0]
        self.dma_last = [None] * n_dma_sems
        self.nops = 0

    def _add(self, op, reads, writes, deps):
        op.idx = self.nops
        self.nops += 1
        dl = []
        for b in reads:
            if b.writer is not None:
                dl.append((b.writer, "raw"))
        for b in writes:
            if b.writer is not None:
                dl.append((b.writer, "waw"))
            for r in b.readers:
                dl.append((r, "war"))
        for d in deps:
            if d is not None:
                dl.append((d, "raw"))
        for b in reads:
            b.readers.append(op)
        for b in writes:
            b.writer = op
            b.readers = []
        seen = set()
        for d, kind in dl:
            if d is op or id(d) in seen:
                continue
            if (not d.is_dma) and d.eng == op.eng and not op.is_dma:
                if d.eng == "pe":
                    continue
            seen.add(id(d))
            d.signal = True
            op.deps.append(d)
        self.ops[op.eng].append(op)
        return op

    def op(self, eng, fn, reads=(), writes=(), deps=()):
        return self._add(Op(eng, fn), list(reads), list(writes), list(deps))

    def dma(self, eng, fn, reads=(), writes=(), deps=()):
        op = Op(eng, fn, is_dma=True)
        half = self.n_dma_sems // 2
        grp = 1 if eng == "pool" else 0
        cnt = self.dma_counts[grp]
        s = grp * half + cnt % half
        op.sem = s
        op.val = 16 * (cnt // half + 1)
        self.dma_counts[grp] += 1
        self.dma_count += 1
        deps = list(deps)
        if self.dma_last[s] is not None:
            deps.append(self.dma_last[s])
        self.dma_last[s] = op
        op.signal = True
        return self._add(op, list(reads), list(writes), deps)

    def emit(self, nc, block, sems, dma_sems, final_waits):
        for e in ENGS:
            c = 0
            for op in self.ops[e]:
                if op.is_dma:
                    continue
                if op.signal:
                    c += 1
                    op.val = c
        plan = self

        def run(eng_name, eng):
            waited = {}
            for op in plan.ops[eng_name]:
                need = {}
                for d in op.deps:
                    key = ("dma", d.sem) if d.is_dma else ("eng", d.eng)
                    if d.val > need.get(key, 0):
                        need[key] = d.val
                for key, v in need.items():
                    if waited.get(key, 0) >= v:
                        continue
                    waited[key] = v
                    sem = dma_sems[key[1]] if key[0] == "dma" else sems[key[1]]
                    eng.wait_ge(sem, v)
                if op.fn is None:
                    continue
                ins = op.fn(eng)
                if op.is_dma:
                    ins.then_inc(dma_sems[op.sem], 16)
                elif op.signal:
                    ins.then_inc(sems[eng_name], 1)
            if eng_name == "sp":
                for d in final_waits:
                    sem = dma_sems[d.sem] if d.is_dma else sems[d.eng]
                    eng.wait_ge(sem, d.val)

        @block.tensor
        def _(pe):
            run("pe", pe)

        @block.scalar
        def _(act):
            run("act", act)

        @block.vector
        def _(dve):
            run("dve", dve)

        @block.gpsimd
        def _(pool):
            run("pool", pool)

        @block.sync
        def _(sp):
            run("sp", sp)


S = 2048
D = 1024
NT = 16
EPS = 1e-6
THETA = 10000.0
LAM_INIT = 0.8 - 0.6 * 1.0
NEG = -30000.0
N_EXP = 32
DFF = 256

C_GA, C_GQKV, C_GF, C_INVD, C_INVM, C_SGND, C_SGNM, C_DIVQ, C_PCOL, C_PMB, NCST = 0, 8, 13, 21, 22, 23, 24, 25, 27, 28, 29
R_FING, R_SUBG, R_LAM, R_BR, R_GF, R_IOTA, R_C8G, NROW = 0, 1024, 1152, 1408, 1444, 2468, 2468 + 96, 2468 + 100


class Arena:
    def __init__(self, nc, es, nbytes):
        self.t = es.enter_context(nc.sbuf_tensor("arena", [128, nbytes // 2], BF16))
        self.top = 0
        self.cap = nbytes
        self.peak = 0

    def alloc(self, free_shape, dt):
        esz = 4 if dt in (F32, I32) else 2
        n = int(np.prod(free_shape))
        nb = (n * esz + 63) // 64 * 64
        off = self.top
        self.top += nb
        self.peak = max(self.peak, self.top)
        assert self.top <= self.cap, ("SBUF arena overflow", self.top, self.cap)
        v = self.t[:, off // 2: off // 2 + (n * esz) // 2]
        if dt != BF16:
            v = v.bitcast(dt)
        if len(free_shape) == 2:
            v = v.rearrange("p (a b) -> p a b", a=free_shape[0])
        elif len(free_shape) == 3:
            v = v.rearrange("p (a b c) -> p a b c", a=free_shape[0], b=free_shape[1])
        return v


def bc(ap, shape):
    return ap.to_broadcast(list(shape))


def MM(out, lhsT, rhs, start=True, stop=True, skip=False):
    if skip:
        return lambda e: e.matmul(out, lhsT, rhs, start=start, stop=stop, skip_group_check=True)
    return lambda e: e.matmul(out, lhsT, rhs, start=start, stop=stop)


def TR(out, in_, ident):
    return lambda e: e.transpose(out, in_, ident)


def ACTV(out, in_, func, bias=None, scale=1.0, accum_out=None):
    kw = {}
    if bias is not None:
        kw["bias"] = bias
    if accum_out is not None:
        kw["accum_out"] = accum_out
    return lambda e: e.activation(out=out, in_=in_, func=func, scale=scale, **kw)


def TT(out, in0, in1, op):
    return lambda e: e.tensor_tensor(out=out, in0=in0, in1=in1, op=op)


def TS(out, in0, s1, op0, s2=None, op1=None):
    if op1 is None:
        return lambda e: e.tensor_scalar(out=out, in0=in0, scalar1=s1, scalar2=None, op0=op0)
    return lambda e: e.tensor_scalar(out=out, in0=in0, scalar1=s1, scalar2=s2, op0=op0, op1=op1)


def STT(out, in0, scalar, in1, op0, op1):
    return lambda e: e.scalar_tensor_tensor(out=out, in0=in0, scalar=scalar, in1=in1, op0=op0, op1=op1)


def CP(out, in_):
    return lambda e: e.tensor_copy(out=out, in_=in_)


def MSET(out, val):
    return lambda e: e.memset(out, val)


def RECIP(out, in_):
    return lambda e: e.reciprocal(out=out, in_=in_)


def RSUM(out, in_):
    return lambda e: e.reduce_sum(out=out, in_=in_, axis=AX.X)


def DMA(out, in_):
    return lambda e: e.dma_start(out=out, in_=in_)


def _rot_cols(base, n, grp):
    idx = np.arange(n).reshape(-1, grp)
    half = grp // 2
    idx = np.concatenate([idx[:, half:], idx[:, :half]], axis=1).reshape(-1)
    return base + idx


def prep_shared(inp):
    f = np.float32
    w_in = np.asarray(inp["w_in"])[0]
    wAV = np.ascontiguousarray(np.concatenate([w_in[:, 0:640], w_in[:, 1696:2208]], axis=1))
    blocks = []
    for base in (672, 1184):
        for j in range(4):
            blocks.append(w_in[:, base + j * 128: base + (j + 1) * 128])
            blocks.append(w_in[:, _rot_cols(base + j * 128, 128, 64)])
    kpe = np.zeros((D, 128), f)
    kpe[:, 64:96] = w_in[:, 640:672]
    kper = np.zeros((D, 128), f)
    kper[:, 64:96] = w_in[:, _rot_cols(640, 32, 32)]
    blocks += [kpe, kper]
    wF = np.ascontiguousarray(np.concatenate(blocks, axis=1))
    w_uq = np.asarray(inp["w_uq"])[0]
    rot = np.zeros_like(w_uq)
    for h in range(8):
        rot[:, h * 96 + 64: h * 96 + 96] = w_uq[:, _rot_cols(h * 96 + 64, 32, 32)]
    wUQ = np.ascontiguousarray(np.concatenate([w_uq, rot], axis=1))
    w_ukv = np.asarray(inp["w_ukv"])[0]
    kcols = np.concatenate([np.arange(h * 128, h * 128 + 64) for h in range(8)])
    wUKV = np.ascontiguousarray(np.concatenate([w_ukv[:, kcols], w_ukv[:, kcols + 64]], axis=1))
    cst = np.zeros((128, NCST), f)
    p = np.arange(128)
    cst[:, C_GA:C_GA + 8] = np.asarray(inp["attn_norm_g"])[0].reshape(8, 128).T
    cst[:, C_GQKV:C_GQKV + 3] = np.asarray(inp["q_norm_g"])[0].reshape(3, 128).T
    cst[:, C_GQKV + 3:C_GQKV + 5] = np.asarray(inp["kv_norm_g"])[0].reshape(2, 128).T
    cst[:, C_GF:C_GF + 8] = np.asarray(inp["ffn_norm_g"])[0].reshape(8, 128).T
    invd = (f(THETA) ** (-(p % 32).astype(f) / f(32))).astype(f)
    invm = (f(THETA) ** (-(p % 16).astype(f) / f(16))).astype(f)
    cst[:, C_INVD] = invd / f(2 * np.pi)
    cst[:, C_INVM] = invm / f(2 * np.pi)
    cst[:, C_SGND] = np.where((p % 64) < 32, -1.0, 1.0)
    cst[:, C_SGNM] = np.where((p % 32) < 16, -1.0, 1.0)
    cst[:, C_DIVQ] = 1.0 / 384
    cst[:, C_DIVQ + 1] = 1.0 / 256
    cst[:, C_PCOL] = p
    cst[:, C_PMB] = p - float(1 << 16)
    rowc = np.zeros((1, NROW), f)
    rowc[0, R_FING:R_FING + 1024] = np.asarray(inp["final_norm_g"])
    rowc[0, R_SUBG:R_SUBG + 128] = np.asarray(inp["subln_g"])[0]
    rowc[0, R_LAM:R_LAM + 64] = np.asarray(inp["lambda_q1"])[0]
    rowc[0, R_LAM + 64:R_LAM + 128] = np.asarray(inp["lambda_q2"])[0]
    rowc[0, R_LAM + 128:R_LAM + 192] = np.asarray(inp["lambda_k1"])[0]
    rowc[0, R_LAM + 192:R_LAM + 256] = np.asarray(inp["lambda_k2"])[0]
    rowc[0, R_BR:R_BR + 4] = np.asarray(inp["b_router_group"])[0]
    rowc[0, R_BR + 4:R_BR + 36] = np.asarray(inp["b_router_expert"])[0].reshape(32)
    rowc[0, R_GF:R_GF + 1024] = np.asarray(inp["ffn_norm_g"])[0]
    rowc[0, R_IOTA:R_IOTA + 96] = np.arange(96)
    rowc[0, R_C8G:R_C8G + 4] = np.arange(4) * 8
    ident = np.eye(128, dtype=f)
    mtri = np.where(p[:, None] > p[None, :], f(NEG), f(0)).astype(f)
    wR = np.ascontiguousarray(np.concatenate([np.asarray(inp["w_router_group"])[0],
                                              np.asarray(inp["w_router_expert"])[0].reshape(D, 32)], axis=1))
    return {
        "wAV": wAV, "wF": wF, "wUQ": wUQ, "wUKV": wUKV, "wO": np.ascontiguousarray(np.asarray(inp["w_o"])[0]),
        "cst": cst, "rowc": rowc, "ident": ident, "mtri": mtri, "wR": wR,
        "wGt": np.ascontiguousarray(np.asarray(inp["w_gate"])[0].reshape(32, 8, 128, 256).transpose(0, 2, 1, 3).reshape(4096, 2048)),
        "wUt": np.ascontiguousarray(np.asarray(inp["w_up"])[0].reshape(32, 8, 128, 256).transpose(0, 2, 1, 3).reshape(4096, 2048)),
        "wDt": np.ascontiguousarray(np.asarray(inp["w_down"])[0].reshape(32, 2, 128, 1024).transpose(0, 2, 1, 3).reshape(4096, 2048)),
        "utri": np.triu(np.ones((128, 128), f)),
    }


_BOUND_REGS = {}


def IDMA(out, in_, out_off=None, in_off=None, bound=None):
    def f(e):
        key = (id(e), bound)
        if key not in _BOUND_REGS:
            _BOUND_REGS[key] = e.to_reg(bound)
        return e.indirect_dma_start(
            out=out, out_offset=(bass.IndirectOffsetOnAxis(ap=out_off, axis=0) if out_off is not None else None),
            in_=in_, in_offset=(bass.IndirectOffsetOnAxis(ap=in_off, axis=0) if in_off is not None else None),
            bounds_check=_BOUND_REGS[key], oob_is_err=False)
    return f


def TSS(out, in_, scalar, op):
    return lambda e: e.tensor_single_scalar(out=out, in_=in_, scalar=scalar, op=op)


def _fence(P):
    lasts = []
    for e in ENGS:
        comp = [o for o in P.ops[e] if (not o.is_dma) and o.fn is not None]
        if comp:
            lasts.append(comp[-1])
    dmas = [o for o in P.dma_last if o is not None]
    for e in ENGS:
        op = Op(e, None)
        op.idx = P.nops
        P.nops += 1
        for d in lasts + dmas:
            if (not d.is_dma) and d.eng == e:
                continue
            d.signal = True
            op.deps.append(d)
        P.ops[e].append(op)


Plan.fence = _fence


def build_nc(dbg=()):
    from contextlib import ExitStack
    _BOUND_REGS.clear()
    nc = bass.Bass("TRN2", target_bir_lowering=False)

    def din(name, shape, dt=F32):
        return nc.dram_tensor(name, list(shape), dt, kind="ExternalInput").ap()

    x_d = din("x", [S, D])
    pos_d = din("pos", [1, S], I32)
    wAV_d = din("wAV", [D, 1152])
    wF_d = din("wF", [D, 2304])
    wUQ_d = din("wUQ", [384, 1536])
    wUKV_d = din("wUKV", [256, 1024])
    wO_d = din("wO", [D, D])
    cst_d = din("cst", [128, NCST])
    rowc_d = din("rowc", [1, NROW])
    ident_d = din("ident", [128, 128])
    mtri_d = din("mtri", [128, 128])
    wR_d = din("wR", [D, 36])
    wGt_d = din("wGt", [4096, 2048])
    wUt_d = din("wUt", [4096, 2048])
    wDt_d = din("wDt", [4096, 2048])
    utri_d = din("utri", [128, 128])
    out_d = nc.dram_tensor("out", [S, D], F32, kind="ExternalOutput").ap()
    dbg_d = {}

    P = Plan()
    es = ExitStack()
    finals = []
    with es:
        A = Arena(nc, es, 207 * 1024)
        ps_all = es.enter_context(nc.psum_tensor("ps_all", [128, 8 * 512], F32))
        sems = {e: es.enter_context(nc.semaphore("s_" + e)) for e in ENGS}
        dsems = [es.enter_context(nc.semaphore("d%d" % i)) for i in range(P.n_dma_sems)]
        block = es.enter_context(nc.Block())

        def bank(b, n=1):
            return ps_all[:, b * 512:(b + n) * 512]

        def bank_bf(b):
            return ps_all[:, b * 512:(b + 1) * 512].bitcast(BF16)

        def dump(name, ap, shape, dt, reads=()):
            if name not in dbg:
                return
            d = nc.dram_tensor("dbg_" + name, list(shape), dt, kind="ExternalOutput").ap()
            dbg_d[name] = d
            finals.append(P.dma("sp", DMA(d, ap), reads=list(reads)))

        cst = A.alloc([NCST], F32)
        rowbc = A.alloc([NROW], F32)
        identf = A.alloc([128], F32)
        onesf = A.alloc([128], F32)
        ones16 = onesf.bitcast(BF16)[:, 0:128]
        rhi = A.alloc([16], F32)
        rlo = A.alloc([16], F32)
        rhb = A.alloc([16], BF16)
        identb = A.alloc([128], BF16)
        mtrib = A.alloc([128], BF16)
        epsb = A.alloc([1], F32)
        ssx = A.alloc([16], F32)
        sqx = A.alloc([16], F32)
        rstdx = A.alloc([16], F32)
        ssqkv = A.alloc([16, 2], F32)
        t_a = A.alloc([16, 2], F32)
        t_b = A.alloc([16, 2], F32)
        t_c = A.alloc([16, 2], F32)
        sqkv = A.alloc([16, 2], F32)
        lamt = A.alloc([128], F32)
        lam2 = A.alloc([2], F32)
        lame = A.alloc([2], F32)
        neglam = A.alloc([1], F32)
        dss = A.alloc([16, 4], F32)
        dsq = A.alloc([16, 4], F32)
        drd = A.alloc([16, 4], F32)
        ssf = A.alloc([16], F32)
        sqf = A.alloc([16], F32)
        rf = A.alloc([16], F32)
        junk = A.alloc([1024], BF16)
        Bcst, Bjunk = Buf("cst"), Buf("junk")

        P.dma("sp", DMA(cst, cst_d[:, :]), writes=[Bcst])
        P.dma("sp", DMA(rowbc, rowc_d.partition_broadcast(128)), writes=[Bcst])
        P.dma("sp", DMA(identf, ident_d[:, :]), writes=[Bcst])
        P.dma("pool", DMA(identb, ident_d[:, :]), writes=[Bcst])
        P.dma("pool", DMA(mtrib, mtri_d[:, :]), writes=[Bcst])
        Bst = Buf("stats")
        Bst_t = [Buf() for _ in range(16)]
        P.op("dve", MSET(epsb, EPS), writes=[Bst])
        P.op("dve", MSET(ones16, 1.0), writes=[Bst])
        for t_ in (ssx, ssqkv, dss, ssf):
            P.op("dve", MSET(t_, 0.0), writes=[Bst] + Bst_t)
        P.op("dve", MSET(epsb, EPS), writes=[Bst] + Bst_t)
        P.op("dve", TT(lamt, rowbc[:, R_LAM:R_LAM + 128], rowbc[:, R_LAM + 128:R_LAM + 256], ALU.mult),
             reads=[Bcst], writes=[Bst])
        P.op("dve", RSUM(lam2, lamt.rearrange("p (a b) -> p a b", a=2)), reads=[Bst], writes=[Bst])
        P.op("act", ACTV(lame, lam2, AF.Exp), reads=[Bst], writes=[Bst])
        P.op("dve", TT(neglam, lame[:, 1:2], lame[:, 0:1], ALU.subtract), reads=[Bst], writes=[Bst])
        P.op("dve", TS(neglam, neglam, -LAM_INIT, ALU.add), reads=[Bst], writes=[Bst])

        R1 = A.alloc([8, 2304], BF16)
        wF = R1
        mark_cnT = A.top
        cnT = A.alloc([5, S], BF16)
        kpeT = A.alloc([S], BF16)
        posf = A.alloc([S], F32)
        mark_VD = A.top
        VD = A.alloc([16, 4, 129], BF16)
        QTD = A.alloc([4, S], BF16)
        KTD = A.alloc([4, S], BF16)
        mark_wAV = A.top
        wAV = A.alloc([8, 1152], BF16)
        xs = [A.alloc([1024], F32)] * 2
        xb = [A.alloc([1024], BF16) for _ in range(2)]
        xT = [A.alloc([8, 512], BF16) for _ in range(2)]
        cn = [A.alloc([640], BF16) for _ in range(2)]
        CD = A.alloc([512], F32)
        SD = A.alloc([512], F32)
        CMr = A.alloc([512], F32)
        SMr = A.alloc([512], F32)
        u2 = A.alloc([2, 512], F32)
        nn = A.alloc([2, 512], F32)
        ni = nn.bitcast(I32)
        cs_set = [A.alloc([2, 512], F32) for _ in range(2)]
        t1 = [A.alloc([512], F32)] * 2
        t2 = [A.alloc([512], F32)] * 2
        diag = [A.alloc([128], F32) for _ in range(2)]
        _save = A.top
        A.top = mark_wAV
        KTz = A.alloc([8, S], BF16)
        mark_after_KTz = A.top
        A.top = _save
        BKTz = Buf("KTz")
        posi = ni.rearrange("p a b -> p (a b)")

        Bpos = Buf("pos")
        for hh in range(2):
            P.dma("sp", DMA(posi, pos_d[:, hh * 1024:(hh + 1) * 1024].partition_broadcast(128)), writes=[Bpos])
            P.op("dve", CP(posf[:, hh * 1024:(hh + 1) * 1024], posi), reads=[Bpos], writes=[Bpos])
        BVD = Buf("VD")
        P.op("pool", MSET(VD[:, :, :, 128:129], 1.0), writes=[BVD])

        BwAV = Buf("wAV")
        BwF = [Buf("wF%d" % i) for i in range(3)]
        P.dma("pool", DMA(wAV, wAV_d.rearrange("(k p) c -> p k c", p=128)), writes=[BwAV])
        for i in range(3):
            P.dma("pool", DMA(wF[:, :, i * 768:(i + 1) * 768],
                              wF_d[:, i * 768:(i + 1) * 768].rearrange("(k p) c -> p k c", p=128)), writes=[BwF[i]])

        gA3 = cst[:, C_GA:C_GA + 8].rearrange("p (a b) -> p a b", b=1)
        gQ3 = cst[:, C_GQKV:C_GQKV + 5].rearrange("p (a b) -> p a b", b=1)
        psT, psT2, psA0, psA1, psV, psB, psF0, psF1 = [bank(i) for i in range(8)]
        psT_b = bank_bf(0)
        psT2_b = bank_bf(1)
        BpsT, BpsT2, BpsA0, BpsA1, BpsV, BpsB, BpsF0, BpsF1 = [Buf("ps%d" % i) for i in range(8)]
        Bxs = [Buf()] * 2
        Bxb = [Buf(), Buf()]
        BxT = [[Buf() for _ in range(4)] for _ in range(2)]
        Bcn = [Buf(), Buf()]
        Btab = Buf("tab")
        Btmp = Buf("tabtmp")
        Bt12 = [Buf()] * 2
        Bdiag = [Buf(), Buf()]
        BcnT = Buf("cnT")
        Bqk = Buf("qkT")
        def a_load(tt):
            sl = slice(tt * 128, (tt + 1) * 128)
            i2 = tt % 2
            P.dma("sp", DMA(xs[i2], x_d[sl, :]), writes=[Bxs[i2]])
            P.dma("pool", DMA(xb[i2], x_d[sl, :]), writes=[Bxb[i2]])

        def a_step1(tc, r):
            tt = 4 * tc + r
            sl = slice(tt * 128, (tt + 1) * 128)
            rs = slice(r * 128, (r + 1) * 128)
            i2 = tt % 2
            buf = tc % 2
            rx = rstdx[:, tt:tt + 1]
            P.op("act", ACTV(junk, xs[i2], AF.Square, accum_out=ssx[:, tt:tt + 1]),
                 reads=[Bxs[i2], Bst_t[tt]], writes=[Bst_t[tt]])
            if tt + 1 < 16:
                a_load(tt + 1)
            P.op("act", ACTV(sqx[:, tt:tt + 1], ssx[:, tt:tt + 1], AF.Sqrt, bias=epsb, scale=1.0 / D),
                 reads=[Bst_t[tt]], writes=[Bst_t[tt]])
            P.op("dve", RECIP(rstdx[:, tt:tt + 1], sqx[:, tt:tt + 1]), reads=[Bst_t[tt]], writes=[Bst_t[tt]])
            for k in range(8):
                P.op("pe", TR(psT_b[:, k * 128:(k + 1) * 128], xb[i2][:, k * 128:(k + 1) * 128], identb),
                     reads=[Bxb[i2], Bcst], writes=[BpsT])
            P.op("dve", TT(xT[buf][:, :, rs], psT_b.rearrange("p (k t) -> p k t", k=8), bc(gA3, [128, 8, 128]),
                           ALU.mult), reads=[BpsT, Bcst], writes=[BxT[buf][r]])

        def a_step2(tc, r):
            tt = 4 * tc + r
            sl = slice(tt * 128, (tt + 1) * 128)
            rs = slice(r * 128, (r + 1) * 128)
            i2 = tt % 2
            buf = tc % 2
            rx = rstdx[:, tt:tt + 1]
            for (c0, c1, pb, Bp) in ((0, 384, psA0, BpsA0), (384, 640, psA1, BpsA1), (640, 1152, psV, BpsV)):
                for k in range(8):
                    P.op("pe", MM(pb[:, 0:c1 - c0], xT[buf][:, k, rs], wAV[:, k, c0:c1], k == 0, k == 7),
                         reads=[BxT[buf][r], BwAV], writes=[Bp])
            P.op("act", ACTV(junk[:, 0:384], psA0[:, 0:384], AF.Square, accum_out=ssqkv[:, tt, 0:1]),
                 reads=[BpsA0, Bst_t[tt]], writes=[Bst_t[tt]])
            P.op("act", ACTV(junk[:, 0:256], psA1[:, 0:256], AF.Square, accum_out=ssqkv[:, tt, 1:2]),
                 reads=[BpsA1, Bst_t[tt]], writes=[Bst_t[tt]])
            P.op("dve", STT(t_a[:, tt, :], ssqkv[:, tt, :], rx, cst[:, C_DIVQ:C_DIVQ + 2], ALU.mult, ALU.mult),
                 reads=[Bst_t[tt], Bcst], writes=[Bst_t[tt]])
            P.op("dve", TS(t_a[:, tt, :], t_a[:, tt, :], rx, ALU.mult), reads=[Bst_t[tt]], writes=[Bst_t[tt]])
            P.op("act", ACTV(t_b[:, tt, :], t_a[:, tt, :], AF.Sqrt, bias=epsb, scale=1.0), reads=[Bst_t[tt]], writes=[Bst_t[tt]])
            P.op("dve", RECIP(t_c[:, tt, :], t_b[:, tt, :]), reads=[Bst_t[tt]], writes=[Bst_t[tt]])
            P.op("dve", TS(sqkv[:, tt, :], t_c[:, tt, :], rx, ALU.mult), reads=[Bst_t[tt]], writes=[Bst_t[tt]])
            P.op("dve", TS(cn[i2][:, 0:384], psA0[:, 0:384], sqkv[:, tt, 0:1], ALU.mult),
                 reads=[BpsA0, Bst_t[tt]], writes=[Bcn[i2]])
            P.op("dve", TS(cn[i2][:, 384:640], psA1[:, 0:256], sqkv[:, tt, 1:2], ALU.mult),
                 reads=[BpsA1, Bst_t[tt]], writes=[Bcn[i2]])

        def a_step3(tc, r):
            tt = 4 * tc + r
            sl = slice(tt * 128, (tt + 1) * 128)
            rs = slice(r * 128, (r + 1) * 128)
            i2 = tt % 2
            buf = tc % 2
            rx = rstdx[:, tt:tt + 1]
            for j in range(5):
                P.op("pe", TR(psT2_b[:, j * 128:(j + 1) * 128], cn[i2][:, j * 128:(j + 1) * 128], identb),
                     reads=[Bcn[i2], Bcst], writes=[BpsT2])
            P.op("dve", TT(cnT[:, :, sl], psT2_b[:, 0:640].rearrange("p (k t) -> p k t", k=5),
                           bc(gQ3, [128, 5, 128]), ALU.mult), reads=[BpsT2, Bcst], writes=[BcnT])
            P.op("act", ACTV(VD[:, tt, :, 0:128], psV.rearrange("p (h d) -> p h d", h=4), AF.Copy, scale=rx),
                 reads=[BpsV, Bst_t[tt]], writes=[BVD])
            dg = diag[i2].bitcast(BF16)
            dh, dl = dg[:, 0:128], dg[:, 128:256]
            P.op("dve", CP(rhb[:, tt:tt + 1], rx), reads=[Bst_t[tt]], writes=[Bst_t[tt]])
            P.op("dve", CP(rhi[:, tt:tt + 1], rhb[:, tt:tt + 1]), reads=[Bst_t[tt]], writes=[Bst_t[tt]])
            P.op("dve", TT(rlo[:, tt:tt + 1], rx, rhi[:, tt:tt + 1], ALU.subtract), reads=[Bst_t[tt]], writes=[Bst_t[tt]])
            P.op("dve", TS(dh, identb, rhi[:, tt:tt + 1], ALU.mult), reads=[Bst_t[tt], Bcst], writes=[Bdiag[i2]])
            P.op("dve", TS(dl, identb, rlo[:, tt:tt + 1], ALU.mult), reads=[Bst_t[tt], Bcst], writes=[Bdiag[i2]])
            P.op("pe", MM(psB[:, rs], ones16, dh, True, False), reads=[Bdiag[i2], Bst_t[tt]], writes=[BpsB])
            P.op("pe", MM(psB[:, rs], ones16, dl, False, True), reads=[Bdiag[i2], Bst_t[tt]], writes=[BpsB])

        MAGIC = 12582912.0
        Bcs = [Buf(), Buf()]

        def a_tabprep(tc, si):
            chunk = slice(tc * 512, (tc + 1) * 512)
            invc = (C_INVD, C_INVM)[si]
            iv = cst[:, invc:invc + 1]
            cs_ = cs_set[si]
            P.op("dve", TS(u2[:, 0, :], posf[:, chunk], iv, ALU.mult), reads=[Bpos, Bcst], writes=[Btmp])
            P.op("dve", TS(u2[:, 1, :], posf[:, chunk], iv, ALU.mult, 0.25, ALU.add), reads=[Bpos, Bcst], writes=[Btmp])
            P.op("dve", TS(nn, u2, MAGIC, ALU.add, MAGIC, ALU.subtract), reads=[Btmp], writes=[Btmp])
            P.op("dve", TT(cs_, u2, nn, ALU.subtract), reads=[Btmp], writes=[Bcs[si]])
            P.op("act", ACTV(cs_, cs_, AF.Sin, scale=float(2 * np.pi)), reads=[Bcs[si]], writes=[Bcs[si]])

        def a_tables(tc):
            for si, (sgnc, Cout, Sout) in enumerate(((C_SGND, CD, SD), (C_SGNM, CMr, SMr))):
                cs_ = cs_set[si]
                P.op("dve", STT(Sout, cs_[:, 0, :], cst[:, sgnc:sgnc + 1], psB, ALU.mult, ALU.mult),
                     reads=[Bcs[si], BpsB, Bcst], writes=[Btab])
                P.op("dve", TT(Cout, cs_[:, 1, :], psB, ALU.mult), reads=[Bcs[si], BpsB], writes=[Btab])

        def a_feat(tc, i):
            buf = tc % 2
            chunk = slice(tc * 512, (tc + 1) * 512)
            for hf, (pb, Bp) in enumerate(((psF0, BpsF0), (psF1, BpsF1))):
                blk = 2 * i + hf
                for k in range(8):
                    P.op("pe", MM(pb, wF[:, k, blk * 128:(blk + 1) * 128], xT[buf][:, k, :], k == 0, k == 7),
                         reads=BxT[buf] + [BwF[blk // 6]], writes=[Bp])
            if i < 8:
                rows = slice(0, 128)
                Ct, St = CD, SD
                dest = (QTD if i < 4 else KTD)[:, i % 4, chunk]
            else:
                rows = slice(64, 96)
                Ct, St = CMr, SMr
                dest = kpeT[64:96, chunk]
            j2 = i % 2
            P.op("dve", TT(t1[j2][rows], psF0[rows], Ct[rows], ALU.mult), reads=[BpsF0, Btab], writes=[Bt12[j2]])
            P.op("dve", TT(t2[j2][rows], psF1[rows], St[rows], ALU.mult), reads=[BpsF1, Btab], writes=[Bt12[j2]])
            P.op("pool", TT(dest, t1[j2][rows], t2[j2][rows], ALU.add), reads=[Bt12[j2]], writes=[Bqk])


        a_load(0)
        for tc in range(4):
            for r in range(4):
                a_step1(tc, r)
                if tc > 0:
                    a_feat(tc - 1, 2 * r)
                a_step2(tc, r)
                if r < 2:
                    a_tabprep(tc, r)
                if tc > 0:
                    a_feat(tc - 1, 2 * r + 1)
                a_step3(tc, r)
            if tc > 0:
                a_feat(tc - 1, 8)
            if tc == 3:
                for h in range(4):
                    for half in range(2):
                        zrows = slice(64 * (1 - half), 64 * (1 - half) + 64)
                        P.op("dve", MSET(KTz[zrows, 2 * h + half, :], 0.0),
                             writes=[BKTz, BwAV, Bxs[0], Bxb[0], Bxb[1]] + BxT[0])
            a_tables(tc)
        for i in range(9):
            a_feat(3, i)
            if 4 <= i < 8:
                h = i - 4
                for half in range(2):
                    rows = slice(64 * half, 64 * half + 64)
                    P.op("act", ACTV(KTz[rows, 2 * h + half, :], KTD[rows, h, :], AF.Copy), reads=[Bqk], writes=[BKTz])

        dump("cnT", cnT, [128, 5, S], BF16, [BcnT])
        dump("QTD", QTD, [128, 4, S], BF16, [Bqk])
        dump("KTD", KTD, [128, 4, S], BF16, [Bqk])
        dump("kpeT", kpeT, [128, S], BF16, [Bqk])
        dump("VD", VD, [128, 16, 4, 129], BF16, [BVD])
        dump("rstdx", rstdx, [128, 16], F32, [Bst])
        P.fence()
        if "stopA" in dbg:
            P.emit(nc, block, sems, dsems, finals)
            return nc, dbg_d
        A.top = mark_after_KTz
        PT = [A.alloc([1024], BF16) for _ in range(3)]
        o_all = R1.rearrange("p a b -> p (a b)")[:, 0:16 * 1024].rearrange("p (t c) -> p t c", t=16)
        A1 = A.alloc([4, 128], F32)
        A2 = A.alloc([4, 128], F32)
        Dd = A.alloc([4, 128], F32)
        Dsq = A.alloc([4, 128], F32)
        rec4 = [A.alloc([4], F32) for _ in range(2)]
        BS = [Buf("S0"), Buf("S1"), Buf("S2")]
        BPT = [Buf(), Buf(), Buf()]
        Bacc = [Buf("acc0"), Buf("acc1")]
        Bo = Buf("o_all")
        BA1, BA2, BDd = Buf(), Buf(), Buf()
        Brec = [Buf(), Buf()]

        def run_attention(units, nS=2):
            groups = []
            for ui, u in enumerate(units):
                c = u["c"]
                gl = []
                for pr in range(2 * c):
                    gl.append(dict(ncols=1024, ents=[(2 * pr, 0, [(r, r * 128) for r in range(4)]),
                                                     (2 * pr + 1, 512, [(r, r * 128) for r in range(4)])]))
                gl.append(dict(ncols=1024, ents=[(4 * c, 0, [(r, r * 128) for r in range(4)]),
                                                 (4 * c + 1, 512, [(r, r * 128) for r in range(1, 4)])]))
                gl.append(dict(ncols=384, ents=[(4 * c + 2, 0, [(2, 0), (3, 128)]),
                                                (4 * c + 3, 0, [(3, 256)])]))
                for gi, g in enumerate(gl):
                    g["u"] = u
                    g["ui"] = ui
                    g["last"] = gi == len(gl) - 1
                    g["second_last"] = gi == len(gl) - 2
                    groups.append(g)

            def emit_qk(gidx):
                g = groups[gidx]
                u = g["u"]
                c = u["c"]
                Sg = bank(2 * (gidx % nS), 2)
                for (kb, base, rl) in g["ents"]:
                    r0 = rl[0][0]
                    c0 = base + rl[0][1]
                    n = len(rl)
                    isdiag = kb >= 4 * c
                    P.op("pe", MM(Sg[:, c0:c0 + n * 128], u["KT"][:, kb * 128:(kb + 1) * 128],
                                  u["QT"][:, (4 * c + r0) * 128:(4 * c + 4) * 128], True, not isdiag, skip=True),
                         writes=[BS[gidx % nS]])
                    if isdiag:
                        P.op("pe", MM(Sg[:, c0:c0 + 128], identb, mtrib, False, True, skip=True),
                             writes=[BS[gidx % nS]])

            def emit_exp_pv(gidx):
                g = groups[gidx]
                u = g["u"]
                c = u["c"]
                W = u["W"]
                Sg = bank(2 * (gidx % nS), 2)
                pt = PT[gidx % 3]
                P.op("act", ACTV(pt[:, 0:g["ncols"]], Sg[:, 0:g["ncols"]], AF.Exp, scale=u["scale"]),
                     reads=[BS[gidx % nS]], writes=[BPT[gidx % 3]])
                ci = g["ui"] % 2
                for (kb, base, rl) in g["ents"]:
                    for (r, col) in rl:
                        tok = u["acctok"](ci, r) if "acctok" in u else Bacc[ci]
                        P.op("pe", MM(u["acc"](ci, r), pt[:, base + col:base + col + 128], u["V"](kb),
                                      kb == 0 and (r % u["rper"]) == 0, kb == 4 * c + r, skip=True),
                             reads=[BPT[gidx % 3]], writes=[tok])
                if "post_half" in u:
                    if g["second_last"]:
                        u["post_half"](u, 0)
                    if g["last"]:
                        u["post_half"](u, 1)
                elif g["last"]:
                    u["post"](ci, u)

            G = len(groups)
            for g0 in range(min(nS - 1, G)):
                emit_qk(g0)
            for gidx in range(G):
                if gidx + nS - 1 < G:
                    emit_qk(gidx + nS - 1)
                emit_exp_pv(gidx)

        BaccD = [Buf("accD0"), Buf("accD1")]
        BrecD = [[Buf(), Buf()], [Buf(), Buf()]]
        BA1D, BA2D, BDdD, BDsqD = [[Buf(), Buf()] for _ in range(4)]

        def acc_diff(ci, r):
            return bank(6 + r // 2)[:, (r % 2) * 129:(r % 2) * 129 + 129]

        def acctok_diff(ci, r):
            return BaccD[r // 2]

        def post_diff_half(u, hb):
            h, half, c = u["h"], u["half"], u["c"]
            q0 = 2 * hb
            accv = bank(6 + hb)[:, 0:258].rearrange("p (s w) -> p s w", w=129)
            rc = rec4[half][:, q0:q0 + 2].rearrange("p (s o) -> p s o", o=1)
            P.op("dve", RECIP(rc, accv[:, :, 128:129]), reads=[BaccD[hb]], writes=[BrecD[half][hb]])
            dst = (A1 if half == 0 else A2)[:, q0:q0 + 2, :]
            P.op("dve", TT(dst, accv[:, :, 0:128], bc(rc, [128, 2, 128]), ALU.mult),
                 reads=[BaccD[hb], BrecD[half][hb]], writes=[BA1D[hb] if half == 0 else BA2D[hb]])
            if half == 1:
                Dd_, Dsq_ = Dd[:, q0:q0 + 2, :], Dsq[:, q0:q0 + 2, :]
                P.op("dve", STT(Dd_, A2[:, q0:q0 + 2, :], neglam[:, 0:1], A1[:, q0:q0 + 2, :], ALU.mult, ALU.add),
                     reads=[BA1D[hb], BA2D[hb], Bst], writes=[BDdD[hb]])
                P.op("pool", TT(Dsq_, Dd_, Dd_, ALU.mult), reads=[BDdD[hb]], writes=[BDsqD[hb]])
                P.op("dve", RSUM(dss[:, 4 * c + q0:4 * c + q0 + 2, h], Dsq_), reads=[BDsqD[hb]], writes=[Bst])
                P.op("pool", CP(o_all[:, 4 * c + q0:4 * c + q0 + 2, 512 + h * 128:512 + (h + 1) * 128], Dd_),
                     reads=[BDdD[hb]], writes=[Bo])

        units = []
        for h in range(4):
            for c in range(4):
                for half in range(2):
                    units.append(dict(KT=KTz[:, 2 * h + half, :], QT=QTD[:, h, :], W=129, rper=2, scale=64 ** -0.5, c=c, h=h,
                                      half=half, V=(lambda kb, h=h: VD[:, kb, h, :]), acc=acc_diff, acctok=acctok_diff,
                                      post_half=post_diff_half))
        run_attention(units, nS=3)
        dump("dss", dss, [128, 16, 4], F32, [Bst])
        dump("o_all", o_all, [128, 16, 1024], BF16, [Bo])
        P.fence()
        if "stopB" in dbg:
            P.emit(nc, block, sems, dsems, finals)
            return nc, dbg_d
        A.top = mark_VD
        QTM = A.alloc([8, S], BF16)
        KTM = A.alloc([8, S], BF16)
        VM = A.alloc([16, 8, 65], BF16)
        wUQ = A.alloc([3, 1536], BF16)
        wUKV = A.alloc([2, 1024], BF16)
        CM = A.alloc([512], F32)
        SM = A.alloc([512], F32)
        u2 = A.alloc([2, 512], F32)
        nf = A.alloc([2, 512], F32)
        cs = A.alloc([2, 512], F32)
        t1c = [A.alloc([512], F32)] * 2
        t2c = [A.alloc([512], F32)] * 2
        PT = [A.alloc([1024], BF16) for _ in range(3)]
        recm = [A.alloc([4], F32) for _ in range(2)]
        BwUQ, BwUKV, BVM, BQTM, BKTM, BQTMn, BKTMn = Buf(), Buf(), Buf(), Buf(), Buf(), Buf(), Buf()
        Btab, Btmp, Bt12c = Buf(), Buf(), [Buf()] * 2
        BpsC3 = [(Buf(), Buf(), Buf()), (Buf(), Buf(), Buf())]
        P.dma("pool", DMA(wUQ, wUQ_d.rearrange("(k p) c -> p k c", p=128)), writes=[BwUQ])
        P.dma("pool", DMA(wUKV, wUKV_d.rearrange("(k p) c -> p k c", p=128)), writes=[BwUKV])
        P.op("pool", MSET(VM[:, :, :, 64:65], 1.0), writes=[BVM])
        for h in range(8):
            P.op("pool", CP(KTM[64:96, h, :], kpeT[64:96, :]), writes=[BKTM])
        psVm = bank(3)
        BpsVm = Buf()
        for tt in range(16):
            sl = slice(tt * 128, (tt + 1) * 128)
            for k in range(2):
                P.op("pe", MM(psVm, cnT[:, 3 + k, sl], wUKV[:, k, 512:1024], k == 0, k == 1),
                     reads=[BwUKV], writes=[BpsVm])
            P.op("act", ACTV(VM[:, tt, :, 0:64], psVm.rearrange("p (h d) -> p h d", h=8), AF.Copy),
                 reads=[BpsVm], writes=[BVM])
        for tc in range(4):
            chunk = slice(tc * 512, (tc + 1) * 512)
            iv = cst[:, C_INVM:C_INVM + 1]
            P.op("dve", TS(u2[:, 0, :], posf[:, chunk], iv, ALU.mult), writes=[Btmp, Bt12c[1]])
            P.op("dve", TS(u2[:, 1, :], posf[:, chunk], iv, ALU.mult, 0.25, ALU.add), writes=[Btmp, Bt12c[1]])
            P.op("dve", TS(nf, u2, MAGIC, ALU.add, MAGIC, ALU.subtract), reads=[Btmp], writes=[Btmp])
            P.op("dve", TT(cs, u2, nf, ALU.subtract), reads=[Btmp], writes=[Btmp])
            P.op("act", ACTV(cs, cs, AF.Sin, scale=float(2 * np.pi)), reads=[Btmp], writes=[Btmp])
            P.op("dve", TS(SM, cs[:, 0, :], cst[:, C_SGNM:C_SGNM + 1], ALU.mult), reads=[Btmp], writes=[Btab])
            P.op("dve", CP(CM, cs[:, 1, :]), reads=[Btmp], writes=[Btab])
            for h in range(8):
                par = h % 2
                psQ0, psQ1, psK = bank(4 * par), bank(4 * par + 1), bank(4 * par + 2)
                BpsQ0, BpsQ1, BpsK = BpsC3[par]
                for hf, (pb, Bp) in enumerate(((psQ0, BpsQ0), (psQ1, BpsQ1))):
                    for k in range(3):
                        P.op("pe", MM(pb[0:96, :], wUQ[:, k, hf * 768 + h * 96:hf * 768 + (h + 1) * 96], cnT[:, k, chunk],
                                      k == 0, k == 2), reads=[BwUQ], writes=[Bp])
                for k in range(2):
                    P.op("pe", MM(psK[0:64, :], wUKV[:, k, h * 64:(h + 1) * 64], cnT[:, 3 + k, chunk], k == 0, k == 1),
                         reads=[BwUKV], writes=[BpsK])
                P.op("act", ACTV(QTM[0:64, h, chunk], psQ0[0:64, :], AF.Copy), reads=[BpsQ0], writes=[BQTMn])
                P.op("dve", TT(t1c[par][64:96], psQ0[64:96, :], CM[64:96], ALU.mult), reads=[BpsQ0, Btab], writes=[Bt12c[par]])
                P.op("dve", TT(t2c[par][64:96], psQ1[64:96, :], SM[64:96], ALU.mult), reads=[BpsQ1, Btab], writes=[Bt12c[par]])
                P.op("dve", TT(QTM[64:96, h, chunk], t1c[par][64:96], t2c[par][64:96], ALU.add), reads=[Bt12c[par]], writes=[BQTM])
                P.op("act", ACTV(KTM[0:64, h, chunk], psK[0:64, :], AF.Copy), reads=[BpsK], writes=[BKTMn])
        dump("QTM", QTM, [128, 8, S], BF16, [BQTM, BQTMn])
        dump("KTM", KTM, [128, 8, S], BF16, [BKTM, BKTMn])
        dump("VM", VM, [128, 16, 8, 65], BF16, [BVM])
        P.fence()

        def acc_mla(ci, r):
            return bank(6 + ci)[:, r * 65:(r + 1) * 65]

        def post_mla(ci, u):
            h, c = u["h"], u["c"]
            accv = bank(6 + ci)[:, 0:260].rearrange("p (r w) -> p r w", w=65)
            rc = recm[ci].rearrange("p (r o) -> p r o", o=1)
            P.op("dve", RECIP(rc, accv[:, :, 64:65]), reads=[Bacc[ci]], writes=[Brec[ci]])
            P.op("dve", TT(o_all[:, 4 * c:4 * c + 4, h * 64:(h + 1) * 64], accv[:, :, 0:64], bc(rc, [128, 4, 64]),
                           ALU.mult), reads=[Bacc[ci], Brec[ci]], writes=[Bo])

        units = []
        for h in range(8):
            for c in range(4):
                units.append(dict(KT=KTM[0:96, h, :], QT=QTM[0:96, h, :], W=65, rper=4, scale=96 ** -0.5, c=c, h=h,
                                  V=(lambda kb, h=h: VM[:, kb, h, :]), acc=acc_mla, post=post_mla))
        run_attention(units, nS=3)
        P.fence()

        A.top = mark_cnT
        hres = A.alloc([16, 1024], F32)
        ob = [A.alloc([1024], F32) for _ in range(2)]
        mark_after_hres = A.top
        wO = A.alloc([8, 1024], BF16)
        mixT = [A.alloc([8, 128], BF16) for _ in range(2)]
        xs2 = [A.alloc([1024], F32) for _ in range(2)]
        BwO, Bmix, Bxs2, Bob, Bh = Buf(), [Buf(), Buf()], [Buf(), Buf()], [Buf(), Buf()], [Buf() for _ in range(16)]
        P.dma("pool", DMA(wO, wO_d.rearrange("(k p) c -> p k c", p=128)), writes=[BwO])
        P.op("act", ACTV(dsq, dss, AF.Sqrt, bias=epsb, scale=1.0 / 128), reads=[Bst], writes=[Bst])
        P.op("dve", RECIP(drd, dsq), reads=[Bst], writes=[Bst])
        P.op("dve", TS(drd, drd, 1.0 - LAM_INIT, ALU.mult), reads=[Bst], writes=[Bst])
        subg = rowbc[:, R_SUBG:R_SUBG + 128].rearrange("p (a b) -> p a b", a=1)
        fing = rowbc[:, R_FING:R_FING + 1024]
        BpsTe, BpsO = Buf(), [Buf(), Buf()]
        psTe = bank_bf(0)
        Bo_t = [Buf() for _ in range(16)]
        for tt in range(16):
            od = o_all[:, tt, 512:1024].rearrange("p (h d) -> p h d", h=4)
            P.op("dve", TT(od, od, bc(drd[:, tt, :].rearrange("p (h o) -> p h o", o=1), [128, 4, 128]), ALU.mult),
                 reads=[Bo, Bst], writes=[Bo_t[tt]])
            P.op("dve", TT(od, od, bc(subg, [128, 4, 128]), ALU.mult), reads=[Bo_t[tt], Bcst], writes=[Bo_t[tt]])

        def e_transposes(tt):
            i2 = tt % 2
            for c8 in range(8):
                P.op("pe", TR(psTe[:, c8 * 128:(c8 + 1) * 128], o_all[:, tt, c8 * 128:(c8 + 1) * 128], identb),
                     reads=[Bo, Bo_t[tt]], writes=[BpsTe])
            P.op("act", ACTV(mixT[i2], psTe.rearrange("p (c t) -> p c t", c=8), AF.Copy), reads=[BpsTe], writes=[Bmix[i2]])

        def e_matmuls(tt):
            i2 = tt % 2
            sl = slice(tt * 128, (tt + 1) * 128)
            psO = bank(2 + 2 * i2, 2)
            for hf in range(2):
                for c8 in range(8):
                    P.op("pe", MM(psO[:, hf * 512:(hf + 1) * 512], mixT[i2][:, c8, :], wO[:, c8, hf * 512:(hf + 1) * 512],
                                  c8 == 0, c8 == 7), reads=[Bmix[i2], BwO], writes=[BpsO[i2]])
            P.dma("sp", DMA(xs2[i2], x_d[sl, :]), writes=[Bxs2[i2]])
            P.op("dve", TT(hres[:, tt, :], psO, xs2[i2], ALU.add), reads=[BpsO[i2], Bxs2[i2]], writes=[Bh[tt]])

        e_transposes(0)
        for tt in range(16):
            if tt + 1 < 16:
                e_transposes(tt + 1)
            e_matmuls(tt)
        dump("hres", hres, [128, 16, 1024], F32, Bh)
        FINAL_DONE = [False]

        def final_tile(tt):
            i2 = tt % 2
            sl = slice(tt * 128, (tt + 1) * 128)
            P.op("act", ACTV(junk, hres[:, tt, :], AF.Square, accum_out=ssf[:, tt:tt + 1]), reads=[Bh[tt], Bst],
                 writes=[Bst])
            P.op("act", ACTV(sqf[:, tt:tt + 1], ssf[:, tt:tt + 1], AF.Sqrt, bias=epsb, scale=1.0 / D), reads=[Bst], writes=[Bst])
            P.op("dve", RECIP(rf[:, tt:tt + 1], sqf[:, tt:tt + 1]), reads=[Bst], writes=[Bst])
            P.op("dve", STT(ob[i2], hres[:, tt, :], rf[:, tt:tt + 1], fing, ALU.mult, ALU.mult),
                 reads=[Bh[tt], Bst, Bcst], writes=[Bob[i2]])
            finals.append(P.dma("sp", DMA(out_d[sl, :], ob[i2]), reads=[Bob[i2]]))

        if "noF" not in dbg and "dense" not in dbg:
            P.fence()
            A.top = mark_after_hres
            BIG = float(1 << 16)
            NTL = 48
            NSL = NTL * 256
            hn_d = nc.dram_tensor("hn_scr", [S, D], BF16).ap()
            tos_d = nc.dram_tensor("tos_scr", [NSL, 16], I32).ap()
            Y_d = nc.dram_tensor("y_scr", [NSL, D], BF16).ap()
            BhnD, BtosD, BYd = Buf(), Buf(), Buf()
            R1f = R1.rearrange("p a b -> p (a b)")
            Wg = [R1f[:, i * 6144: i * 6144 + 2048].rearrange("p (k f) -> p k f", k=8) for i in range(3)]
            Wu = [R1f[:, i * 6144 + 2048: i * 6144 + 4096].rearrange("p (k f) -> p k f", k=8) for i in range(3)]
            Wd = [R1f[:, i * 6144 + 4096: i * 6144 + 6144].rearrange("p (c d) -> p c d", c=2) for i in range(3)]
            wR32 = A.alloc([8, 36], F32)
            Whi = A.alloc([8, 36], BF16)
            Wlo = A.alloc([8, 36], BF16)
            wRt = A.alloc([8, 36], F32)
            ssh = A.alloc([16], F32)
            sqh = A.alloc([16], F32)
            rh = A.alloc([16], F32)
            lg = A.alloc([16, 36], F32)
            utri = A.alloc([128], BF16)
            onesb = A.alloc([128], BF16)

            def f16(n):
                return A.alloc([16, n], F32)
            gmax, sume, pg, v0, v1, dlt, exd, w1, w2, gi8, i1, i2_, eid1, eid2, rank1, rank2 = [A.alloc([16], F32) for _ in range(16)]
            ohg, gsh = f16(4), f16(4)
            selg = A.alloc([64, 8], F32)
            sel, m1, m2, sel2, tm8 = f16(8), f16(8), f16(8), f16(8), f16(8)
            oh1, oh2, OH, incl, t32 = f16(32), f16(32), f16(32), f16(32), f16(32)
            OHb = A.alloc([16, 32], BF16)
            g12 = A.alloc([16, 2], F32)
            posf2 = A.alloc([16, 2], F32)
            posi2 = A.alloc([16, 2], I32)
            tokf = A.alloc([16], F32)
            tokrow = A.alloc([16, 16], I32)
            ne, nt, csum, csum2, excl, sbase = [A.alloc([32], F32) for _ in range(6)]
            eot, used, widxf = [A.alloc([NTL], F32) for _ in range(3)]
            widx = A.alloc([NTL], I32)
            tosT = A.alloc([2 * NTL, 16], I32)
            tosf, yvalid, yidxf = [A.alloc([2 * NTL], F32) for _ in range(3)]
            yidx = A.alloc([2 * NTL], I32)
            m_ = A.top
            hn32 = [A.alloc([1024], F32) for _ in range(2)]
            hnb = [A.alloc([1024], BF16) for _ in range(2)]
            hnl = [A.alloc([1024], BF16) for _ in range(2)]
            hiT = [A.alloc([8, 128], BF16) for _ in range(2)]
            loT = [A.alloc([8, 128], BF16) for _ in range(2)]
            Bje = A.alloc([NTL, 32], F32)
            Aje = A.alloc([NTL, 32], F32)
            bigt = A.alloc([NSL * 16 // 128], I32)
            top_r = A.top
            A.top = m_
            Xg = [[A.alloc([1024], BF16) for _ in range(2)] for _ in range(3)]
            sa = A.alloc([512], F32)
            hdnT = A.alloc([2, 256], BF16)
            xgT = [A.alloc([8, 256], BF16) for _ in range(2)]
            ysb = [A.alloc([1024], BF16) for _ in range(2)]
            yk = [A.alloc([1024], BF16) for _ in range(4)]
            A.top = max(A.top, top_r)
            iota = rowbc[:, R_IOTA:R_IOTA + 96]
            c8g = rowbc[:, R_C8G:R_C8G + 4]
            pcol = cst[:, C_PCOL:C_PCOL + 1]
            pmB = cst[:, C_PMB:C_PMB + 1]
            BwR, Bhn32, Bhnb, Brt, Bcnt = Buf(), [Buf(), Buf()], [Buf(), Buf()], Buf(), Buf()
            Bhnl, BhiT, BloT, BpsH, BpsLo = [[Buf(), Buf()] for _ in range(5)]
            BwR0 = Buf()
            P.dma("sp", DMA(wR32, wR_d.rearrange("(k p) c -> p k c", p=128)), writes=[BwR0])
            P.op("dve", CP(Whi, wR32), reads=[BwR0], writes=[BwR])
            P.op("dve", TT(wRt, wR32, Whi, ALU.subtract), reads=[BwR0, BwR], writes=[BwR])
            P.op("dve", CP(Wlo, wRt), reads=[BwR], writes=[BwR])
            P.dma("pool", DMA(utri, utri_d[:, :]), writes=[Bcnt])
            P.op("dve", MSET(ssh, 0.0), writes=[Brt])
            P.op("pool", MSET(onesb, 1.0), writes=[Bcnt])
            P.op("pool", MSET(bigt, 1 << 16), writes=[Bcnt])
            P.dma("sp", DMA(tos_d.rearrange("(p r) w -> p (r w)", p=128), bigt), reads=[Bcnt], writes=[BtosD])
            psX = bank(6, 2)
            psL = bank(1)
            BpsX, BpsL = Buf(), Buf()
            gFbc = rowbc[:, R_GF:R_GF + 1024]
            for tt in range(16):
                P.op("act", ACTV(junk, hres[:, tt, :], AF.Square, accum_out=ssh[:, tt:tt + 1]), reads=[Bh[tt], Brt],
                     writes=[Brt])
            P.op("act", ACTV(sqh, ssh, AF.Sqrt, bias=epsb, scale=1.0 / D), reads=[Brt], writes=[Brt])
            P.op("dve", RECIP(rh, sqh), reads=[Brt], writes=[Brt])
            Blg = Buf()
            BpsXs, BpsLs = [Buf(), Buf()], [Buf(), Buf()]
            def rt_front(tt):
                i2 = tt % 2
                sl = slice(tt * 128, (tt + 1) * 128)
                P.op("dve", STT(hn32[i2], hres[:, tt, :], rh[:, tt:tt + 1], gFbc, ALU.mult, ALU.mult),
                     reads=[Bh[tt], Brt, Bcst], writes=[Bhn32[i2]])
                P.op("act", ACTV(hnb[i2], hn32[i2], AF.Copy), reads=[Bhn32[i2]], writes=[Bhnb[i2]])
                P.dma("sp", DMA(hn_d[sl, :], hnb[i2]), reads=[Bhnb[i2]], writes=[BhnD])
                P.op("dve", TT(hnl[i2], hn32[i2], hnb[i2], ALU.subtract), reads=[Bhn32[i2], Bhnb[i2]], writes=[Bhnl[i2]])
                pH, pL = bank_bf(4 + 2 * i2), bank_bf(5 + 2 * i2)
                for k in range(8):
                    P.op("pe", TR(pH[:, k * 128:(k + 1) * 128], hnb[i2][:, k * 128:(k + 1) * 128], identb),
                         reads=[Bhnb[i2]], writes=[BpsH[i2]])
                for k in range(8):
                    P.op("pe", TR(pL[:, k * 128:(k + 1) * 128], hnl[i2][:, k * 128:(k + 1) * 128], identb),
                         reads=[Bhnl[i2]], writes=[BpsLo[i2]])
                P.op("act", ACTV(hiT[i2], pH.rearrange("p (k t) -> p k t", k=8), AF.Copy), reads=[BpsH[i2]], writes=[BhiT[i2]])
                P.op("act", ACTV(loT[i2], pL.rearrange("p (k t) -> p k t", k=8), AF.Copy), reads=[BpsLo[i2]], writes=[BloT[i2]])

            def rt_back(tt):
                i2 = tt % 2
                pl = bank(1 + i2)[:, 0:36]
                n_ = 0
                for (xT_, W_, Bx) in ((hiT[i2], Whi, BhiT[i2]), (loT[i2], Whi, BloT[i2]), (hiT[i2], Wlo, BhiT[i2])):
                    for k in range(8):
                        P.op("pe", MM(pl, xT_[:, k, :], W_[:, k, :], n_ == 0, n_ == 23), reads=[Bx, BwR], writes=[BpsLs[i2]])
                        n_ += 1
                P.op("act", ACTV(lg[:, tt, :], pl, AF.Copy), reads=[BpsLs[i2]], writes=[Blg])

            rt_front(0)
            for tt in range(16):
                if tt + 1 < 16:
                    rt_front(tt + 1)
                rt_back(tt)

            def R(eng, fn):
                return P.op(eng, fn, reads=[Brt, Blg, Bcst], writes=[Brt])

            def col(t):
                return t.rearrange("p (t o) -> p t o", o=1)
            R("dve", TT(lg, lg, bc(rowbc[:, R_BR:R_BR + 36].rearrange("p (o c) -> p o c", o=1), [128, 16, 36]), ALU.add))
            gl = lg[:, :, 0:4]
            R("dve", lambda e: e.tensor_reduce(out=gmax, in_=gl, axis=AX.X, op=ALU.max))
            R("dve", TT(ohg, gl, bc(col(gmax), [128, 16, 4]), ALU.is_equal))
            R("dve", TT(gsh, gl, bc(col(gmax), [128, 16, 4]), ALU.subtract))
            R("act", ACTV(gsh, gsh, AF.Exp))
            R("dve", RSUM(sume, gsh))
            R("dve", RECIP(pg, sume))
            R("dve", CP(t32, lg[:, :, 4:36]))
            el = t32.rearrange("p t (g e) -> p (t g) e", g=4)
            R("dve", TT(selg, el, bc(ohg.rearrange("p t g -> p (t g)").rearrange("p (x o) -> p x o", o=1), [128, 64, 8]), ALU.mult))
            R("dve", RSUM(sel, selg.rearrange("p (t g) e -> p t e g", g=4)))
            R("dve", lambda e: e.tensor_reduce(out=v0, in_=sel, axis=AX.X, op=ALU.max))
            R("dve", TT(m1, sel, bc(col(v0), [128, 16, 8]), ALU.is_equal))
            R("dve", STT(sel2, m1, -1e30, sel, ALU.mult, ALU.add))
            R("dve", lambda e: e.tensor_reduce(out=v1, in_=sel2, axis=AX.X, op=ALU.max))
            R("dve", TT(m2, sel2, bc(col(v1), [128, 16, 8]), ALU.is_equal))
            R("dve", TT(dlt, v1, v0, ALU.subtract))
            R("act", ACTV(exd, dlt, AF.Exp))
            R("dve", TS(w1, exd, 1.0, ALU.add))
            R("dve", RECIP(w1, w1))
            R("dve", TT(w2, exd, w1, ALU.mult))
            R("dve", TT(g12[:, :, 0], w1, pg, ALU.mult))
            R("dve", TT(g12[:, :, 1], w2, pg, ALU.mult))
            R("dve", TT(gsh, ohg, bc(c8g.rearrange("p (o g) -> p o g", o=1), [128, 16, 4]), ALU.mult))
            R("dve", RSUM(gi8, gsh))
            io8 = bc(iota[:, 0:8].rearrange("p (o e) -> p o e", o=1), [128, 16, 8])
            R("dve", TT(tm8, m1, io8, ALU.mult))
            R("dve", RSUM(i1, tm8))
            R("dve", TT(tm8, m2, io8, ALU.mult))
            R("dve", RSUM(i2_, tm8))
            R("dve", TT(eid1, gi8, i1, ALU.add))
            R("dve", TT(eid2, gi8, i2_, ALU.add))
            io32 = bc(iota[:, 0:32].rearrange("p (o e) -> p o e", o=1), [128, 16, 32])
            R("dve", TT(oh1, io32, bc(col(eid1), [128, 16, 32]), ALU.is_equal))
            R("dve", TT(oh2, io32, bc(col(eid2), [128, 16, 32]), ALU.is_equal))
            R("dve", TT(OH, oh1, oh2, ALU.add))
            R("dve", CP(OHb, OH))
            psC = bank(0)
            psN = bank(2)
            BpsC, BpsN = Buf(), Buf()
            for tt in range(16):
                for j in range(tt):
                    P.op("pe", MM(psC[:, tt * 32:(tt + 1) * 32], onesb, OHb[:, j, :], j == 0, False, skip=True),
                         reads=[Brt, Bcnt], writes=[BpsC])
                P.op("pe", MM(psC[:, tt * 32:(tt + 1) * 32], utri, OHb[:, tt, :], tt == 0, True, skip=True),
                     reads=[Brt, Bcnt], writes=[BpsC])
            for tt in range(16):
                P.op("pe", MM(psN[:, 0:32], onesb, OHb[:, tt, :], tt == 0, tt == 15), reads=[Brt, Bcnt], writes=[BpsN])
            P.op("dve", CP(incl, psC.rearrange("p (t e) -> p t e", t=16)), reads=[BpsC], writes=[Brt])
            P.op("dve", CP(ne, psN[:, 0:32]), reads=[BpsN], writes=[Brt])
            R("dve", TT(t32, oh1, incl, ALU.mult))
            R("dve", RSUM(rank1, t32))
            R("dve", TT(t32, oh2, incl, ALU.mult))
            R("dve", RSUM(rank2, t32))
            R("dve", TSS(nt, ne, 0.0, ALU.is_gt))
            for j in range(1, 8):
                R("dve", STT(nt, ne, 256.0 * j, nt, ALU.is_gt, ALU.add))
            R("dve", CP(csum, nt))
            cur, oth = csum, csum2
            for s_ in (1, 2, 4, 8, 16):
                R("dve", CP(oth[:, 0:s_], cur[:, 0:s_]))
                R("dve", TT(oth[:, s_:32], cur[:, s_:32], cur[:, 0:32 - s_], ALU.add))
                cur, oth = oth, cur
            cfin = cur
            R("dve", TT(excl, cfin, nt, ALU.subtract))
            R("dve", TS(sbase, excl, 256.0, ALU.mult))
            sb3 = bc(sbase.rearrange("p (o e) -> p o e", o=1), [128, 16, 32])
            R("dve", TT(t32, oh1, sb3, ALU.mult))
            R("dve", RSUM(posf2[:, :, 0], t32))
            R("dve", TT(t32, oh2, sb3, ALU.mult))
            R("dve", RSUM(posf2[:, :, 1], t32))
            R("dve", TT(posf2[:, :, 0], posf2[:, :, 0], rank1, ALU.add))
            R("dve", TT(posf2[:, :, 1], posf2[:, :, 1], rank2, ALU.add))
            R("dve", TS(posf2, posf2, -1.0, ALU.add))
            R("dve", CP(posi2, posf2))
            jt = bc(iota[:, 0:NTL].rearrange("p (j o) -> p j o", o=1), [128, NTL, 32])
            R("dve", TT(Aje, jt, bc(excl.rearrange("p (o e) -> p o e", o=1), [128, NTL, 32]), ALU.is_ge))
            R("dve", TT(Bje, jt, bc(cfin.rearrange("p (o e) -> p o e", o=1), [128, NTL, 32]), ALU.is_lt))
            R("dve", TT(Aje, Aje, Bje, ALU.mult))
            R("dve", RSUM(used, Aje))
            R("dve", TT(Bje, Aje, bc(iota[:, 0:32].rearrange("p (o e) -> p o e", o=1), [128, NTL, 32]), ALU.mult))
            R("dve", RSUM(eot, Bje))
            R("dve", TS(widxf, eot, 128.0, ALU.mult, pcol, ALU.add))
            R("dve", TS(used, used, -BIG, ALU.mult, BIG, ALU.add))
            R("dve", TT(widxf, widxf, used, ALU.add))
            R("dve", CP(widx, widxf))
            R("dve", TS(tokf, iota[:, 0:16], 128.0, ALU.mult, pcol, ALU.add))
            R("dve", CP(tokrow, bc(col(tokf), [128, 16, 16])))
            dump("posi2", posi2, [128, 16, 2], I32, [Brt])
            dump("widx", widx, [128, NTL], I32, [Brt])
            dump("g12", g12, [128, 16, 2], F32, [Brt])
            Btos_list = []
            for tt in range(16):
                for k in range(2):
                    bt = Buf()
                    Btos_list.append(bt)
                    P.dma("pool", IDMA(tos_d[:, :], tokrow[:, tt, :], out_off=posi2[:, tt, k:k + 1], bound=NSL - 1),
                          reads=[Brt, BtosD], writes=[bt])
            BtosT = Buf()
            P.dma("sp", DMA(tosT, tos_d.rearrange("(js p) w -> p js w", p=128)), reads=[BtosD] + Btos_list, writes=[BtosT])
            P.op("dve", CP(tosf, tosT[:, :, 0]), reads=[BtosT], writes=[BtosT])
            P.op("dve", TSS(yvalid, tosf, 2048.0, ALU.is_lt), reads=[BtosT], writes=[BtosT])
            P.op("dve", TS(yidxf, rowbc[:, R_IOTA:R_IOTA + 2 * NTL], 128.0, ALU.mult, pmB, ALU.add),
                 reads=[BtosT, Bcst], writes=[BtosT])
            P.op("dve", TT(yidxf, yidxf, yvalid, ALU.mult), reads=[BtosT], writes=[BtosT])
            P.op("dve", TS(yidxf, yidxf, BIG, ALU.add), reads=[BtosT], writes=[BtosT])
            P.op("dve", CP(yidx, yidxf), reads=[BtosT], writes=[BtosT])
            dump("tosT", tosT, [128, 2 * NTL, 16], I32, [BtosT])
            dump("yidx", yidx, [128, 2 * NTL], I32, [BtosT])
            P.fence()
            BXg0 = Buf()
            for a_ in range(3):
                for b_ in range(2):
                    P.op("pool", MSET(Xg[a_][b_], 0.0), writes=[BXg0])
            BWg, BWu, BWd = [Buf(), Buf(), Buf()], [Buf(), Buf(), Buf()], [Buf(), Buf(), Buf()]
            BYd_list = []
            BXg = [[Buf(), Buf()], [Buf(), Buf()], [Buf(), Buf()]]
            BxgT = [Buf(), Buf()]
            Bsa, Bhd, Bpa, Bpu, Bpy, Bysb = Buf(), Buf(), Buf(), Buf(), [Buf(), Buf()], [Buf(), Buf()]
            BpsXg = [Buf(), Buf()]
            psa, psu = bank(0), bank(1)
            NT_RUN = NTL
            for nm in dbg:
                if nm.startswith("ntl"):
                    NT_RUN = int(nm[3:])
            def ffn_xgathers(j):
                wb = j % 3
                for s_ in range(2):
                    js = 2 * j + s_
                    P.dma("pool", IDMA(Xg[wb][s_], hn_d[:, :], in_off=tosT[:, js, 0:1], bound=S - 1),
                          reads=[BtosT, BhnD, BXg0], writes=[BXg[wb][s_]])

            def ffn_gathers(j):
                wb = j % 3
                P.dma("pool", IDMA(Wg[wb].rearrange("p k f -> p (k f)"), wGt_d[:, :], in_off=widx[:, j:j + 1], bound=4095),
                      reads=[Brt], writes=[BWg[wb]])
                P.dma("pool", IDMA(Wu[wb].rearrange("p k f -> p (k f)"), wUt_d[:, :], in_off=widx[:, j:j + 1], bound=4095),
                      reads=[Brt], writes=[BWu[wb]])
                P.dma("pool", IDMA(Wd[wb].rearrange("p c d -> p (c d)"), wDt_d[:, :], in_off=widx[:, j:j + 1], bound=4095),
                      reads=[Brt], writes=[BWd[wb]])

            def ffn_transposes(j):
                wb = j % 3
                xb_ = j % 2
                for s_ in range(2):
                    pX = bank_bf(6 + s_)
                    for k in range(8):
                        P.op("pe", TR(pX[:, k * 128:(k + 1) * 128], Xg[wb][s_][:, k * 128:(k + 1) * 128], identb),
                             reads=[BXg[wb][s_]], writes=[BpsXg[s_]])
                    P.op("act" if s_ == 0 else "dve",
                         (ACTV(xgT[xb_][:, :, s_ * 128:(s_ + 1) * 128], pX.rearrange("p (k t) -> p k t", k=8), AF.Copy) if s_ == 0
                          else CP(xgT[xb_][:, :, s_ * 128:(s_ + 1) * 128], pX.rearrange("p (k t) -> p k t", k=8))),
                         reads=[BpsXg[s_]], writes=[BxgT[xb_]])

            def ffn_compute(j):
                wb = j % 3
                xb_ = j % 2
                for fc in range(4):
                    pb, Bp = (psa, Bpa) if fc < 2 else (psu, Bpu)
                    Wsrc = Wg[wb] if fc < 2 else Wu[wb]
                    BWs = BWg[wb] if fc < 2 else BWu[wb]
                    for k in range(8):
                        P.op("pe", MM(pb[:, (fc % 2) * 256:(fc % 2) * 256 + 256], Wsrc[:, k, (fc % 2) * 128:(fc % 2) * 128 + 128],
                                      xgT[xb_][:, k, :], k == 0, k == 7), reads=[BWs, BxgT[xb_]], writes=[Bp])
                P.op("act", ACTV(sa, psa, AF.Silu), reads=[Bpa], writes=[Bsa])
                P.op("dve", TT(hdnT.rearrange("p c t -> p (c t)"), sa, psu, ALU.mult), reads=[Bsa, Bpu], writes=[Bhd])

            def ffn_down(j):
                wb = j % 3
                for s_ in range(2):
                    js = 2 * j + s_
                    ys = js % 2
                    py = bank(2 + 2 * ys, 2)
                    for hf in range(2):
                        for c2 in range(2):
                            P.op("pe", MM(py[:, hf * 512:(hf + 1) * 512], hdnT[:, c2, s_ * 128:(s_ + 1) * 128],
                                          Wd[wb][:, c2, hf * 512:(hf + 1) * 512], c2 == 0, c2 == 1),
                                 reads=[Bhd, BWd[wb]], writes=[Bpy[ys]])
                    P.op("act" if ys == 0 else "dve",
                         (ACTV(ysb[ys], py, AF.Copy) if ys == 0 else CP(ysb[ys], py)), reads=[Bpy[ys]], writes=[Bysb[ys]])
                    byd = Buf()
                    BYd_list.append(byd)
                    P.dma("sp", DMA(Y_d[js * 128:(js + 1) * 128, :], ysb[ys]), reads=[Bysb[ys]], writes=[byd])

            for j0 in range(min(3, NT_RUN)):
                ffn_xgathers(j0)
                ffn_gathers(j0)
            ffn_transposes(0)
            if NT_RUN > 3:
                ffn_xgathers(3)
            for j in range(NT_RUN):
                ffn_compute(j)
                if j + 1 < NT_RUN:
                    ffn_transposes(j + 1)
                    if j + 4 < NT_RUN:
                        ffn_xgathers(j + 4)
                ffn_down(j)
                if j + 3 < NT_RUN:
                    ffn_gathers(j + 3)
            P.fence()
            yk_all = list(yk) + list(ysb)
            Byk = [Buf() for _ in range(len(yk_all))]
            cnt_ = 0
            for tt in range(16):
                for k in range(2):
                    bi = cnt_ % len(yk_all)
                    cnt_ += 1
                    P.dma("pool", IDMA(yk_all[bi], Y_d[:, :], in_off=posi2[:, tt, k:k + 1], bound=NSL - 1),
                          reads=BYd_list + [Brt], writes=[Byk[bi]])
                    P.op("dve", STT(hres[:, tt, :], yk_all[bi], g12[:, tt, k:k + 1], hres[:, tt, :], ALU.mult, ALU.add),
                         reads=[Byk[bi], Brt, Bh[tt]], writes=[Bh[tt]])
                if tt >= 1:
                    final_tile(tt - 1)
            final_tile(15)
            FINAL_DONE[0] = True
        elif "noF" not in dbg:
            P.fence()
            A.top = mark_after_hres
            gF_off = R_GF
            hn32 = [A.alloc([1024], F32) for _ in range(2)]
            hnT32 = A.alloc([8, 128], F32)
            wR32 = A.alloc([8, 36], F32)
            hnT = R1.rearrange("p a b -> p (a b)")[:, 0:8 * S].rearrange("p (k t) -> p k t", k=8)
            ssh = A.alloc([16], F32)
            sqh = A.alloc([16], F32)
            rh = A.alloc([16], F32)
            lg = A.alloc([36], F32)
            g8 = A.alloc([8], F32)
            m8 = A.alloc([8], F32)
            m8b = A.alloc([8], F32)
            ohg = A.alloc([4], F32)
            negm = A.alloc([1], F32)
            ejunk = A.alloc([4], F32)
            sume = A.alloc([1], F32)
            pg = A.alloc([1], F32)
            selg = A.alloc([4, 8], F32)
            sel = A.alloc([8], F32)
            m1 = A.alloc([8], F32)
            m2 = A.alloc([8], F32)
            dlt = A.alloc([1], F32)
            exd = A.alloc([1], F32)
            w12 = A.alloc([2], F32)
            g12 = A.alloc([16, 2], F32)
            me = A.alloc([8], F32)
            gates = A.alloc([16, 32], F32)
            BwR, Bhn32, BhnT32, BhnT, Brt, Bgates = Buf(), [Buf(), Buf()], Buf(), Buf(), Buf(), Buf()
            P.dma("sp", DMA(wR32, wR_d.rearrange("(k p) c -> p k c", p=128)), writes=[BwR])
            P.op("dve", MSET(ssh, 0.0), writes=[Brt])
            P.op("dve", MSET(g8, -1e30), writes=[Brt])
            psX = bank(6, 2)
            psL = bank(1)
            BpsX, BpsL = Buf(), Buf()
            gFbc = rowbc[:, gF_off:gF_off + 1024]
            lvl = 9
            for nm in dbg:
                if nm.startswith("stoprt"):
                    lvl = int(nm[6:])
            for tt in range(16):
                i2 = tt % 2
                sl = slice(tt * 128, (tt + 1) * 128)
                P.op("act", ACTV(junk, hres[:, tt, :], AF.Square, accum_out=ssh[:, tt:tt + 1]), reads=[Bh[tt], Brt],
                     writes=[Brt])
                P.op("act", ACTV(sqh[:, tt:tt + 1], ssh[:, tt:tt + 1], AF.Sqrt, bias=epsb, scale=1.0 / D), reads=[Brt], writes=[Brt])
                P.op("dve", RECIP(rh[:, tt:tt + 1], sqh[:, tt:tt + 1]), reads=[Brt], writes=[Brt])
                P.op("dve", STT(hn32[i2], hres[:, tt, :], rh[:, tt:tt + 1], gFbc, ALU.mult, ALU.mult),
                     reads=[Bh[tt], Brt, Bcst], writes=[Bhn32[i2]])
                if lvl < 2:
                    continue
                for k in range(8):
                    P.op("pe", MM(psX[:, k * 128:(k + 1) * 128], hn32[i2][:, k * 128:(k + 1) * 128], identf, True, True),
                         reads=[Bhn32[i2]], writes=[BpsX])
                P.op("act", ACTV(hnT32, psX.rearrange("p (k t) -> p k t", k=8), AF.Copy), reads=[BpsX], writes=[BhnT32])
                P.op("pool", CP(hnT[:, :, sl], hnT32), reads=[BhnT32], writes=[BhnT])
                if lvl < 3:
                    continue
                for k in range(8):
                    P.op("pe", MM(psL[:, 0:36], hnT32[:, k, :], wR32[:, k, :], k == 0, k == 7), reads=[BhnT32, BwR], writes=[BpsL])
                P.op("dve", TT(lg, psL[:, 0:36], rowbc[:, R_BR:R_BR + 36], ALU.add), reads=[BpsL, Bcst], writes=[Brt])
                if lvl < 4:
                    continue
                P.op("dve", CP(g8[:, 0:4], lg[:, 0:4]), reads=[Brt], writes=[Brt])
                P.op("dve", lambda e: e.max(out=m8, in_=g8), reads=[Brt], writes=[Brt])
                P.op("dve", TS(ohg, lg[:, 0:4], m8[:, 0:1], ALU.is_equal), reads=[Brt], writes=[Brt])
                P.op("dve", TS(negm, m8[:, 0:1], -1.0, ALU.mult), reads=[Brt], writes=[Brt])
                P.op("act", ACTV(ejunk, lg[:, 0:4], AF.Exp, bias=negm, accum_out=sume), reads=[Brt], writes=[Brt])
                P.op("dve", RECIP(pg, sume), reads=[Brt], writes=[Brt])
                el = lg[:, 4:36].rearrange("p (g e) -> p g e", g=4)
                P.op("dve", TT(selg, el, bc(ohg.rearrange("p (g o) -> p g o", o=1), [128, 4, 8]), ALU.mult), reads=[Brt], writes=[Brt])
                P.op("dve", RSUM(sel, selg.rearrange("p g e -> p e g")), reads=[Brt], writes=[Brt])
                P.op("dve", lambda e: e.max(out=m8b, in_=sel), reads=[Brt], writes=[Brt])
                P.op("dve", TS(m1, sel, m8b[:, 0:1], ALU.is_equal), reads=[Brt], writes=[Brt])
                P.op("dve", TS(m2, sel, m8b[:, 1:2], ALU.is_equal), reads=[Brt], writes=[Brt])
                P.op("dve", TT(dlt, m8b[:, 1:2], m8b[:, 0:1], ALU.subtract), reads=[Brt], writes=[Brt])
                P.op("act", ACTV(exd, dlt, AF.Exp), reads=[Brt], writes=[Brt])
                P.op("dve", TS(w12[:, 0:1], exd, 1.0, ALU.add), reads=[Brt], writes=[Brt])
                P.op("dve", RECIP(w12[:, 0:1], w12[:, 0:1]), reads=[Brt], writes=[Brt])
                P.op("dve", TT(w12[:, 1:2], exd, w12[:, 0:1], ALU.mult), reads=[Brt], writes=[Brt])
                P.op("dve", TS(g12[:, tt, :], w12, pg[:, 0:1], ALU.mult), reads=[Brt], writes=[Brt])
                P.op("dve", TS(me, m1, g12[:, tt, 0:1], ALU.mult), reads=[Brt], writes=[Brt])
                P.op("dve", STT(me, m2, g12[:, tt, 1:2], me, ALU.mult, ALU.add), reads=[Brt], writes=[Brt])
                P.op("dve", TT(gates[:, tt, :].rearrange("p (g e) -> p g e", g=4),
                               bc(me.rearrange("p (o e) -> p o e", o=1), [128, 4, 8]),
                               bc(ohg.rearrange("p (g o) -> p g o", o=1), [128, 4, 8]), ALU.mult), reads=[Brt], writes=[Bgates])
            dump("gates", gates, [128, 16, 32], F32, [Bgates])
            dump("hnT", hnT, [128, 8, S], BF16, [BhnT])
            P.fence()
            DENSE_EXPERTS = N_EXP
            for nm in dbg:
                if nm.startswith("nexp"):
                    DENSE_EXPERTS = int(nm[4:])
            Wgu = [A.alloc([8, 512], BF16) for _ in range(2)]
            Wd = [A.alloc([2, 1024], BF16) for _ in range(2)]
            sa = A.alloc([512], F32)
            hdnT = A.alloc([2, 256], BF16)
            BW = [Buf(), Buf()]
            Bsa, Bhd, Bpa, Bpu, Bpy = Buf(), Buf(), Buf(), Buf(), [Buf(), Buf()]
            psa, psu = bank(0), bank(1)
            ycnt = 0
            for e_ in range(DENSE_EXPERTS):
                wb = e_ % 2
                er = slice(e_ * 128, (e_ + 1) * 128)
                P.dma("pool", DMA(Wgu[wb][:, :, 0:256], wGt_d[er, :].rearrange("p (k f) -> p k f", k=8)), writes=[BW[wb]])
                P.dma("pool", DMA(Wgu[wb][:, :, 256:512], wUt_d[er, :].rearrange("p (k f) -> p k f", k=8)), writes=[BW[wb]])
                P.dma("pool", DMA(Wd[wb], wDt_d[er, :].rearrange("p (c d) -> p c d", c=2)), writes=[BW[wb]])
                for pr in range(8):
                    tok = slice(pr * 256, (pr + 1) * 256)
                    for fc in range(4):
                        pb, Bp = (psa, Bpa) if fc < 2 else (psu, Bpu)
                        for k in range(8):
                            P.op("pe", MM(pb[:, (fc % 2) * 256:(fc % 2) * 256 + 256], Wgu[wb][:, k, fc * 128:(fc + 1) * 128],
                                          hnT[:, k, tok], k == 0, k == 7), reads=[BW[wb], BhnT], writes=[Bp])
                    P.op("act", ACTV(sa, psa, AF.Silu), reads=[Bpa], writes=[Bsa])
                    P.op("dve", TT(hdnT.rearrange("p c t -> p (c t)"), sa, psu, ALU.mult), reads=[Bsa, Bpu], writes=[Bhd])
                    for sub in range(2):
                        tt = 2 * pr + sub
                        ys = ycnt % 2
                        ycnt += 1
                        py = bank(2 + 2 * ys, 2)
                        for hf in range(2):
                            for c2 in range(2):
                                P.op("pe", MM(py[:, hf * 512:(hf + 1) * 512], hdnT[:, c2, sub * 128:(sub + 1) * 128],
                                              Wd[wb][:, c2, hf * 512:(hf + 1) * 512], c2 == 0, c2 == 1),
                                     reads=[Bhd, BW[wb]], writes=[Bpy[ys]])
                        P.op("dve", STT(hres[:, tt, :], py, gates[:, tt, e_:e_ + 1], hres[:, tt, :], ALU.mult, ALU.add),
                             reads=[Bpy[ys], Bgates, Bh[tt]], writes=[Bh[tt]])
        if not FINAL_DONE[0]:
            for tt in range(16):
                final_tile(tt)
        print('SBUF arena peak bytes', A.peak, 'cap', A.cap)
        P.emit(nc, block, sems, dsems, finals)
    return nc, dbg_d


_NC_CACHE = {}


def kernel(**inputs):
    inp = {k: np.asarray(v) for k, v in inputs.items()}
    if "nc" not in _NC_CACHE:
        _NC_CACHE["nc"] = build_nc()[0]
    nc = _NC_CACHE["nc"]
    sh = prep_shared(inp)
    in_maps = []
    for b in range(8):
        m = dict(sh)
        m["x"] = np.ascontiguousarray(inp["x"][b], dtype=np.float32)
        m["pos"] = np.ascontiguousarray(inp["positions"][b:b + 1]).astype(np.int32)
        in_maps.append(m)
    res = run_bass_kernel_spmd(nc, in_maps, core_ids=list(range(8)))
    out = np.stack([np.asarray(r["out"], dtype=np.float32) for r in res.results], axis=0)
    return out
```

```python
import numpy as np
import ml_dtypes
import concourse.bass as bass
import concourse.mybir as mybir
from concourse.bass_utils import run_bass_kernel_spmd

F32 = mybir.dt.float32
BF16 = mybir.dt.bfloat16
I32 = mybir.dt.int32
AF = mybir.ActivationFunctionType
ALU = mybir.AluOpType
AX = mybir.AxisListType


class Buf:
    __slots__ = ("name", "writer", "readers")

    def __init__(self, name=""):
        self.name = name
        self.writer = None
        self.readers = []


class Op:
    __slots__ = ("eng", "fn", "deps", "signal", "val", "sem", "is_dma", "idx")

    def __init__(self, eng, fn, is_dma=False):
        self.eng = eng
        self.fn = fn
        self.deps = []
        self.signal = False
        self.val = None
        self.sem = None
        self.is_dma = is_dma
        self.idx = None


ENGS = ("pe", "act", "dve", "pool", "sp")


class Plan:
    def __init__(self, n_dma_sems=24):
        self.ops = {e: [] for e in ENGS}
        self.n_dma_sems = n_dma_sems
        self.dma_count = 0
        self.dma_counts = [0, 0]
        self.dma_last = [None] * n_dma_sems
        self.nops = 0

    def _add(self, op, reads, writes, deps):
        op.idx = self.nops
        self.nops += 1
        dl = []
        for b in reads:
            if b.writer is not None:
                dl.append((b.writer, "raw"))
        for b in writes:
            if b.writer is not None:
                dl.append((b.writer, "waw"))
            for r in b.readers:
                dl.append((r, "war"))
        for d in deps:
            if d is not None:
                dl.append((d, "raw"))
        for b in reads:
            b.readers.append(op)
        for b in writes:
            b.writer = op
            b.readers = []
        seen = set()
        for d, kind in dl:
            if d is op or id(d) in seen:
                continue
            if (not d.is_dma) and d.eng == op.eng and not op.is_dma:
                if d.eng == "pe":
                    continue
            seen.add(id(d))
            d.signal = True
            op.deps.append(d)
        self.ops[op.eng].append(op)
        return op

    def op(self, eng, fn, reads=(), writes=(), deps=()):
        return self._add(Op(eng, fn), list(reads), list(writes), list(deps))

    def dma(self, eng, fn, reads=(), writes=(), deps=()):
        op = Op(eng, fn, is_dma=True)
        half = self.n_dma_sems // 2
        grp = 1 if eng == "pool" else 0
        cnt = self.dma_counts[grp]
        s = grp * half + cnt % half
        op.sem = s
        op.val = 16 * (cnt // half + 1)
        self.dma_counts[grp] += 1
        self.dma_count += 1
        deps = list(deps)
        if self.dma_last[s] is not None:
            deps.append(self.dma_last[s])
        self.dma_last[s] = op
        op.signal = True
        return self._add(op, list(reads), list(writes), deps)

    def emit(self, nc, block, sems, dma_sems, final_waits):
        for e in ENGS:
            c = 0
            for op in self.ops[e]:
                if op.is_dma:
                    continue
                if op.signal:
                    c += 1
                    op.val = c
        plan = self

        def run(eng_name, eng):
            waited = {}
            for op in plan.ops[eng_name]:
                need = {}
                for d in op.deps:
                    key = ("dma", d.sem) if d.is_dma else ("eng", d.eng)
                    if d.val > need.get(key, 0):
                        need[key] = d.val
                for key, v in need.items():
                    if waited.get(key, 0) >= v:
                        continue
                    waited[key] = v
                    sem = dma_sems[key[1]] if key[0] == "dma" else sems[key[1]]
                    eng.wait_ge(sem, v)
                if op.fn is None:
                    continue
                ins = op.fn(eng)
                if op.is_dma:
                    ins.then_inc(dma_sems[op.sem], 16)
                elif op.signal:
                    ins.then_inc(sems[eng_name], 1)
            if eng_name == "sp":
                for d in final_waits:
                    sem = dma_sems[d.sem] if d.is_dma else sems[d.eng]
                    eng.wait_ge(sem, d.val)

        @block.tensor
        def _(pe):
            run("pe", pe)

        @block.scalar
        def _(act):
            run("act", act)

        @block.vector
        def _(dve):
            run("dve", dve)

        @block.gpsimd
        def _(pool):
            run("pool", pool)

        @block.sync
        def _(sp):
            run("sp", sp)


S = 2048
D = 1024
NT = 16
EPS = 1e-6
THETA = 10000.0
LAM_INIT = 0.8 - 0.6 * 1.0
NEG = -30000.0
N_EXP = 32
DFF = 256

C_GA, C_GQKV, C_GF, C_INVD, C_INVM, C_SGND, C_SGNM, C_DIVQ, C_PCOL, C_PMB, NCST = 0, 8, 13, 21, 22, 23, 24, 25, 27, 28, 29
R_FING, R_SUBG, R_LAM, R_BR, R_GF, R_IOTA, R_C8G, NROW = 0, 1024, 1152, 1408, 1444, 2468, 2468 + 96, 2468 + 100


class Arena:
    def __init__(self, nc, es, nbytes):
        self.t = es.enter_context(nc.sbuf_tensor("arena", [128, nbytes // 2], BF16))
        self.top = 0
        self.cap = nbytes
        self.peak = 0

    def alloc(self, free_shape, dt):
        esz = 4 if dt in (F32, I32) else 2
        n = int(np.prod(free_shape))
        nb = (n * esz + 63) // 64 * 64
        off = self.top
        self.top += nb
        self.peak = max(self.peak, self.top)
        assert self.top <= self.cap, ("SBUF arena overflow", self.top, self.cap)
        v = self.t[:, off // 2: off // 2 + (n * esz) // 2]
        if dt != BF16:
            v = v.bitcast(dt)
        if len(free_shape) == 2:
            v = v.rearrange("p (a b) -> p a b", a=free_shape[0])
        elif len(free_shape) == 3:
            v = v.rearrange("p (a b c) -> p a b c", a=free_shape[0], b=free_shape[1])
        return v


def bc(ap, shape):
    return ap.to_broadcast(list(shape))


def MM(out, lhsT, rhs, start=True, stop=True, skip=False):
    if skip:
        return lambda e: e.matmul(out, lhsT, rhs, start=start, stop=stop, skip_group_check=True)
    return lambda e: e.matmul(out, lhsT, rhs, start=start, stop=stop)


def TR(out, in_, ident):
    return lambda e: e.transpose(out, in_, ident)


def ACTV(out, in_, func, bias=None, scale=1.0, accum_out=None):
    kw = {}
    if bias is not None:
        kw["bias"] = bias
    if accum_out is not None:
        kw["accum_out"] = accum_out
    return lambda e: e.activation(out=out, in_=in_, func=func, scale=scale, **kw)


def TT(out, in0, in1, op):
    return lambda e: e.tensor_tensor(out=out, in0=in0, in1=in1, op=op)


def TS(out, in0, s1, op0, s2=None, op1=None):
    if op1 is None:
        return lambda e: e.tensor_scalar(out=out, in0=in0, scalar1=s1, scalar2=None, op0=op0)
    return lambda e: e.tensor_scalar(out=out, in0=in0, scalar1=s1, scalar2=s2, op0=op0, op1=op1)


def STT(out, in0, scalar, in1, op0, op1):
    return lambda e: e.scalar_tensor_tensor(out=out, in0=in0, scalar=scalar, in1=in1, op0=op0, op1=op1)


def CP(out, in_):
    return lambda e: e.tensor_copy(out=out, in_=in_)


def MSET(out, val):
    return lambda e: e.memset(out, val)


def RECIP(out, in_):
    return lambda e: e.reciprocal(out=out, in_=in_)


def RSUM(out, in_):
    return lambda e: e.reduce_sum(out=out, in_=in_, axis=AX.X)


def DMA(out, in_):
    return lambda e: e.dma_start(out=out, in_=in_)


def _rot_cols(base, n, grp):
    idx = np.arange(n).reshape(-1, grp)
    half = grp // 2
    idx = np.concatenate([idx[:, half:], idx[:, :half]], axis=1).reshape(-1)
    return base + idx


def prep_shared(inp):
    f = np.float32
    w_in = np.asarray(inp["w_in"])[0]
    wAV = np.ascontiguousarray(np.concatenate([w_in[:, 0:640], w_in[:, 1696:2208]], axis=1))
    blocks = []
    for base in (672, 1184):
        for j in range(4):
            blocks.append(w_in[:, base + j * 128: base + (j + 1) * 128])
            blocks.append(w_in[:, _rot_cols(base + j * 128, 128, 64)])
    kpe = np.zeros((D, 128), f)
    kpe[:, 64:96] = w_in[:, 640:672]
    kper = np.zeros((D, 128), f)
    kper[:, 64:96] = w_in[:, _rot_cols(640, 32, 32)]
    blocks += [kpe, kper]
    wF = np.ascontiguousarray(np.concatenate(blocks, axis=1))
    w_uq = np.asarray(inp["w_uq"])[0]
    rot = np.zeros_like(w_uq)
    for h in range(8):
        rot[:, h * 96 + 64: h * 96 + 96] = w_uq[:, _rot_cols(h * 96 + 64, 32, 32)]
    wUQ = np.ascontiguousarray(np.concatenate([w_uq, rot], axis=1))
    w_ukv = np.asarray(inp["w_ukv"])[0]
    kcols = np.concatenate([np.arange(h * 128, h * 128 + 64) for h in range(8)])
    wUKV = np.ascontiguousarray(np.concatenate([w_ukv[:, kcols], w_ukv[:, kcols + 64]], axis=1))
    cst = np.zeros((128, NCST), f)
    p = np.arange(128)
    cst[:, C_GA:C_GA + 8] = np.asarray(inp["attn_norm_g"])[0].reshape(8, 128).T
    cst[:, C_GQKV:C_GQKV + 3] = np.asarray(inp["q_norm_g"])[0].reshape(3, 128).T
    cst[:, C_GQKV + 3:C_GQKV + 5] = np.asarray(inp["kv_norm_g"])[0].reshape(2, 128).T
    cst[:, C_GF:C_GF + 8] = np.asarray(inp["ffn_norm_g"])[0].reshape(8, 128).T
    invd = (f(THETA) ** (-(p % 32).astype(f) / f(32))).astype(f)
    invm = (f(THETA) ** (-(p % 16).astype(f) / f(16))).astype(f)
    cst[:, C_INVD] = invd / f(2 * np.pi)
    cst[:, C_INVM] = invm / f(2 * np.pi)
    cst[:, C_SGND] = np.where((p % 64) < 32, -1.0, 1.0)
    cst[:, C_SGNM] = np.where((p % 32) < 16, -1.0, 1.0)
    cst[:, C_DIVQ] = 1.0 / 384
    cst[:, C_DIVQ + 1] = 1.0 / 256
    cst[:, C_PCOL] = p
    cst[:, C_PMB] = p - float(1 << 16)
    rowc = np.zeros((1, NROW), f)
    rowc[0, R_FING:R_FING + 1024] = np.asarray(inp["final_norm_g"])
    rowc[0, R_SUBG:R_SUBG + 128] = np.asarray(inp["subln_g"])[0]
    rowc[0, R_LAM:R_LAM + 64] = np.asarray(inp["lambda_q1"])[0]
    rowc[0, R_LAM + 64:R_LAM + 128] = np.asarray(inp["lambda_q2"])[0]
    rowc[0, R_LAM + 128:R_LAM + 192] = np.asarray(inp["lambda_k1"])[0]
    rowc[0, R_LAM + 192:R_LAM + 256] = np.asarray(inp["lambda_k2"])[0]
    rowc[0, R_BR:R_BR + 4] = np.asarray(inp["b_router_group"])[0]
    rowc[0, R_BR + 4:R_BR + 36] = np.asarray(inp["b_router_expert"])[0].reshape(32)
    rowc[0, R_GF:R_GF + 1024] = np.asarray(inp["ffn_norm_g"])[0]
    rowc[0, R_IOTA:R_IOTA + 96] = np.arange(96)
    rowc[0, R_C8G:R_C8G + 4] = np.arange(4) * 8
    ident = np.eye(128, dtype=f)
    mtri = np.where(p[:, None] > p[None, :], f(NEG), f(0)).astype(f)
    wR = np.ascontiguousarray(np.concatenate([np.asarray(inp["w_router_group"])[0],
                                              np.asarray(inp["w_router_expert"])[0].reshape(D, 32)], axis=1))
    return {
        "wAV": wAV, "wF": wF, "wUQ": wUQ, "wUKV": wUKV, "wO": np.ascontiguousarray(np.asarray(inp["w_o"])[0]),
        "cst": cst, "rowc": rowc, "ident": ident, "mtri": mtri, "wR": wR,
        "wGt": np.ascontiguousarray(np.asarray(inp["w_gate"])[0].reshape(32, 8, 128, 256).transpose(0, 2, 1, 3).reshape(4096, 2048)),
        "wUt": np.ascontiguousarray(np.asarray(inp["w_up"])[0].reshape(32, 8, 128, 256).transpose(0, 2, 1, 3).reshape(4096, 2048)),
        "wDt": np.ascontiguousarray(np.asarray(inp["w_down"])[0].reshape(32, 2, 128, 1024).transpose(0, 2, 1, 3).reshape(4096, 2048)),
        "utri": np.triu(np.ones((128, 128), f)),
    }


_BOUND_REGS = {}


def IDMA(out, in_, out_off=None, in_off=None, bound=None):
    def f(e):
        key = (id(e), bound)
        if key not in _BOUND_REGS:
            _BOUND_REGS[key] = e.to_reg(bound)
        return e.indirect_dma_start(
            out=out, out_offset=(bass.IndirectOffsetOnAxis(ap=out_off, axis=0) if out_off is not None else None),
            in_=in_, in_offset=(bass.IndirectOffsetOnAxis(ap=in_off, axis=0) if in_off is not None else None),
            bounds_check=_BOUND_REGS[key], oob_is_err=False)
    return f


def TSS(out, in_, scalar, op):
    return lambda e: e.tensor_single_scalar(out=out, in_=in_, scalar=scalar, op=op)


def _fence(P):
    lasts = []
    for e in ENGS:
        comp = [o for o in P.ops[e] if (not o.is_dma) and o.fn is not None]
        if comp:
            lasts.append(comp[-1])
    dmas = [o for o in P.dma_last if o is not None]
    for e in ENGS:
        op = Op(e, None)
        op.idx = P.nops
        P.nops += 1
        for d in lasts + dmas:
            d.signal = True
            op.deps.append(d)
        P.ops[e].append(op)


Plan.fence = _fence


def build_nc(dbg=()):
    from contextlib import ExitStack
    _BOUND_REGS.clear()
    nc = bass.Bass("TRN2", target_bir_lowering=False)

    def din(name, shape, dt=F32):
        return nc.dram_tensor(name, list(shape), dt, kind="ExternalInput").ap()

    x_d = din("x", [S, D])
    pos_d = din("pos", [1, S], I32)
    wAV_d = din("wAV", [D, 1152])
    wF_d = din("wF", [D, 2304])
    wUQ_d = din("wUQ", [384, 1536])
    wUKV_d = din("wUKV", [256, 1024])
    wO_d = din("wO", [D, D])
    cst_d = din("cst", [128, NCST])
    rowc_d = din("rowc", [1, NROW])
    ident_d = din("ident", [128, 128])
    mtri_d = din("mtri", [128, 128])
    wR_d = din("wR", [D, 36])
    wGt_d = din("wGt", [4096, 2048])
    wUt_d = din("wUt", [4096, 2048])
    wDt_d = din("wDt", [4096, 2048])
    utri_d = din("utri", [128, 128])
    out_d = nc.dram_tensor("out", [S, D], F32, kind="ExternalOutput").ap()
    dbg_d = {}

    P = Plan()
    es = ExitStack()
    finals = []
    with es:
        A = Arena(nc, es, 207 * 1024)
        ps_all = es.enter_context(nc.psum_tensor("ps_all", [128, 8 * 512], F32))
        sems = {e: es.enter_context(nc.semaphore("s_" + e)) for e in ENGS}
        dsems = [es.enter_context(nc.semaphore("d%d" % i)) for i in range(P.n_dma_sems)]
        block = es.enter_context(nc.Block())

        def bank(b, n=1):
            return ps_all[:, b * 512:(b + n) * 512]

        def bank_bf(b):
            return ps_all[:, b * 512:(b + 1) * 512].bitcast(BF16)

        def dump(name, ap, shape, dt, reads=()):
            if name not in dbg:
                return
            d = nc.dram_tensor("dbg_" + name, list(shape), dt, kind="ExternalOutput").ap()
            dbg_d[name] = d
            finals.append(P.dma("sp", DMA(d, ap), reads=list(reads)))

        cst = A.alloc([NCST], F32)
        rowbc = A.alloc([NROW], F32)
        identf = A.alloc([128], F32)
        onesf = A.alloc([128], F32)
        identb = A.alloc([128], BF16)
        mtrib = A.alloc([128], BF16)
        epsb = A.alloc([1], F32)
        ssx = A.alloc([16], F32)
        sqx = A.alloc([16], F32)
        rstdx = A.alloc([16], F32)
        ssqkv = A.alloc([16, 2], F32)
        t_a = A.alloc([16, 2], F32)
        t_b = A.alloc([16, 2], F32)
        t_c = A.alloc([16, 2], F32)
        sqkv = A.alloc([16, 2], F32)
        lamt = A.alloc([128], F32)
        lam2 = A.alloc([2], F32)
        lame = A.alloc([2], F32)
        neglam = A.alloc([1], F32)
        dss = A.alloc([16, 4], F32)
        dsq = A.alloc([16, 4], F32)
        drd = A.alloc([16, 4], F32)
        ssf = A.alloc([16], F32)
        sqf = A.alloc([16], F32)
        rf = A.alloc([16], F32)
        junk = A.alloc([1024], BF16)
        Bcst, Bjunk = Buf("cst"), Buf("junk")

        P.dma("sp", DMA(cst, cst_d[:, :]), writes=[Bcst])
        P.dma("sp", DMA(rowbc, rowc_d.partition_broadcast(128)), writes=[Bcst])
        P.dma("sp", DMA(identf, ident_d[:, :]), writes=[Bcst])
        P.dma("pool", DMA(identb, ident_d[:, :]), writes=[Bcst])
        P.dma("pool", DMA(mtrib, mtri_d[:, :]), writes=[Bcst])
        Bst = Buf("stats")
        Bst_t = [Buf() for _ in range(16)]
        P.op("dve", MSET(epsb, EPS), writes=[Bst])
        P.op("dve", MSET(onesf, 1.0), writes=[Bst])
        for t_ in (ssx, ssqkv, dss, ssf):
            P.op("dve", MSET(t_, 0.0), writes=[Bst] + Bst_t)
        P.op("dve", MSET(epsb, EPS), writes=[Bst] + Bst_t)
        P.op("dve", TT(lamt, rowbc[:, R_LAM:R_LAM + 128], rowbc[:, R_LAM + 128:R_LAM + 256], ALU.mult),
             reads=[Bcst], writes=[Bst])
        P.op("dve", RSUM(lam2, lamt.rearrange("p (a b) -> p a b", a=2)), reads=[Bst], writes=[Bst])
        P.op("act", ACTV(lame, lam2, AF.Exp), reads=[Bst], writes=[Bst])
        P.op("dve", TT(neglam, lame[:, 1:2], lame[:, 0:1], ALU.subtract), reads=[Bst], writes=[Bst])
        P.op("dve", TS(neglam, neglam, -LAM_INIT, ALU.add), reads=[Bst], writes=[Bst])

        R1 = A.alloc([8, 2304], BF16)
        wF = R1
        mark_cnT = A.top
        cnT = A.alloc([5, S], BF16)
        kpeT = A.alloc([S], BF16)
        posf = A.alloc([S], F32)
        mark_VD = A.top
        VD = A.alloc([16, 4, 129], BF16)
        QTD = A.alloc([4, S], BF16)
        KTD = A.alloc([4, S], BF16)
        mark_wAV = A.top
        wAV = A.alloc([8, 1152], BF16)
        xs = [A.alloc([1024], F32)] * 2
        xb = [A.alloc([1024], BF16) for _ in range(2)]
        xT = [A.alloc([8, 512], BF16) for _ in range(2)]
        cn = [A.alloc([640], BF16) for _ in range(2)]
        CD = A.alloc([512], F32)
        SD = A.alloc([512], F32)
        CMr = A.alloc([512], F32)
        SMr = A.alloc([512], F32)
        u2 = A.alloc([2, 512], F32)
        nn = A.alloc([2, 512], F32)
        ni = nn.bitcast(I32)
        cs_set = [A.alloc([2, 512], F32) for _ in range(2)]
        t1 = [A.alloc([512], F32)] * 2
        t2 = [A.alloc([512], F32)] * 2
        diag = [A.alloc([128], F32) for _ in range(2)]
        _save = A.top
        A.top = mark_wAV
        KTz = A.alloc([8, S], BF16)
        mark_after_KTz = A.top
        A.top = _save
        BKTz = Buf("KTz")
        posi = ni.rearrange("p a b -> p (a b)")

        Bpos = Buf("pos")
        for hh in range(2):
            P.dma("sp", DMA(posi, pos_d[:, hh * 1024:(hh + 1) * 1024].partition_broadcast(128)), writes=[Bpos])
            P.op("dve", CP(posf[:, hh * 1024:(hh + 1) * 1024], posi), reads=[Bpos], writes=[Bpos])
        BVD = Buf("VD")
        P.op("pool", MSET(VD[:, :, :, 128:129], 1.0), writes=[BVD])

        BwAV = Buf("wAV")
        BwF = [Buf("wF%d" % i) for i in range(3)]
        P.dma("pool", DMA(wAV, wAV_d.rearrange("(k p) c -> p k c", p=128)), writes=[BwAV])
        for i in range(3):
            P.dma("pool", DMA(wF[:, :, i * 768:(i + 1) * 768],
                              wF_d[:, i * 768:(i + 1) * 768].rearrange("(k p) c -> p k c", p=128)), writes=[BwF[i]])

        gA3 = cst[:, C_GA:C_GA + 8].rearrange("p (a b) -> p a b", b=1)
        gQ3 = cst[:, C_GQKV:C_GQKV + 5].rearrange("p (a b) -> p a b", b=1)
        psT, psT2, psA0, psA1, psV, psB, psF0, psF1 = [bank(i) for i in range(8)]
        psT_b = bank_bf(0)
        psT2_b = bank_bf(1)
        BpsT, BpsT2, BpsA0, BpsA1, BpsV, BpsB, BpsF0, BpsF1 = [Buf("ps%d" % i) for i in range(8)]
        Bxs = [Buf()] * 2
        Bxb = [Buf(), Buf()]
        BxT = [[Buf() for _ in range(4)] for _ in range(2)]
        Bcn = [Buf(), Buf()]
        Btab = Buf("tab")
        Btmp = Buf("tabtmp")
        Bt12 = [Buf()] * 2
        Bdiag = [Buf(), Buf()]
        BcnT = Buf("cnT")
        Bqk = Buf("qkT")
        def a_load(tt):
            sl = slice(tt * 128, (tt + 1) * 128)
            i2 = tt % 2
            P.dma("sp", DMA(xs[i2], x_d[sl, :]), writes=[Bxs[i2]])
            P.dma("pool", DMA(xb[i2], x_d[sl, :]), writes=[Bxb[i2]])

        def a_step1(tc, r):
            tt = 4 * tc + r
            sl = slice(tt * 128, (tt + 1) * 128)
            rs = slice(r * 128, (r + 1) * 128)
            i2 = tt % 2
            buf = tc % 2
            rx = rstdx[:, tt:tt + 1]
            P.op("act", ACTV(junk, xs[i2], AF.Square, accum_out=ssx[:, tt:tt + 1]),
                 reads=[Bxs[i2], Bst_t[tt]], writes=[Bst_t[tt]])
            if tt + 1 < 16:
                a_load(tt + 1)
            P.op("act", ACTV(sqx[:, tt:tt + 1], ssx[:, tt:tt + 1], AF.Sqrt, bias=epsb, scale=1.0 / D),
                 reads=[Bst_t[tt]], writes=[Bst_t[tt]])
            P.op("dve", RECIP(rstdx[:, tt:tt + 1], sqx[:, tt:tt + 1]), reads=[Bst_t[tt]], writes=[Bst_t[tt]])
            for k in range(8):
                P.op("pe", TR(psT_b[:, k * 128:(k + 1) * 128], xb[i2][:, k * 128:(k + 1) * 128], identb),
                     reads=[Bxb[i2], Bcst], writes=[BpsT])
            P.op("dve", TT(xT[buf][:, :, rs], psT_b.rearrange("p (k t) -> p k t", k=8), bc(gA3, [128, 8, 128]),
                           ALU.mult), reads=[BpsT, Bcst], writes=[BxT[buf][r]])

        def a_step2(tc, r):
            tt = 4 * tc + r
            sl = slice(tt * 128, (tt + 1) * 128)
            rs = slice(r * 128, (r + 1) * 128)
            i2 = tt % 2
            buf = tc % 2
            rx = rstdx[:, tt:tt + 1]
            for (c0, c1, pb, Bp) in ((0, 384, psA0, BpsA0), (384, 640, psA1, BpsA1), (640, 1152, psV, BpsV)):
                for k in range(8):
                    P.op("pe", MM(pb[:, 0:c1 - c0], xT[buf][:, k, rs], wAV[:, k, c0:c1], k == 0, k == 7),
                         reads=[BxT[buf][r], BwAV], writes=[Bp])
            P.op("act", ACTV(junk[:, 0:384], psA0[:, 0:384], AF.Square, accum_out=ssqkv[:, tt, 0:1]),
                 reads=[BpsA0, Bst_t[tt]], writes=[Bst_t[tt]])
            P.op("act", ACTV(junk[:, 0:256], psA1[:, 0:256], AF.Square, accum_out=ssqkv[:, tt, 1:2]),
                 reads=[BpsA1, Bst_t[tt]], writes=[Bst_t[tt]])
            P.op("dve", STT(t_a[:, tt, :], ssqkv[:, tt, :], rx, cst[:, C_DIVQ:C_DIVQ + 2], ALU.mult, ALU.mult),
                 reads=[Bst_t[tt], Bcst], writes=[Bst_t[tt]])
            P.op("dve", TS(t_a[:, tt, :], t_a[:, tt, :], rx, ALU.mult), reads=[Bst_t[tt]], writes=[Bst_t[tt]])
            P.op("act", ACTV(t_b[:, tt, :], t_a[:, tt, :], AF.Sqrt, bias=epsb, scale=1.0), reads=[Bst_t[tt]], writes=[Bst_t[tt]])
            P.op("dve", RECIP(t_c[:, tt, :], t_b[:, tt, :]), reads=[Bst_t[tt]], writes=[Bst_t[tt]])
            P.op("dve", TS(sqkv[:, tt, :], t_c[:, tt, :], rx, ALU.mult), reads=[Bst_t[tt]], writes=[Bst_t[tt]])
            P.op("dve", TS(cn[i2][:, 0:384], psA0[:, 0:384], sqkv[:, tt, 0:1], ALU.mult),
                 reads=[BpsA0, Bst_t[tt]], writes=[Bcn[i2]])
            P.op("dve", TS(cn[i2][:, 384:640], psA1[:, 0:256], sqkv[:, tt, 1:2], ALU.mult),
                 reads=[BpsA1, Bst_t[tt]], writes=[Bcn[i2]])

        def a_step3(tc, r):
            tt = 4 * tc + r
            sl = slice(tt * 128, (tt + 1) * 128)
            rs = slice(r * 128, (r + 1) * 128)
            i2 = tt % 2
            buf = tc % 2
            rx = rstdx[:, tt:tt + 1]
            for j in range(5):
                P.op("pe", TR(psT2_b[:, j * 128:(j + 1) * 128], cn[i2][:, j * 128:(j + 1) * 128], identb),
                     reads=[Bcn[i2], Bcst], writes=[BpsT2])
            P.op("dve", TT(cnT[:, :, sl], psT2_b[:, 0:640].rearrange("p (k t) -> p k t", k=5),
                           bc(gQ3, [128, 5, 128]), ALU.mult), reads=[BpsT2, Bcst], writes=[BcnT])
            P.op("act", ACTV(VD[:, tt, :, 0:128], psV.rearrange("p (h d) -> p h d", h=4), AF.Copy, scale=rx),
                 reads=[BpsV, Bst_t[tt]], writes=[BVD])
            P.op("dve", TS(diag[i2], identf, rx, ALU.mult), reads=[Bst_t[tt], Bcst], writes=[Bdiag[i2]])
            P.op("pe", MM(psB[:, rs], onesf, diag[i2], True, True), reads=[Bdiag[i2], Bst_t[tt]], writes=[BpsB])

        MAGIC = 12582912.0
        Bcs = [Buf(), Buf()]

        def a_tabprep(tc, si):
            chunk = slice(tc * 512, (tc + 1) * 512)
            invc = (C_INVD, C_INVM)[si]
            iv = cst[:, invc:invc + 1]
            cs_ = cs_set[si]
            P.op("dve", TS(u2[:, 0, :], posf[:, chunk], iv, ALU.mult), reads=[Bpos, Bcst], writes=[Btmp])
            P.op("dve", TS(u2[:, 1, :], posf[:, chunk], iv, ALU.mult, 0.25, ALU.add), reads=[Bpos, Bcst], writes=[Btmp])
            P.op("dve", TS(nn, u2, MAGIC, ALU.add, MAGIC, ALU.subtract), reads=[Btmp], writes=[Btmp])
            P.op("dve", TT(cs_, u2, nn, ALU.subtract), reads=[Btmp], writes=[Bcs[si]])
            P.op("act", ACTV(cs_, cs_, AF.Sin, scale=float(2 * np.pi)), reads=[Bcs[si]], writes=[Bcs[si]])

        def a_tables(tc):
            for si, (sgnc, Cout, Sout) in enumerate(((C_SGND, CD, SD), (C_SGNM, CMr, SMr))):
                cs_ = cs_set[si]
                P.op("dve", STT(Sout, cs_[:, 0, :], cst[:, sgnc:sgnc + 1], psB, ALU.mult, ALU.mult),
                     reads=[Bcs[si], BpsB, Bcst], writes=[Btab])
                P.op("dve", TT(Cout, cs_[:, 1, :], psB, ALU.mult), reads=[Bcs[si], BpsB], writes=[Btab])

        def a_feat(tc, i):
            buf = tc % 2
            chunk = slice(tc * 512, (tc + 1) * 512)
            for hf, (pb, Bp) in enumerate(((psF0, BpsF0), (psF1, BpsF1))):
                blk = 2 * i + hf
                for k in range(8):
                    P.op("pe", MM(pb, wF[:, k, blk * 128:(blk + 1) * 128], xT[buf][:, k, :], k == 0, k == 7),
                         reads=BxT[buf] + [BwF[blk // 6]], writes=[Bp])
            if i < 8:
                rows = slice(0, 128)
                Ct, St = CD, SD
                dest = (QTD if i < 4 else KTD)[:, i % 4, chunk]
            else:
                rows = slice(64, 96)
                Ct, St = CMr, SMr
                dest = kpeT[64:96, chunk]
            j2 = i % 2
            P.op("dve", TT(t1[j2][rows], psF0[rows], Ct[rows], ALU.mult), reads=[BpsF0, Btab], writes=[Bt12[j2]])
            P.op("dve", TT(t2[j2][rows], psF1[rows], St[rows], ALU.mult), reads=[BpsF1, Btab], writes=[Bt12[j2]])
            P.op("pool", TT(dest, t1[j2][rows], t2[j2][rows], ALU.add), reads=[Bt12[j2]], writes=[Bqk])


        a_load(0)
        for tc in range(4):
            for r in range(4):
                a_step1(tc, r)
                if tc > 0:
                    a_feat(tc - 1, 2 * r)
                a_step2(tc, r)
                if r < 2:
                    a_tabprep(tc, r)
                if tc > 0:
                    a_feat(tc - 1, 2 * r + 1)
                a_step3(tc, r)
            if tc > 0:
                a_feat(tc - 1, 8)
            if tc == 3:
                for h in range(4):
                    for half in range(2):
                        zrows = slice(64 * (1 - half), 64 * (1 - half) + 64)
                        P.op("dve", MSET(KTz[zrows, 2 * h + half, :], 0.0),
                             writes=[BKTz, BwAV, Bxs[0], Bxb[0], Bxb[1]] + BxT[0])
            a_tables(tc)
        for i in range(9):
            a_feat(3, i)
            if 4 <= i < 8:
                h = i - 4
                for half in range(2):
                    rows = slice(64 * half, 64 * half + 64)
                    P.op("act", ACTV(KTz[rows, 2 * h + half, :], KTD[rows, h, :], AF.Copy), reads=[Bqk], writes=[BKTz])

        dump("cnT", cnT, [128, 5, S], BF16, [BcnT])
        dump("QTD", QTD, [128, 4, S], BF16, [Bqk])
        dump("KTD", KTD, [128, 4, S], BF16, [Bqk])
        dump("kpeT", kpeT, [128, S], BF16, [Bqk])
        dump("VD", VD, [128, 16, 4, 129], BF16, [BVD])
        dump("rstdx", rstdx, [128, 16], F32, [Bst])
        P.fence()
        if "stopA" in dbg:
            P.emit(nc, block, sems, dsems, finals)
            return nc, dbg_d
        A.top = mark_after_KTz
        PT = [A.alloc([1024], BF16) for _ in range(3)]
        o_all = R1.rearrange("p a b -> p (a b)")[:, 0:16 * 1024].rearrange("p (t c) -> p t c", t=16)
        A1 = A.alloc([4, 128], F32)
        A2 = A.alloc([4, 128], F32)
        Dd = A.alloc([4, 128], F32)
        Dsq = A.alloc([4, 128], F32)
        rec4 = [A.alloc([4], F32) for _ in range(2)]
        BS = [Buf("S0"), Buf("S1"), Buf("S2")]
        BPT = [Buf(), Buf(), Buf()]
        Bacc = [Buf("acc0"), Buf("acc1")]
        Bo = Buf("o_all")
        BA1, BA2, BDd = Buf(), Buf(), Buf()
        Brec = [Buf(), Buf()]

        def run_attention(units, nS=2):
            groups = []
            for ui, u in enumerate(units):
                c = u["c"]
                gl = []
                for pr in range(2 * c):
                    gl.append(dict(ncols=1024, ents=[(2 * pr, 0, [(r, r * 128) for r in range(4)]),
                                                     (2 * pr + 1, 512, [(r, r * 128) for r in range(4)])]))
                gl.append(dict(ncols=1024, ents=[(4 * c, 0, [(r, r * 128) for r in range(4)]),
                                                 (4 * c + 1, 512, [(r, r * 128) for r in range(1, 4)])]))
                gl.append(dict(ncols=384, ents=[(4 * c + 2, 0, [(2, 0), (3, 128)]),
                                                (4 * c + 3, 0, [(3, 256)])]))
                for gi, g in enumerate(gl):
                    g["u"] = u
                    g["ui"] = ui
                    g["last"] = gi == len(gl) - 1
                    g["second_last"] = gi == len(gl) - 2
                    groups.append(g)

            def emit_qk(gidx):
                g = groups[gidx]
                u = g["u"]
                c = u["c"]
                Sg = bank(2 * (gidx % nS), 2)
                for (kb, base, rl) in g["ents"]:
                    r0 = rl[0][0]
                    c0 = base + rl[0][1]
                    n = len(rl)
                    isdiag = kb >= 4 * c
                    P.op("pe", MM(Sg[:, c0:c0 + n * 128], u["KT"][:, kb * 128:(kb + 1) * 128],
                                  u["QT"][:, (4 * c + r0) * 128:(4 * c + 4) * 128], True, not isdiag, skip=True),
                         writes=[BS[gidx % nS]])
                    if isdiag:
                        P.op("pe", MM(Sg[:, c0:c0 + 128], identb, mtrib, False, True, skip=True),
                             writes=[BS[gidx % nS]])

            def emit_exp_pv(gidx):
                g = groups[gidx]
                u = g["u"]
                c = u["c"]
                W = u["W"]
                Sg = bank(2 * (gidx % nS), 2)
                pt = PT[gidx % 3]
                P.op("act", ACTV(pt[:, 0:g["ncols"]], Sg[:, 0:g["ncols"]], AF.Exp, scale=u["scale"]),
                     reads=[BS[gidx % nS]], writes=[BPT[gidx % 3]])
                ci = g["ui"] % 2
                for (kb, base, rl) in g["ents"]:
                    for (r, col) in rl:
                        tok = u["acctok"](ci, r) if "acctok" in u else Bacc[ci]
                        P.op("pe", MM(u["acc"](ci, r), pt[:, base + col:base + col + 128], u["V"](kb),
                                      kb == 0 and (r % u["rper"]) == 0, kb == 4 * c + r, skip=True),
                             reads=[BPT[gidx % 3]], writes=[tok])
                if "post_half" in u:
                    if g["second_last"]:
                        u["post_half"](u, 0)
                    if g["last"]:
                        u["post_half"](u, 1)
                elif g["last"]:
                    u["post"](ci, u)

            G = len(groups)
            for g0 in range(min(nS - 1, G)):
                emit_qk(g0)
            for gidx in range(G):
                if gidx + nS - 1 < G:
                    emit_qk(gidx + nS - 1)
                emit_exp_pv(gidx)

        BaccD = [Buf("accD0"), Buf("accD1")]
        BrecD = [[Buf(), Buf()], [Buf(), Buf()]]
        BA1D, BA2D, BDdD, BDsqD = [[Buf(), Buf()] for _ in range(4)]

        def acc_diff(ci, r):
            return bank(6 + r // 2)[:, (r % 2) * 129:(r % 2) * 129 + 129]

        def acctok_diff(ci, r):
            return BaccD[r // 2]

        def post_diff_half(u, hb):
            h, half, c = u["h"], u["half"], u["c"]
            q0 = 2 * hb
            accv = bank(6 + hb)[:, 0:258].rearrange("p (s w) -> p s w", w=129)
            rc = rec4[half][:, q0:q0 + 2].rearrange("p (s o) -> p s o", o=1)
            P.op("dve", RECIP(rc, accv[:, :, 128:129]), reads=[BaccD[hb]], writes=[BrecD[half][hb]])
            dst = (A1 if half == 0 else A2)[:, q0:q0 + 2, :]
            P.op("dve", TT(dst, accv[:, :, 0:128], bc(rc, [128, 2, 128]), ALU.mult),
                 reads=[BaccD[hb], BrecD[half][hb]], writes=[BA1D[hb] if half == 0 else BA2D[hb]])
            if half == 1:
                Dd_, Dsq_ = Dd[:, q0:q0 + 2, :], Dsq[:, q0:q0 + 2, :]
                P.op("dve", STT(Dd_, A2[:, q0:q0 + 2, :], neglam[:, 0:1], A1[:, q0:q0 + 2, :], ALU.mult, ALU.add),
                     reads=[BA1D[hb], BA2D[hb], Bst], writes=[BDdD[hb]])
                P.op("pool", TT(Dsq_, Dd_, Dd_, ALU.mult), reads=[BDdD[hb]], writes=[BDsqD[hb]])
                P.op("dve", RSUM(dss[:, 4 * c + q0:4 * c + q0 + 2, h], Dsq_), reads=[BDsqD[hb]], writes=[Bst])
                P.op("pool", CP(o_all[:, 4 * c + q0:4 * c + q0 + 2, 512 + h * 128:512 + (h + 1) * 128], Dd_),
                     reads=[BDdD[hb]], writes=[Bo])

        units = []
        for h in range(4):
            for c in range(4):
                for half in range(2):
                    units.append(dict(KT=KTz[:, 2 * h + half, :], QT=QTD[:, h, :], W=129, rper=2, scale=64 ** -0.5, c=c, h=h,
                                      half=half, V=(lambda kb, h=h: VD[:, kb, h, :]), acc=acc_diff, acctok=acctok_diff,
                                      post_half=post_diff_half))
        run_attention(units, nS=3)
        dump("dss", dss, [128, 16, 4], F32, [Bst])
        dump("o_all", o_all, [128, 16, 1024], BF16, [Bo])
        P.fence()
        if "stopB" in dbg:
            P.emit(nc, block, sems, dsems, finals)
            return nc, dbg_d
        A.top = mark_VD
        QTM = A.alloc([8, S], BF16)
        KTM = A.alloc([8, S], BF16)
        VM = A.alloc([16, 8, 65], BF16)
        wUQ = A.alloc([3, 1536], BF16)
        wUKV = A.alloc([2, 1024], BF16)
        CM = A.alloc([512], F32)
        SM = A.alloc([512], F32)
        u2 = A.alloc([2, 512], F32)
        nf = A.alloc([2, 512], F32)
        cs = A.alloc([2, 512], F32)
        t1c = [A.alloc([512], F32)] * 2
        t2c = [A.alloc([512], F32)] * 2
        PT = [A.alloc([1024], BF16) for _ in range(3)]
        recm = [A.alloc([4], F32) for _ in range(2)]
        BwUQ, BwUKV, BVM, BQTM, BKTM, BQTMn, BKTMn = Buf(), Buf(), Buf(), Buf(), Buf(), Buf(), Buf()
        Btab, Btmp, Bt12c = Buf(), Buf(), [Buf()] * 2
        BpsC3 = [(Buf(), Buf(), Buf()), (Buf(), Buf(), Buf())]
        P.dma("pool", DMA(wUQ, wUQ_d.rearrange("(k p) c -> p k c", p=128)), writes=[BwUQ])
        P.dma("pool", DMA(wUKV, wUKV_d.rearrange("(k p) c -> p k c", p=128)), writes=[BwUKV])
        P.op("pool", MSET(VM[:, :, :, 64:65], 1.0), writes=[BVM])
        for h in range(8):
            P.op("pool", CP(KTM[64:96, h, :], kpeT[64:96, :]), writes=[BKTM])
        psVm = bank(3)
        BpsVm = Buf()
        for tt in range(16):
            sl = slice(tt * 128, (tt + 1) * 128)
            for k in range(2):
                P.op("pe", MM(psVm, cnT[:, 3 + k, sl], wUKV[:, k, 512:1024], k == 0, k == 1),
                     reads=[BwUKV], writes=[BpsVm])
            P.op("act", ACTV(VM[:, tt, :, 0:64], psVm.rearrange("p (h d) -> p h d", h=8), AF.Copy),
                 reads=[BpsVm], writes=[BVM])
        for tc in range(4):
            chunk = slice(tc * 512, (tc + 1) * 512)
            iv = cst[:, C_INVM:C_INVM + 1]
            P.op("dve", TS(u2[:, 0, :], posf[:, chunk], iv, ALU.mult), writes=[Btmp, Bt12c[1]])
            P.op("dve", TS(u2[:, 1, :], posf[:, chunk], iv, ALU.mult, 0.25, ALU.add), writes=[Btmp, Bt12c[1]])
            P.op("dve", TS(nf, u2, MAGIC, ALU.add, MAGIC, ALU.subtract), reads=[Btmp], writes=[Btmp])
            P.op("dve", TT(cs, u2, nf, ALU.subtract), reads=[Btmp], writes=[Btmp])
            P.op("act", ACTV(cs, cs, AF.Sin, scale=float(2 * np.pi)), reads=[Btmp], writes=[Btmp])
            P.op("dve", TS(SM, cs[:, 0, :], cst[:, C_SGNM:C_SGNM + 1], ALU.mult), reads=[Btmp], writes=[Btab])
            P.op("dve", CP(CM, cs[:, 1, :]), reads=[Btmp], writes=[Btab])
            for h in range(8):
                par = h % 2
                psQ0, psQ1, psK = bank(4 * par), bank(4 * par + 1), bank(4 * par + 2)
                BpsQ0, BpsQ1, BpsK = BpsC3[par]
                for hf, (pb, Bp) in enumerate(((psQ0, BpsQ0), (psQ1, BpsQ1))):
                    for k in range(3):
                        P.op("pe", MM(pb[0:96, :], wUQ[:, k, hf * 768 + h * 96:hf * 768 + (h + 1) * 96], cnT[:, k, chunk],
                                      k == 0, k == 2), reads=[BwUQ], writes=[Bp])
                for k in range(2):
                    P.op("pe", MM(psK[0:64, :], wUKV[:, k, h * 64:(h + 1) * 64], cnT[:, 3 + k, chunk], k == 0, k == 1),
                         reads=[BwUKV], writes=[BpsK])
                P.op("act", ACTV(QTM[0:64, h, chunk], psQ0[0:64, :], AF.Copy), reads=[BpsQ0], writes=[BQTMn])
                P.op("dve", TT(t1c[par][64:96], psQ0[64:96, :], CM[64:96], ALU.mult), reads=[BpsQ0, Btab], writes=[Bt12c[par]])
                P.op("dve", TT(t2c[par][64:96], psQ1[64:96, :], SM[64:96], ALU.mult), reads=[BpsQ1, Btab], writes=[Bt12c[par]])
                P.op("dve", TT(QTM[64:96, h, chunk], t1c[par][64:96], t2c[par][64:96], ALU.add), reads=[Bt12c[par]], writes=[BQTM])
                P.op("act", ACTV(KTM[0:64, h, chunk], psK[0:64, :], AF.Copy), reads=[BpsK], writes=[BKTMn])
        dump("QTM", QTM, [128, 8, S], BF16, [BQTM, BQTMn])
        dump("KTM", KTM, [128, 8, S], BF16, [BKTM, BKTMn])
        dump("VM", VM, [128, 16, 8, 65], BF16, [BVM])
        P.fence()

        def acc_mla(ci, r):
            return bank(6 + ci)[:, r * 65:(r + 1) * 65]

        def post_mla(ci, u):
            h, c = u["h"], u["c"]
            accv = bank(6 + ci)[:, 0:260].rearrange("p (r w) -> p r w", w=65)
            rc = recm[ci].rearrange("p (r o) -> p r o", o=1)
            P.op("dve", RECIP(rc, accv[:, :, 64:65]), reads=[Bacc[ci]], writes=[Brec[ci]])
            P.op("dve", TT(o_all[:, 4 * c:4 * c + 4, h * 64:(h + 1) * 64], accv[:, :, 0:64], bc(rc, [128, 4, 64]),
                           ALU.mult), reads=[Bacc[ci], Brec[ci]], writes=[Bo])

        units = []
        for h in range(8):
            for c in range(4):
                units.append(dict(KT=KTM[0:96, h, :], QT=QTM[0:96, h, :], W=65, rper=4, scale=96 ** -0.5, c=c, h=h,
                                  V=(lambda kb, h=h: VM[:, kb, h, :]), acc=acc_mla, post=post_mla))
        run_attention(units, nS=3)
        P.fence()

        A.top = mark_cnT
        hres = A.alloc([16, 1024], F32)
        ob = [A.alloc([1024], F32) for _ in range(2)]
        mark_after_hres = A.top
        wO = A.alloc([8, 1024], BF16)
        mixT = [A.alloc([8, 128], BF16) for _ in range(2)]
        xs2 = [A.alloc([1024], F32) for _ in range(2)]
        BwO, Bmix, Bxs2, Bob, Bh = Buf(), [Buf(), Buf()], [Buf(), Buf()], [Buf(), Buf()], [Buf() for _ in range(16)]
        P.dma("pool", DMA(wO, wO_d.rearrange("(k p) c -> p k c", p=128)), writes=[BwO])
        P.op("act", ACTV(dsq, dss, AF.Sqrt, bias=epsb, scale=1.0 / 128), reads=[Bst], writes=[Bst])
        P.op("dve", RECIP(drd, dsq), reads=[Bst], writes=[Bst])
        P.op("dve", TS(drd, drd, 1.0 - LAM_INIT, ALU.mult), reads=[Bst], writes=[Bst])
        subg = rowbc[:, R_SUBG:R_SUBG + 128].rearrange("p (a b) -> p a b", a=1)
        fing = rowbc[:, R_FING:R_FING + 1024]
        BpsTe, BpsO = Buf(), [Buf(), Buf()]
        psTe = bank_bf(0)
        Bo_t = [Buf() for _ in range(16)]
        for tt in range(16):
            od = o_all[:, tt, 512:1024].rearrange("p (h d) -> p h d", h=4)
            P.op("dve", TT(od, od, bc(drd[:, tt, :].rearrange("p (h o) -> p h o", o=1), [128, 4, 128]), ALU.mult),
                 reads=[Bo, Bst], writes=[Bo_t[tt]])
            P.op("dve", TT(od, od, bc(subg, [128, 4, 128]), ALU.mult), reads=[Bo_t[tt], Bcst], writes=[Bo_t[tt]])

        def e_transposes(tt):
            i2 = tt % 2
            for c8 in range(8):
                P.op("pe", TR(psTe[:, c8 * 128:(c8 + 1) * 128], o_all[:, tt, c8 * 128:(c8 + 1) * 128], identb),
                     reads=[Bo, Bo_t[tt]], writes=[BpsTe])
            P.op("act", ACTV(mixT[i2], psTe.rearrange("p (c t) -> p c t", c=8), AF.Copy), reads=[BpsTe], writes=[Bmix[i2]])

        def e_matmuls(tt):
            i2 = tt % 2
            sl = slice(tt * 128, (tt + 1) * 128)
            psO = bank(2 + 2 * i2, 2)
            for hf in range(2):
                for c8 in range(8):
                    P.op("pe", MM(psO[:, hf * 512:(hf + 1) * 512], mixT[i2][:, c8, :], wO[:, c8, hf * 512:(hf + 1) * 512],
                                  c8 == 0, c8 == 7), reads=[Bmix[i2], BwO], writes=[BpsO[i2]])
            P.dma("sp", DMA(xs2[i2], x_d[sl, :]), writes=[Bxs2[i2]])
            P.op("dve", TT(hres[:, tt, :], psO, xs2[i2], ALU.add), reads=[BpsO[i2], Bxs2[i2]], writes=[Bh[tt]])

        e_transposes(0)
        for tt in range(16):
            if tt + 1 < 16:
                e_transposes(tt + 1)
            e_matmuls(tt)
        dump("hres", hres, [128, 16, 1024], F32, Bh)
        FINAL_DONE = [False]

        def final_tile(tt):
            i2 = tt % 2
            sl = slice(tt * 128, (tt + 1) * 128)
            P.op("act", ACTV(junk, hres[:, tt, :], AF.Square, accum_out=ssf[:, tt:tt + 1]), reads=[Bh[tt], Bst],
                 writes=[Bst])
            P.op("act", ACTV(sqf[:, tt:tt + 1], ssf[:, tt:tt + 1], AF.Sqrt, bias=epsb, scale=1.0 / D), reads=[Bst], writes=[Bst])
            P.op("dve", RECIP(rf[:, tt:tt + 1], sqf[:, tt:tt + 1]), reads=[Bst], writes=[Bst])
            P.op("dve", STT(ob[i2], hres[:, tt, :], rf[:, tt:tt + 1], fing, ALU.mult, ALU.mult),
                 reads=[Bh[tt], Bst, Bcst], writes=[Bob[i2]])
            finals.append(P.dma("sp", DMA(out_d[sl, :], ob[i2]), reads=[Bob[i2]]))

        if "noF" not in dbg and "dense" not in dbg:
            P.fence()
            A.top = mark_after_hres
            BIG = float(1 << 16)
            NTL = 48
            NSL = NTL * 256
            hn_d = nc.dram_tensor("hn_scr", [S, D], BF16).ap()
            tos_d = nc.dram_tensor("tos_scr", [NSL, 16], I32).ap()
            Y_d = nc.dram_tensor("y_scr", [NSL, D], BF16).ap()
            BhnD, BtosD, BYd = Buf(), Buf(), Buf()
            R1f = R1.rearrange("p a b -> p (a b)")
            Wg = [R1f[:, i * 6144: i * 6144 + 2048].rearrange("p (k f) -> p k f", k=8) for i in range(3)]
            Wu = [R1f[:, i * 6144 + 2048: i * 6144 + 4096].rearrange("p (k f) -> p k f", k=8) for i in range(3)]
            Wd = [R1f[:, i * 6144 + 4096: i * 6144 + 6144].rearrange("p (c d) -> p c d", c=2) for i in range(3)]
            wR32 = A.alloc([8, 36], F32)
            Whi = A.alloc([8, 36], BF16)
            Wlo = A.alloc([8, 36], BF16)
            wRt = A.alloc([8, 36], F32)
            ssh = A.alloc([16], F32)
            sqh = A.alloc([16], F32)
            rh = A.alloc([16], F32)
            lg = A.alloc([16, 36], F32)
            utri = A.alloc([128], BF16)
            onesb = A.alloc([128], BF16)

            def f16(n):
                return A.alloc([16, n], F32)
            gmax, sume, pg, v0, v1, dlt, exd, w1, w2, gi8, i1, i2_, eid1, eid2, rank1, rank2 = [A.alloc([16], F32) for _ in range(16)]
            ohg, gsh = f16(4), f16(4)
            selg = A.alloc([64, 8], F32)
            sel, m1, m2, sel2, tm8 = f16(8), f16(8), f16(8), f16(8), f16(8)
            oh1, oh2, OH, incl, t32 = f16(32), f16(32), f16(32), f16(32), f16(32)
            OHb = A.alloc([16, 32], BF16)
            g12 = A.alloc([16, 2], F32)
            posf2 = A.alloc([16, 2], F32)
            posi2 = A.alloc([16, 2], I32)
            tokf = A.alloc([16], F32)
            tokrow = A.alloc([16, 16], I32)
            ne, nt, csum, csum2, excl, sbase = [A.alloc([32], F32) for _ in range(6)]
            eot, used, widxf = [A.alloc([NTL], F32) for _ in range(3)]
            widx = A.alloc([NTL], I32)
            tosT = A.alloc([2 * NTL, 16], I32)
            tosf, yvalid, yidxf = [A.alloc([2 * NTL], F32) for _ in range(3)]
            yidx = A.alloc([2 * NTL], I32)
            m_ = A.top
            hn32 = [A.alloc([1024], F32) for _ in range(2)]
            hnb = [A.alloc([1024], BF16) for _ in range(2)]
            hnl = [A.alloc([1024], BF16) for _ in range(2)]
            hiT = [A.alloc([8, 128], BF16) for _ in range(2)]
            loT = [A.alloc([8, 128], BF16) for _ in range(2)]
            Bje = A.alloc([NTL, 32], F32)
            Aje = A.alloc([NTL, 32], F32)
            bigt = A.alloc([NSL * 16 // 128], I32)
            top_r = A.top
            A.top = m_
            Xg = [[A.alloc([1024], BF16) for _ in range(2)] for _ in range(3)]
            sa = A.alloc([512], F32)
            hdnT = A.alloc([2, 256], BF16)
            xgT = [A.alloc([8, 256], BF16) for _ in range(2)]
            ysb = [A.alloc([1024], BF16) for _ in range(2)]
            yk = [A.alloc([1024], BF16) for _ in range(4)]
            A.top = max(A.top, top_r)
            iota = rowbc[:, R_IOTA:R_IOTA + 96]
            c8g = rowbc[:, R_C8G:R_C8G + 4]
            pcol = cst[:, C_PCOL:C_PCOL + 1]
            pmB = cst[:, C_PMB:C_PMB + 1]
            BwR, Bhn32, Bhnb, Brt, Bcnt = Buf(), [Buf(), Buf()], [Buf(), Buf()], Buf(), Buf()
            Bhnl, BhiT, BloT, BpsH, BpsLo = [[Buf(), Buf()] for _ in range(5)]
            BwR0 = Buf()
            P.dma("sp", DMA(wR32, wR_d.rearrange("(k p) c -> p k c", p=128)), writes=[BwR0])
            P.op("dve", CP(Whi, wR32), reads=[BwR0], writes=[BwR])
            P.op("dve", TT(wRt, wR32, Whi, ALU.subtract), reads=[BwR0, BwR], writes=[BwR])
            P.op("dve", CP(Wlo, wRt), reads=[BwR], writes=[BwR])
            P.dma("pool", DMA(utri, utri_d[:, :]), writes=[Bcnt])
            P.op("dve", MSET(ssh, 0.0), writes=[Brt])
            P.op("pool", MSET(onesb, 1.0), writes=[Bcnt])
            P.op("pool", MSET(bigt, 1 << 16), writes=[Bcnt])
            P.dma("sp", DMA(tos_d.rearrange("(p r) w -> p (r w)", p=128), bigt), reads=[Bcnt], writes=[BtosD])
            psX = bank(6, 2)
            psL = bank(1)
            BpsX, BpsL = Buf(), Buf()
            gFbc = rowbc[:, R_GF:R_GF + 1024]
            for tt in range(16):
                P.op("act", ACTV(junk, hres[:, tt, :], AF.Square, accum_out=ssh[:, tt:tt + 1]), reads=[Bh[tt], Brt],
                     writes=[Brt])
            P.op("act", ACTV(sqh, ssh, AF.Sqrt, bias=epsb, scale=1.0 / D), reads=[Brt], writes=[Brt])
            P.op("dve", RECIP(rh, sqh), reads=[Brt], writes=[Brt])
            Blg = Buf()
            BpsXs, BpsLs = [Buf(), Buf()], [Buf(), Buf()]
            def rt_front(tt):
                i2 = tt % 2
                sl = slice(tt * 128, (tt + 1) * 128)
                P.op("dve", STT(hn32[i2], hres[:, tt, :], rh[:, tt:tt + 1], gFbc, ALU.mult, ALU.mult),
                     reads=[Bh[tt], Brt, Bcst], writes=[Bhn32[i2]])
                P.op("act", ACTV(hnb[i2], hn32[i2], AF.Copy), reads=[Bhn32[i2]], writes=[Bhnb[i2]])
                P.dma("sp", DMA(hn_d[sl, :], hnb[i2]), reads=[Bhnb[i2]], writes=[BhnD])
                P.op("dve", TT(hnl[i2], hn32[i2], hnb[i2], ALU.subtract), reads=[Bhn32[i2], Bhnb[i2]], writes=[Bhnl[i2]])
                pH, pL = bank_bf(4 + 2 * i2), bank_bf(5 + 2 * i2)
                for k in range(8):
                    P.op("pe", TR(pH[:, k * 128:(k + 1) * 128], hnb[i2][:, k * 128:(k + 1) * 128], identb),
                         reads=[Bhnb[i2]], writes=[BpsH[i2]])
                for k in range(8):
                    P.op("pe", TR(pL[:, k * 128:(k + 1) * 128], hnl[i2][:, k * 128:(k + 1) * 128], identb),
                         reads=[Bhnl[i2]], writes=[BpsLo[i2]])
                P.op("act", ACTV(hiT[i2], pH.rearrange("p (k t) -> p k t", k=8), AF.Copy), reads=[BpsH[i2]], writes=[BhiT[i2]])
                P.op("act", ACTV(loT[i2], pL.rearrange("p (k t) -> p k t", k=8), AF.Copy), reads=[BpsLo[i2]], writes=[BloT[i2]])

            def rt_back(tt):
                i2 = tt % 2
                pl = bank(1 + i2)[:, 0:36]
                n_ = 0
                for (xT_, W_, Bx) in ((hiT[i2], Whi, BhiT[i2]), (loT[i2], Whi, BloT[i2]), (hiT[i2], Wlo, BhiT[i2])):
                    for k in range(8):
                        P.op("pe", MM(pl, xT_[:, k, :], W_[:, k, :], n_ == 0, n_ == 23), reads=[Bx, BwR], writes=[BpsLs[i2]])
                        n_ += 1
                P.op("act", ACTV(lg[:, tt, :], pl, AF.Copy), reads=[BpsLs[i2]], writes=[Blg])

            rt_front(0)
            for tt in range(16):
                if tt + 1 < 16:
                    rt_front(tt + 1)
                rt_back(tt)

            def R(eng, fn):
                return P.op(eng, fn, reads=[Brt, Blg, Bcst], writes=[Brt])

            def col(t):
                return t.rearrange("p (t o) -> p t o", o=1)
            R("dve", TT(lg, lg, bc(rowbc[:, R_BR:R_BR + 36].rearrange("p (o c) -> p o c", o=1), [128, 16, 36]), ALU.add))
            gl = lg[:, :, 0:4]
            R("dve", lambda e: e.tensor_reduce(out=gmax, in_=gl, axis=AX.X, op=ALU.max))
            R("dve", TT(ohg, gl, bc(col(gmax), [128, 16, 4]), ALU.is_equal))
            R("dve", TT(gsh, gl, bc(col(gmax), [128, 16, 4]), ALU.subtract))
            R("act", ACTV(gsh, gsh, AF.Exp))
            R("dve", RSUM(sume, gsh))
            R("dve", RECIP(pg, sume))
            R("dve", CP(t32, lg[:, :, 4:36]))
            el = t32.rearrange("p t (g e) -> p (t g) e", g=4)
            R("dve", TT(selg, el, bc(ohg.rearrange("p t g -> p (t g)").rearrange("p (x o) -> p x o", o=1), [128, 64, 8]), ALU.mult))
            R("dve", RSUM(sel, selg.rearrange("p (t g) e -> p t e g", g=4)))
            R("dve", lambda e: e.tensor_reduce(out=v0, in_=sel, axis=AX.X, op=ALU.max))
            R("dve", TT(m1, sel, bc(col(v0), [128, 16, 8]), ALU.is_equal))
            R("dve", STT(sel2, m1, -1e30, sel, ALU.mult, ALU.add))
            R("dve", lambda e: e.tensor_reduce(out=v1, in_=sel2, axis=AX.X, op=ALU.max))
            R("dve", TT(m2, sel2, bc(col(v1), [128, 16, 8]), ALU.is_equal))
            R("dve", TT(dlt, v1, v0, ALU.subtract))
            R("act", ACTV(exd, dlt, AF.Exp))
            R("dve", TS(w1, exd, 1.0, ALU.add))
            R("dve", RECIP(w1, w1))
            R("dve", TT(w2, exd, w1, ALU.mult))
            R("dve", TT(g12[:, :, 0], w1, pg, ALU.mult))
            R("dve", TT(g12[:, :, 1], w2, pg, ALU.mult))
            R("dve", TT(gsh, ohg, bc(c8g.rearrange("p (o g) -> p o g", o=1), [128, 16, 4]), ALU.mult))
            R("dve", RSUM(gi8, gsh))
            io8 = bc(iota[:, 0:8].rearrange("p (o e) -> p o e", o=1), [128, 16, 8])
            R("dve", TT(tm8, m1, io8, ALU.mult))
            R("dve", RSUM(i1, tm8))
            R("dve", TT(tm8, m2, io8, ALU.mult))
            R("dve", RSUM(i2_, tm8))
            R("dve", TT(eid1, gi8, i1, ALU.add))
            R("dve", TT(eid2, gi8, i2_, ALU.add))
            io32 = bc(iota[:, 0:32].rearrange("p (o e) -> p o e", o=1), [128, 16, 32])
            R("dve", TT(oh1, io32, bc(col(eid1), [128, 16, 32]), ALU.is_equal))
            R("dve", TT(oh2, io32, bc(col(eid2), [128, 16, 32]), ALU.is_equal))
            R("dve", TT(OH, oh1, oh2, ALU.add))
            R("dve", CP(OHb, OH))
            psC = bank(0)
            psN = bank(2)
            BpsC, BpsN = Buf(), Buf()
            for tt in range(16):
                for j in range(tt):
                    P.op("pe", MM(psC[:, tt * 32:(tt + 1) * 32], onesb, OHb[:, j, :], j == 0, False, skip=True),
                         reads=[Brt, Bcnt], writes=[BpsC])
                P.op("pe", MM(psC[:, tt * 32:(tt + 1) * 32], utri, OHb[:, tt, :], tt == 0, True, skip=True),
                     reads=[Brt, Bcnt], writes=[BpsC])
            for tt in range(16):
                P.op("pe", MM(psN[:, 0:32], onesb, OHb[:, tt, :], tt == 0, tt == 15), reads=[Brt, Bcnt], writes=[BpsN])
            P.op("dve", CP(incl, psC.rearrange("p (t e) -> p t e", t=16)), reads=[BpsC], writes=[Brt])
            P.op("dve", CP(ne, psN[:, 0:32]), reads=[BpsN], writes=[Brt])
            R("dve", TT(t32, oh1, incl, ALU.mult))
            R("dve", RSUM(rank1, t32))
            R("dve", TT(t32, oh2, incl, ALU.mult))
            R("dve", RSUM(rank2, t32))
            R("dve", TSS(nt, ne, 0.0, ALU.is_gt))
            for j in range(1, 8):
                R("dve", STT(nt, ne, 256.0 * j, nt, ALU.is_gt, ALU.add))
            R("dve", CP(csum, nt))
            cur, oth = csum, csum2
            for s_ in (1, 2, 4, 8, 16):
                R("dve", CP(oth[:, 0:s_], cur[:, 0:s_]))
                R("dve", TT(oth[:, s_:32], cur[:, s_:32], cur[:, 0:32 - s_], ALU.add))
                cur, oth = oth, cur
            cfin = cur
            R("dve", TT(excl, cfin, nt, ALU.subtract))
            R("dve", TS(sbase, excl, 256.0, ALU.mult))
            sb3 = bc(sbase.rearrange("p (o e) -> p o e", o=1), [128, 16, 32])
            R("dve", TT(t32, oh1, sb3, ALU.mult))
            R("dve", RSUM(posf2[:, :, 0], t32))
            R("dve", TT(t32, oh2, sb3, ALU.mult))
            R("dve", RSUM(posf2[:, :, 1], t32))
            R("dve", TT(posf2[:, :, 0], posf2[:, :, 0], rank1, ALU.add))
            R("dve", TT(posf2[:, :, 1], posf2[:, :, 1], rank2, ALU.add))
            R("dve", TS(posf2, posf2, -1.0, ALU.add))
            R("dve", CP(posi2, posf2))
            jt = bc(iota[:, 0:NTL].rearrange("p (j o) -> p j o", o=1), [128, NTL, 32])
            R("dve", TT(Aje, jt, bc(excl.rearrange("p (o e) -> p o e", o=1), [128, NTL, 32]), ALU.is_ge))
            R("dve", TT(Bje, jt, bc(cfin.rearrange("p (o e) -> p o e", o=1), [128, NTL, 32]), ALU.is_lt))
            R("dve", TT(Aje, Aje, Bje, ALU.mult))
            R("dve", RSUM(used, Aje))
            R("dve", TT(Bje, Aje, bc(iota[:, 0:32].rearrange("p (o e) -> p o e", o=1), [128, NTL, 32]), ALU.mult))
            R("dve", RSUM(eot, Bje))
            R("dve", TS(widxf, eot, 128.0, ALU.mult, pcol, ALU.add))
            R("dve", TS(used, used, -BIG, ALU.mult, BIG, ALU.add))
            R("dve", TT(widxf, widxf, used, ALU.add))
            R("dve", CP(widx, widxf))
            R("dve", TS(tokf, iota[:, 0:16], 128.0, ALU.mult, pcol, ALU.add))
            R("dve", CP(tokrow, bc(col(tokf), [128, 16, 16])))
            dump("posi2", posi2, [128, 16, 2], I32, [Brt])
            dump("widx", widx, [128, NTL], I32, [Brt])
            dump("g12", g12, [128, 16, 2], F32, [Brt])
            Btos_list = []
            for tt in range(16):
                for k in range(2):
                    bt = Buf()
                    Btos_list.append(bt)
                    P.dma("pool", IDMA(tos_d[:, :], tokrow[:, tt, :], out_off=posi2[:, tt, k:k + 1], bound=NSL - 1),
                          reads=[Brt, BtosD], writes=[bt])
            BtosT = Buf()
            P.dma("sp", DMA(tosT, tos_d.rearrange("(js p) w -> p js w", p=128)), reads=[BtosD] + Btos_list, writes=[BtosT])
            P.op("dve", CP(tosf, tosT[:, :, 0]), reads=[BtosT], writes=[BtosT])
            P.op("dve", TSS(yvalid, tosf, 2048.0, ALU.is_lt), reads=[BtosT], writes=[BtosT])
            P.op("dve", TS(yidxf, rowbc[:, R_IOTA:R_IOTA + 2 * NTL], 128.0, ALU.mult, pmB, ALU.add),
                 reads=[BtosT, Bcst], writes=[BtosT])
            P.op("dve", TT(yidxf, yidxf, yvalid, ALU.mult), reads=[BtosT], writes=[BtosT])
            P.op("dve", TS(yidxf, yidxf, BIG, ALU.add), reads=[BtosT], writes=[BtosT])
            P.op("dve", CP(yidx, yidxf), reads=[BtosT], writes=[BtosT])
            dump("tosT", tosT, [128, 2 * NTL, 16], I32, [BtosT])
            dump("yidx", yidx, [128, 2 * NTL], I32, [BtosT])
            P.fence()
            BXg0 = Buf()
            for a_ in range(3):
                for b_ in range(2):
                    P.op("pool", MSET(Xg[a_][b_], 0.0), writes=[BXg0])
            BWg, BWu, BWd = [Buf(), Buf(), Buf()], [Buf(), Buf(), Buf()], [Buf(), Buf(), Buf()]
            BYd_list = []
            BXg = [[Buf(), Buf()], [Buf(), Buf()], [Buf(), Buf()]]
            BxgT = [Buf(), Buf()]
            Bsa, Bhd, Bpa, Bpu, Bpy, Bysb = Buf(), Buf(), Buf(), Buf(), [Buf(), Buf()], [Buf(), Buf()]
            BpsXg = [Buf(), Buf()]
            psa, psu = bank(0), bank(1)
            NT_RUN = NTL
            for nm in dbg:
                if nm.startswith("ntl"):
                    NT_RUN = int(nm[3:])
            def ffn_xgathers(j):
                wb = j % 3
                for s_ in range(2):
                    js = 2 * j + s_
                    P.dma("pool", IDMA(Xg[wb][s_], hn_d[:, :], in_off=tosT[:, js, 0:1], bound=S - 1),
                          reads=[BtosT, BhnD, BXg0], writes=[BXg[wb][s_]])

            def ffn_gathers(j):
                wb = j % 3
                P.dma("pool", IDMA(Wg[wb].rearrange("p k f -> p (k f)"), wGt_d[:, :], in_off=widx[:, j:j + 1], bound=4095),
                      reads=[Brt], writes=[BWg[wb]])
                P.dma("pool", IDMA(Wu[wb].rearrange("p k f -> p (k f)"), wUt_d[:, :], in_off=widx[:, j:j + 1], bound=4095),
                      reads=[Brt], writes=[BWu[wb]])
                P.dma("pool", IDMA(Wd[wb].rearrange("p c d -> p (c d)"), wDt_d[:, :], in_off=widx[:, j:j + 1], bound=4095),
                      reads=[Brt], writes=[BWd[wb]])

            def ffn_transposes(j):
                wb = j % 3
                xb_ = j % 2
                for s_ in range(2):
                    pX = bank_bf(6 + s_)
                    for k in range(8):
                        P.op("pe", TR(pX[:, k * 128:(k + 1) * 128], Xg[wb][s_][:, k * 128:(k + 1) * 128], identb),
                             reads=[BXg[wb][s_]], writes=[BpsXg[s_]])
                    P.op("act" if s_ == 0 else "dve",
                         (ACTV(xgT[xb_][:, :, s_ * 128:(s_ + 1) * 128], pX.rearrange("p (k t) -> p k t", k=8), AF.Copy) if s_ == 0
                          else CP(xgT[xb_][:, :, s_ * 128:(s_ + 1) * 128], pX.rearrange("p (k t) -> p k t", k=8))),
                         reads=[BpsXg[s_]], writes=[BxgT[xb_]])

            def ffn_compute(j):
                wb = j % 3
                xb_ = j % 2
                for fc in range(4):
                    pb, Bp = (psa, Bpa) if fc < 2 else (psu, Bpu)
                    Wsrc = Wg[wb] if fc < 2 else Wu[wb]
                    BWs = BWg[wb] if fc < 2 else BWu[wb]
                    for k in range(8):
                        P.op("pe", MM(pb[:, (fc % 2) * 256:(fc % 2) * 256 + 256], Wsrc[:, k, (fc % 2) * 128:(fc % 2) * 128 + 128],
                                      xgT[xb_][:, k, :], k == 0, k == 7), reads=[BWs, BxgT[xb_]], writes=[Bp])
                P.op("act", ACTV(sa, psa, AF.Silu), reads=[Bpa], writes=[Bsa])
                P.op("dve", TT(hdnT.rearrange("p c t -> p (c t)"), sa, psu, ALU.mult), reads=[Bsa, Bpu], writes=[Bhd])

            def ffn_down(j):
                wb = j % 3
                for s_ in range(2):
                    js = 2 * j + s_
                    ys = js % 2
                    py = bank(2 + 2 * ys, 2)
                    for hf in range(2):
                        for c2 in range(2):
                            P.op("pe", MM(py[:, hf * 512:(hf + 1) * 512], hdnT[:, c2, s_ * 128:(s_ + 1) * 128],
                                          Wd[wb][:, c2, hf * 512:(hf + 1) * 512], c2 == 0, c2 == 1),
                                 reads=[Bhd, BWd[wb]], writes=[Bpy[ys]])
                    P.op("act" if ys == 0 else "dve",
                         (ACTV(ysb[ys], py, AF.Copy) if ys == 0 else CP(ysb[ys], py)), reads=[Bpy[ys]], writes=[Bysb[ys]])
                    byd = Buf()
                    BYd_list.append(byd)
                    P.dma("sp", DMA(Y_d[js * 128:(js + 1) * 128, :], ysb[ys]), reads=[Bysb[ys]], writes=[byd])

            for j0 in range(min(3, NT_RUN)):
                ffn_xgathers(j0)
                ffn_gathers(j0)
            ffn_transposes(0)
            if NT_RUN > 3:
                ffn_xgathers(3)
            for j in range(NT_RUN):
                ffn_compute(j)
                if j + 1 < NT_RUN:
                    ffn_transposes(j + 1)
                    if j + 4 < NT_RUN:
                        ffn_xgathers(j + 4)
                ffn_down(j)
                if j + 3 < NT_RUN:
                    ffn_gathers(j + 3)
            P.fence()
            yk_all = list(yk) + list(ysb)
            Byk = [Buf() for _ in range(len(yk_all))]
            cnt_ = 0
            for tt in range(16):
                for k in range(2):
                    bi = cnt_ % len(yk_all)
                    cnt_ += 1
                    P.dma("pool", IDMA(yk_all[bi], Y_d[:, :], in_off=posi2[:, tt, k:k + 1], bound=NSL - 1),
                          reads=BYd_list + [Brt], writes=[Byk[bi]])
                    P.op("dve", STT(hres[:, tt, :], yk_all[bi], g12[:, tt, k:k + 1], hres[:, tt, :], ALU.mult, ALU.add),
                         reads=[Byk[bi], Brt, Bh[tt]], writes=[Bh[tt]])
                if tt >= 1:
                    final_tile(tt - 1)
            final_tile(15)
            FINAL_DONE[0] = True
        elif "noF" not in dbg:
            P.fence()
            A.top = mark_after_hres
            gF_off = R_GF
            hn32 = [A.alloc([1024], F32) for _ in range(2)]
            hnT32 = A.alloc([8, 128], F32)
            wR32 = A.alloc([8, 36], F32)
            hnT = R1.rearrange("p a b -> p (a b)")[:, 0:8 * S].rearrange("p (k t) -> p k t", k=8)
            ssh = A.alloc([16], F32)
            sqh = A.alloc([16], F32)
            rh = A.alloc([16], F32)
            lg = A.alloc([36], F32)
            g8 = A.alloc([8], F32)
            m8 = A.alloc([8], F32)
            m8b = A.alloc([8], F32)
            ohg = A.alloc([4], F32)
            negm = A.alloc([1], F32)
            ejunk = A.alloc([4], F32)
            sume = A.alloc([1], F32)
            pg = A.alloc([1], F32)
            selg = A.alloc([4, 8], F32)
            sel = A.alloc([8], F32)
            m1 = A.alloc([8], F32)
            m2 = A.alloc([8], F32)
            dlt = A.alloc([1], F32)
            exd = A.alloc([1], F32)
            w12 = A.alloc([2], F32)
            g12 = A.alloc([16, 2], F32)
            me = A.alloc([8], F32)
            gates = A.alloc([16, 32], F32)
            BwR, Bhn32, BhnT32, BhnT, Brt, Bgates = Buf(), [Buf(), Buf()], Buf(), Buf(), Buf(), Buf()
            P.dma("sp", DMA(wR32, wR_d.rearrange("(k p) c -> p k c", p=128)), writes=[BwR])
            P.op("dve", MSET(ssh, 0.0), writes=[Brt])
            P.op("dve", MSET(g8, -1e30), writes=[Brt])
            psX = bank(6, 2)
            psL = bank(1)
            BpsX, BpsL = Buf(), Buf()
            gFbc = rowbc[:, gF_off:gF_off + 1024]
            lvl = 9
            for nm in dbg:
                if nm.startswith("stoprt"):
                    lvl = int(nm[6:])
            for tt in range(16):
                i2 = tt % 2
                sl = slice(tt * 128, (tt + 1) * 128)
                P.op("act", ACTV(junk, hres[:, tt, :], AF.Square, accum_out=ssh[:, tt:tt + 1]), reads=[Bh[tt], Brt],
                     writes=[Brt])
                P.op("act", ACTV(sqh[:, tt:tt + 1], ssh[:, tt:tt + 1], AF.Sqrt, bias=epsb, scale=1.0 / D), reads=[Brt], writes=[Brt])
                P.op("dve", RECIP(rh[:, tt:tt + 1], sqh[:, tt:tt + 1]), reads=[Brt], writes=[Brt])
                P.op("dve", STT(hn32[i2], hres[:, tt, :], rh[:, tt:tt + 1], gFbc, ALU.mult, ALU.mult),
                     reads=[Bh[tt], Brt, Bcst], writes=[Bhn32[i2]])
                if lvl < 2:
                    continue
                for k in range(8):
                    P.op("pe", MM(psX[:, k * 128:(k + 1) * 128], hn32[i2][:, k * 128:(k + 1) * 128], identf, True, True),
                         reads=[Bhn32[i2]], writes=[BpsX])
                P.op("act", ACTV(hnT32, psX.rearrange("p (k t) -> p k t", k=8), AF.Copy), reads=[BpsX], writes=[BhnT32])
                P.op("pool", CP(hnT[:, :, sl], hnT32), reads=[BhnT32], writes=[BhnT])
                if lvl < 3:
                    continue
                for k in range(8):
                    P.op("pe", MM(psL[:, 0:36], hnT32[:, k, :], wR32[:, k, :], k == 0, k == 7), reads=[BhnT32, BwR], writes=[BpsL])
                P.op("dve", TT(lg, psL[:, 0:36], rowbc[:, R_BR:R_BR + 36], ALU.add), reads=[BpsL, Bcst], writes=[Brt])
                if lvl < 4:
                    continue
                P.op("dve", CP(g8[:, 0:4], lg[:, 0:4]), reads=[Brt], writes=[Brt])
                P.op("dve", lambda e: e.max(out=m8, in_=g8), reads=[Brt], writes=[Brt])
                P.op("dve", TS(ohg, lg[:, 0:4], m8[:, 0:1], ALU.is_equal), reads=[Brt], writes=[Brt])
                P.op("dve", TS(negm, m8[:, 0:1], -1.0, ALU.mult), reads=[Brt], writes=[Brt])
                P.op("act", ACTV(ejunk, lg[:, 0:4], AF.Exp, bias=negm, accum_out=sume), reads=[Brt], writes=[Brt])
                P.op("dve", RECIP(pg, sume), reads=[Brt], writes=[Brt])
                el = lg[:, 4:36].rearrange("p (g e) -> p g e", g=4)
                P.op("dve", TT(selg, el, bc(ohg.rearrange("p (g o) -> p g o", o=1), [128, 4, 8]), ALU.mult), reads=[Brt], writes=[Brt])
                P.op("dve", RSUM(sel, selg.rearrange("p g e -> p e g")), reads=[Brt], writes=[Brt])
                P.op("dve", lambda e: e.max(out=m8b, in_=sel), reads=[Brt], writes=[Brt])
                P.op("dve", TS(m1, sel, m8b[:, 0:1], ALU.is_equal), reads=[Brt], writes=[Brt])
                P.op("dve", TS(m2, sel, m8b[:, 1:2], ALU.is_equal), reads=[Brt], writes=[Brt])
                P.op("dve", TT(dlt, m8b[:, 1:2], m8b[:, 0:1], ALU.subtract), reads=[Brt], writes=[Brt])
                P.op("act", ACTV(exd, dlt, AF.Exp), reads=[Brt], writes=[Brt])
                P.op("dve", TS(w12[:, 0:1], exd, 1.0, ALU.add), reads=[Brt], writes=[Brt])
                P.op("dve", RECIP(w12[:, 0:1], w12[:, 0:1]), reads=[Brt], writes=[Brt])
                P.op("dve", TT(w12[:, 1:2], exd, w12[:, 0:1], ALU.mult), reads=[Brt], writes=[Brt])
                P.op("dve", TS(g12[:, tt, :], w12, pg[:, 0:1], ALU.mult), reads=[Brt], writes=[Brt])
                P.op("dve", TS(me, m1, g12[:, tt, 0:1], ALU.mult), reads=[Brt], writes=[Brt])
                P.op("dve", STT(me, m2, g12[:, tt, 1:2], me, ALU.mult, ALU.add), reads=[Brt], writes=[Brt])
                P.op("dve", TT(gates[:, tt, :].rearrange("p (g e) -> p g e", g=4),
                               bc(me.rearrange("p (o e) -> p o e", o=1), [128, 4, 8]),
                               bc(ohg.rearrange("p (g o) -> p g o", o=1), [128, 4, 8]), ALU.mult), reads=[Brt], writes=[Bgates])
            dump("gates", gates, [128, 16, 32], F32, [Bgates])
            dump("hnT", hnT, [128, 8, S], BF16, [BhnT])
            P.fence()
            DENSE_EXPERTS = N_EXP
            for nm in dbg:
                if nm.startswith("nexp"):
                    DENSE_EXPERTS = int(nm[4:])
            Wgu = [A.alloc([8, 512], BF16) for _ in range(2)]
            Wd = [A.alloc([2, 1024], BF16) for _ in range(2)]
            sa = A.alloc([512], F32)
            hdnT = A.alloc([2, 256], BF16)
            BW = [Buf(), Buf()]
            Bsa, Bhd, Bpa, Bpu, Bpy = Buf(), Buf(), Buf(), Buf(), [Buf(), Buf()]
            psa, psu = bank(0), bank(1)
            ycnt = 0
            for e_ in range(DENSE_EXPERTS):
                wb = e_ % 2
                er = slice(e_ * 128, (e_ + 1) * 128)
                P.dma("pool", DMA(Wgu[wb][:, :, 0:256], wGt_d[er, :].rearrange("p (k f) -> p k f", k=8)), writes=[BW[wb]])
                P.dma("pool", DMA(Wgu[wb][:, :, 256:512], wUt_d[er, :].rearrange("p (k f) -> p k f", k=8)), writes=[BW[wb]])
                P.dma("pool", DMA(Wd[wb], wDt_d[er, :].rearrange("p (c d) -> p c d", c=2)), writes=[BW[wb]])
                for pr in range(8):
                    tok = slice(pr * 256, (pr + 1) * 256)
                    for fc in range(4):
                        pb, Bp = (psa, Bpa) if fc < 2 else (psu, Bpu)
                        for k in range(8):
                            P.op("pe", MM(pb[:, (fc % 2) * 256:(fc % 2) * 256 + 256], Wgu[wb][:, k, fc * 128:(fc + 1) * 128],
                                          hnT[:, k, tok], k == 0, k == 7), reads=[BW[wb], BhnT], writes=[Bp])
                    P.op("act", ACTV(sa, psa, AF.Silu), reads=[Bpa], writes=[Bsa])
                    P.op("dve", TT(hdnT.rearrange("p c t -> p (c t)"), sa, psu, ALU.mult), reads=[Bsa, Bpu], writes=[Bhd])
                    for sub in range(2):
                        tt = 2 * pr + sub
                        ys = ycnt % 2
                        ycnt += 1
                        py = bank(2 + 2 * ys, 2)
                        for hf in range(2):
                            for c2 in range(2):
                                P.op("pe", MM(py[:, hf * 512:(hf + 1) * 512], hdnT[:, c2, sub * 128:(sub + 1) * 128],
                                              Wd[wb][:, c2, hf * 512:(hf + 1) * 512], c2 == 0, c2 == 1),
                                     reads=[Bhd, BW[wb]], writes=[Bpy[ys]])
                        P.op("dve", STT(hres[:, tt, :], py, gates[:, tt, e_:e_ + 1], hres[:, tt, :], ALU.mult, ALU.add),
                             reads=[Bpy[ys], Bgates, Bh[tt]], writes=[Bh[tt]])
        if not FINAL_DONE[0]:
            for tt in range(16):
                final_tile(tt)
        print('SBUF arena peak bytes', A.peak, 'cap', A.cap)
        P.emit(nc, block, sems, dsems, finals)
    return nc, dbg_d


_NC_CACHE = {}


def kernel(**inputs):
    inp = {k: np.asarray(v) for k, v in inputs.items()}
    if "nc" not in _NC_CACHE:
        _NC_CACHE["nc"] = build_nc()[0]
    nc = _NC_CACHE["nc"]
    sh = prep_shared(inp)
    in_maps = []
    for b in range(8):
        m = dict(sh)
        m["x"] = np.ascontiguousarray(inp["x"][b], dtype=np.float32)
        m["pos"] = np.ascontiguousarray(inp["positions"][b:b + 1]).astype(np.int32)
        in_maps.append(m)
    res = run_bass_kernel_spmd(nc, in_maps, core_ids=list(range(8)))
    out = np.stack([np.asarray(r["out"], dtype=np.float32) for r in res.results], axis=0)
    return out
```

```python
import numpy as np
import ml_dtypes
import concourse.bass as bass
import concourse.mybir as mybir
from concourse.bass_utils import run_bass_kernel_spmd

F32 = mybir.dt.float32
BF16 = mybir.dt.bfloat16
I32 = mybir.dt.int32
AF = mybir.ActivationFunctionType
ALU = mybir.AluOpType
AX = mybir.AxisListType


class Buf:
    __slots__ = ("name", "writer", "readers")

    def __init__(self, name=""):
        self.name = name
        self.writer = None
        self.readers = []


class Op:
    __slots__ = ("eng", "fn", "deps", "signal", "val", "sem", "is_dma", "idx")

    def __init__(self, eng, fn, is_dma=False):
        self.eng = eng
        self.fn = fn
        self.deps = []
        self.signal = False
        self.val = None
        self.sem = None
        self.is_dma = is_dma
        self.idx = None


ENGS = ("pe", "act", "dve", "pool", "sp")


class Plan:
    def __init__(self, n_dma_sems=24):
        self.ops = {e: [] for e in ENGS}
        self.n_dma_sems = n_dma_sems
        self.dma_count = 0
        self.dma_counts = [0, 0]
        self.dma_last = [None] * n_dma_sems
        self.nops = 0

    def _add(self, op, reads, writes, deps):
        op.idx = self.nops
        self.nops += 1
        dl = []
        for b in reads:
            if b.writer is not None:
                dl.append((b.writer, "raw"))
        for b in writes:
            if b.writer is not None:
                dl.append((b.writer, "waw"))
            for r in b.readers:
                dl.append((r, "war"))
        for d in deps:
            if d is not None:
                dl.append((d, "raw"))
        for b in reads:
            b.readers.append(op)
        for b in writes:
            b.writer = op
            b.readers = []
        seen = set()
        for d, kind in dl:
            if d is op or id(d) in seen:
                continue
            if (not d.is_dma) and d.eng == op.eng and not op.is_dma:
                if d.eng == "pe":
                    continue
            seen.add(id(d))
            d.signal = True
            op.deps.append(d)
        self.ops[op.eng].append(op)
        return op

    def op(self, eng, fn, reads=(), writes=(), deps=()):
        return self._add(Op(eng, fn), list(reads), list(writes), list(deps))

    def dma(self, eng, fn, reads=(), writes=(), deps=()):
        op = Op(eng, fn, is_dma=True)
        half = self.n_dma_sems // 2
        grp = 1 if eng == "pool" else 0
        cnt = self.dma_counts[grp]
        s = grp * half + cnt % half
        op.sem = s
        op.val = 16 * (cnt // half + 1)
        self.dma_counts[grp] += 1
        self.dma_count += 1
        deps = list(deps)
        if self.dma_last[s] is not None:
            deps.append(self.dma_last[s])
        self.dma_last[s] = op
        op.signal = True
        return self._add(op, list(reads), list(writes), deps)

    def emit(self, nc, block, sems, dma_sems, final_waits):
        for e in ENGS:
            c = 0
            for op in self.ops[e]:
                if op.is_dma:
                    continue
                if op.signal:
                    c += 1
                    op.val = c
        plan = self

        def run(eng_name, eng):
            waited = {}
            for op in plan.ops[eng_name]:
                need = {}
                for d in op.deps:
                    key = ("dma", d.sem) if d.is_dma else ("eng", d.eng)
                    if d.val > need.get(key, 0):
                        need[key] = d.val
                for key, v in need.items():
                    if waited.get(key, 0) >= v:
                        continue
                    waited[key] = v
                    sem = dma_sems[key[1]] if key[0] == "dma" else sems[key[1]]
                    eng.wait_ge(sem, v)
                if op.fn is None:
                    continue
                ins = op.fn(eng)
                if op.is_dma:
                    ins.then_inc(dma_sems[op.sem], 16)
                elif op.signal:
                    ins.then_inc(sems[eng_name], 1)
            if eng_name == "sp":
                for d in final_waits:
                    sem = dma_sems[d.sem] if d.is_dma else sems[d.eng]
                    eng.wait_ge(sem, d.val)

        @block.tensor
        def _(pe):
            run("pe", pe)

        @block.scalar
        def _(act):
            run("act", act)

        @block.vector
        def _(dve):
            run("dve", dve)

        @block.gpsimd
        def _(pool):
            run("pool", pool)

        @block.sync
        def _(sp):
            run("sp", sp)


S = 2048
D = 1024
NT = 16
EPS = 1e-6
THETA = 10000.0
LAM_INIT = 0.8 - 0.6 * 1.0
NEG = -30000.0
N_EXP = 32
DFF = 256

C_GA, C_GQKV, C_GF, C_INVD, C_INVM, C_SGND, C_SGNM, C_DIVQ, C_PCOL, C_PMB, NCST = 0, 8, 13, 21, 22, 23, 24, 25, 27, 28, 29
R_FING, R_SUBG, R_LAM, R_BR, R_GF, R_IOTA, R_C8G, NROW = 0, 1024, 1152, 1408, 1444, 2468, 2468 + 96, 2468 + 100


class Arena:
    def __init__(self, nc, es, nbytes):
        self.t = es.enter_context(nc.sbuf_tensor("arena", [128, nbytes // 2], BF16))
        self.top = 0
        self.cap = nbytes
        self.peak = 0

    def alloc(self, free_shape, dt):
        esz = 4 if dt in (F32, I32) else 2
        n = int(np.prod(free_shape))
        nb = (n * esz + 63) // 64 * 64
        off = self.top
        self.top += nb
        self.peak = max(self.peak, self.top)
        assert self.top <= self.cap, ("SBUF arena overflow", self.top, self.cap)
        v = self.t[:, off // 2: off // 2 + (n * esz) // 2]
        if dt != BF16:
            v = v.bitcast(dt)
        if len(free_shape) == 2:
            v = v.rearrange("p (a b) -> p a b", a=free_shape[0])
        elif len(free_shape) == 3:
            v = v.rearrange("p (a b c) -> p a b c", a=free_shape[0], b=free_shape[1])
        return v


def bc(ap, shape):
    return ap.to_broadcast(list(shape))


def MM(out, lhsT, rhs, start=True, stop=True, skip=False):
    if skip:
        return lambda e: e.matmul(out, lhsT, rhs, start=start, stop=stop, skip_group_check=True)
    return lambda e: e.matmul(out, lhsT, rhs, start=start, stop=stop)


def TR(out, in_, ident):
    return lambda e: e.transpose(out, in_, ident)


def ACTV(out, in_, func, bias=None, scale=1.0, accum_out=None):
    kw = {}
    if bias is not None:
        kw["bias"] = bias
    if accum_out is not None:
        kw["accum_out"] = accum_out
    return lambda e: e.activation(out=out, in_=in_, func=func, scale=scale, **kw)


def TT(out, in0, in1, op):
    return lambda e: e.tensor_tensor(out=out, in0=in0, in1=in1, op=op)


def TS(out, in0, s1, op0, s2=None, op1=None):
    if op1 is None:
        return lambda e: e.tensor_scalar(out=out, in0=in0, scalar1=s1, scalar2=None, op0=op0)
    return lambda e: e.tensor_scalar(out=out, in0=in0, scalar1=s1, scalar2=s2, op0=op0, op1=op1)


def STT(out, in0, scalar, in1, op0, op1):
    return lambda e: e.scalar_tensor_tensor(out=out, in0=in0, scalar=scalar, in1=in1, op0=op0, op1=op1)


def CP(out, in_):
    return lambda e: e.tensor_copy(out=out, in_=in_)


def MSET(out, val):
    return lambda e: e.memset(out, val)


def RECIP(out, in_):
    return lambda e: e.reciprocal(out=out, in_=in_)


def RSUM(out, in_):
    return lambda e: e.reduce_sum(out=out, in_=in_, axis=AX.X)


def DMA(out, in_):
    return lambda e: e.dma_start(out=out, in_=in_)


def _rot_cols(base, n, grp):
    idx = np.arange(n).reshape(-1, grp)
    half = grp // 2
    idx = np.concatenate([idx[:, half:], idx[:, :half]], axis=1).reshape(-1)
    return base + idx


def prep_shared(inp):
    f = np.float32
    w_in = np.asarray(inp["w_in"])[0]
    wAV = np.ascontiguousarray(np.concatenate([w_in[:, 0:640], w_in[:, 1696:2208]], axis=1))
    blocks = []
    for base in (672, 1184):
        for j in range(4):
            blocks.append(w_in[:, base + j * 128: base + (j + 1) * 128])
            blocks.append(w_in[:, _rot_cols(base + j * 128, 128, 64)])
    kpe = np.zeros((D, 128), f)
    kpe[:, 64:96] = w_in[:, 640:672]
    kper = np.zeros((D, 128), f)
    kper[:, 64:96] = w_in[:, _rot_cols(640, 32, 32)]
    blocks += [kpe, kper]
    wF = np.ascontiguousarray(np.concatenate(blocks, axis=1))
    w_uq = np.asarray(inp["w_uq"])[0]
    rot = np.zeros_like(w_uq)
    for h in range(8):
        rot[:, h * 96 + 64: h * 96 + 96] = w_uq[:, _rot_cols(h * 96 + 64, 32, 32)]
    wUQ = np.ascontiguousarray(np.concatenate([w_uq, rot], axis=1))
    w_ukv = np.asarray(inp["w_ukv"])[0]
    kcols = np.concatenate([np.arange(h * 128, h * 128 + 64) for h in range(8)])
    wUKV = np.ascontiguousarray(np.concatenate([w_ukv[:, kcols], w_ukv[:, kcols + 64]], axis=1))
    cst = np.zeros((128, NCST), f)
    p = np.arange(128)
    cst[:, C_GA:C_GA + 8] = np.asarray(inp["attn_norm_g"])[0].reshape(8, 128).T
    cst[:, C_GQKV:C_GQKV + 3] = np.asarray(inp["q_norm_g"])[0].reshape(3, 128).T
    cst[:, C_GQKV + 3:C_GQKV + 5] = np.asarray(inp["kv_norm_g"])[0].reshape(2, 128).T
    cst[:, C_GF:C_GF + 8] = np.asarray(inp["ffn_norm_g"])[0].reshape(8, 128).T
    invd = (f(THETA) ** (-(p % 32).astype(f) / f(32))).astype(f)
    invm = (f(THETA) ** (-(p % 16).astype(f) / f(16))).astype(f)
    cst[:, C_INVD] = invd / f(2 * np.pi)
    cst[:, C_INVM] = invm / f(2 * np.pi)
    cst[:, C_SGND] = np.where((p % 64) < 32, -1.0, 1.0)
    cst[:, C_SGNM] = np.where((p % 32) < 16, -1.0, 1.0)
    cst[:, C_DIVQ] = 1.0 / 384
    cst[:, C_DIVQ + 1] = 1.0 / 256
    cst[:, C_PCOL] = p
    cst[:, C_PMB] = p - float(1 << 16)
    rowc = np.zeros((1, NROW), f)
    rowc[0, R_FING:R_FING + 1024] = np.asarray(inp["final_norm_g"])
    rowc[0, R_SUBG:R_SUBG + 128] = np.asarray(inp["subln_g"])[0]
    rowc[0, R_LAM:R_LAM + 64] = np.asarray(inp["lambda_q1"])[0]
    rowc[0, R_LAM + 64:R_LAM + 128] = np.asarray(inp["lambda_q2"])[0]
    rowc[0, R_LAM + 128:R_LAM + 192] = np.asarray(inp["lambda_k1"])[0]
    rowc[0, R_LAM + 192:R_LAM + 256] = np.asarray(inp["lambda_k2"])[0]
    rowc[0, R_BR:R_BR + 4] = np.asarray(inp["b_router_group"])[0]
    rowc[0, R_BR + 4:R_BR + 36] = np.asarray(inp["b_router_expert"])[0].reshape(32)
    rowc[0, R_GF:R_GF + 1024] = np.asarray(inp["ffn_norm_g"])[0]
    rowc[0, R_IOTA:R_IOTA + 96] = np.arange(96)
    rowc[0, R_C8G:R_C8G + 4] = np.arange(4) * 8
    ident = np.eye(128, dtype=f)
    mtri = np.where(p[:, None] > p[None, :], f(NEG), f(0)).astype(f)
    wR = np.ascontiguousarray(np.concatenate([np.asarray(inp["w_router_group"])[0],
                                              np.asarray(inp["w_router_expert"])[0].reshape(D, 32)], axis=1))
    return {
        "wAV": wAV, "wF": wF, "wUQ": wUQ, "wUKV": wUKV, "wO": np.ascontiguousarray(np.asarray(inp["w_o"])[0]),
        "cst": cst, "rowc": rowc, "ident": ident, "mtri": mtri, "wR": wR,
        "wGt": np.ascontiguousarray(np.asarray(inp["w_gate"])[0].reshape(32, 8, 128, 256).transpose(0, 2, 1, 3).reshape(4096, 2048)),
        "wUt": np.ascontiguousarray(np.asarray(inp["w_up"])[0].reshape(32, 8, 128, 256).transpose(0, 2, 1, 3).reshape(4096, 2048)),
        "wDt": np.ascontiguousarray(np.asarray(inp["w_down"])[0].reshape(32, 2, 128, 1024).transpose(0, 2, 1, 3).reshape(4096, 2048)),
        "utri": np.triu(np.ones((128, 128), f)),
    }


_BOUND_REGS = {}


def IDMA(out, in_, out_off=None, in_off=None, bound=None):
    def f(e):
        key = (id(e), bound)
        if key not in _BOUND_REGS:
            _BOUND_REGS[key] = e.to_reg(bound)
        return e.indirect_dma_start(
            out=out, out_offset=(bass.IndirectOffsetOnAxis(ap=out_off, axis=0) if out_off is not None else None),
            in_=in_, in_offset=(bass.IndirectOffsetOnAxis(ap=in_off, axis=0) if in_off is not None else None),
            bounds_check=_BOUND_REGS[key], oob_is_err=False)
    return f


def TSS(out, in_, scalar, op):
    return lambda e: e.tensor_single_scalar(out=out, in_=in_, scalar=scalar, op=op)


def _fence(P):
    lasts = []
    for e in ENGS:
        comp = [o for o in P.ops[e] if (not o.is_dma) and o.fn is not None]
        if comp:
            lasts.append(comp[-1])
    dmas = [o for o in P.dma_last if o is not None]
    for e in ENGS:
        op = Op(e, None)
        op.idx = P.nops
        P.nops += 1
        for d in lasts + dmas:
            d.signal = True
            op.deps.append(d)
        P.ops[e].append(op)


Plan.fence = _fence


def build_nc(dbg=()):
    from contextlib import ExitStack
    _BOUND_REGS.clear()
    nc = bass.Bass("TRN2", target_bir_lowering=False)

    def din(name, shape, dt=F32):
        return nc.dram_tensor(name, list(shape), dt, kind="ExternalInput").ap()

    x_d = din("x", [S, D])
    pos_d = din("pos", [1, S], I32)
    wAV_d = din("wAV", [D, 1152])
    wF_d = din("wF", [D, 2304])
    wUQ_d = din("wUQ", [384, 1536])
    wUKV_d = din("wUKV", [256, 1024])
    wO_d = din("wO", [D, D])
    cst_d = din("cst", [128, NCST])
    rowc_d = din("rowc", [1, NROW])
    ident_d = din("ident", [128, 128])
    mtri_d = din("mtri", [128, 128])
    wR_d = din("wR", [D, 36])
    wGt_d = din("wGt", [4096, 2048])
    wUt_d = din("wUt", [4096, 2048])
    wDt_d = din("wDt", [4096, 2048])
    utri_d = din("utri", [128, 128])
    out_d = nc.dram_tensor("out", [S, D], F32, kind="ExternalOutput").ap()
    dbg_d = {}

    P = Plan()
    es = ExitStack()
    finals = []
    with es:
        A = Arena(nc, es, 207 * 1024)
        ps_all = es.enter_context(nc.psum_tensor("ps_all", [128, 8 * 512], F32))
        sems = {e: es.enter_context(nc.semaphore("s_" + e)) for e in ENGS}
        dsems = [es.enter_context(nc.semaphore("d%d" % i)) for i in range(P.n_dma_sems)]
        block = es.enter_context(nc.Block())

        def bank(b, n=1):
            return ps_all[:, b * 512:(b + n) * 512]

        def bank_bf(b):
            return ps_all[:, b * 512:(b + 1) * 512].bitcast(BF16)

        def dump(name, ap, shape, dt, reads=()):
            if name not in dbg:
                return
            d = nc.dram_tensor("dbg_" + name, list(shape), dt, kind="ExternalOutput").ap()
            dbg_d[name] = d
            finals.append(P.dma("sp", DMA(d, ap), reads=list(reads)))

        cst = A.alloc([NCST], F32)
        rowbc = A.alloc([NROW], F32)
        identf = A.alloc([128], F32)
        onesf = A.alloc([128], F32)
        identb = A.alloc([128], BF16)
        mtrib = A.alloc([128], BF16)
        epsb = A.alloc([1], F32)
        ssx = A.alloc([16], F32)
        sqx = A.alloc([16], F32)
        rstdx = A.alloc([16], F32)
        ssqkv = A.alloc([16, 2], F32)
        t_a = A.alloc([16, 2], F32)
        t_b = A.alloc([16, 2], F32)
        t_c = A.alloc([16, 2], F32)
        sqkv = A.alloc([16, 2], F32)
        lamt = A.alloc([128], F32)
        lam2 = A.alloc([2], F32)
        lame = A.alloc([2], F32)
        neglam = A.alloc([1], F32)
        dss = A.alloc([16, 4], F32)
        dsq = A.alloc([16, 4], F32)
        drd = A.alloc([16, 4], F32)
        ssf = A.alloc([16], F32)
        sqf = A.alloc([16], F32)
        rf = A.alloc([16], F32)
        junk = A.alloc([1024], BF16)
        Bcst, Bjunk = Buf("cst"), Buf("junk")

        P.dma("sp", DMA(cst, cst_d[:, :]), writes=[Bcst])
        P.dma("sp", DMA(rowbc, rowc_d.partition_broadcast(128)), writes=[Bcst])
        P.dma("sp", DMA(identf, ident_d[:, :]), writes=[Bcst])
        P.dma("pool", DMA(identb, ident_d[:, :]), writes=[Bcst])
        P.dma("pool", DMA(mtrib, mtri_d[:, :]), writes=[Bcst])
        Bst = Buf("stats")
        Bst_t = [Buf() for _ in range(16)]
        P.op("dve", MSET(epsb, EPS), writes=[Bst])
        P.op("dve", MSET(onesf, 1.0), writes=[Bst])
        for t_ in (ssx, ssqkv, dss, ssf):
            P.op("dve", MSET(t_, 0.0), writes=[Bst] + Bst_t)
        P.op("dve", MSET(epsb, EPS), writes=[Bst] + Bst_t)
        P.op("dve", TT(lamt, rowbc[:, R_LAM:R_LAM + 128], rowbc[:, R_LAM + 128:R_LAM + 256], ALU.mult),
             reads=[Bcst], writes=[Bst])
        P.op("dve", RSUM(lam2, lamt.rearrange("p (a b) -> p a b", a=2)), reads=[Bst], writes=[Bst])
        P.op("act", ACTV(lame, lam2, AF.Exp), reads=[Bst], writes=[Bst])
        P.op("dve", TT(neglam, lame[:, 1:2], lame[:, 0:1], ALU.subtract), reads=[Bst], writes=[Bst])
        P.op("dve", TS(neglam, neglam, -LAM_INIT, ALU.add), reads=[Bst], writes=[Bst])

        R1 = A.alloc([8, 2304], BF16)
        wF = R1
        mark_cnT = A.top
        cnT = A.alloc([5, S], BF16)
        kpeT = A.alloc([S], BF16)
        posf = A.alloc([S], F32)
        mark_VD = A.top
        VD = A.alloc([16, 4, 129], BF16)
        QTD = A.alloc([4, S], BF16)
        KTD = A.alloc([4, S], BF16)
        mark_wAV = A.top
        wAV = A.alloc([8, 1152], BF16)
        xs = [A.alloc([1024], F32)] * 2
        xb = [A.alloc([1024], BF16) for _ in range(2)]
        xT = [A.alloc([8, 512], BF16) for _ in range(2)]
        cn = [A.alloc([640], BF16) for _ in range(2)]
        CD = A.alloc([512], F32)
        SD = A.alloc([512], F32)
        CMr = A.alloc([512], F32)
        SMr = A.alloc([512], F32)
        u2 = A.alloc([2, 512], F32)
        nn = A.alloc([2, 512], F32)
        ni = nn.bitcast(I32)
        cs_set = [A.alloc([2, 512], F32) for _ in range(2)]
        t1 = [A.alloc([512], F32)] * 2
        t2 = [A.alloc([512], F32)] * 2
        diag = [A.alloc([128], F32) for _ in range(2)]
        _save = A.top
        A.top = mark_wAV
        KTz = A.alloc([8, S], BF16)
        mark_after_KTz = A.top
        A.top = _save
        BKTz = Buf("KTz")
        posi = ni.rearrange("p a b -> p (a b)")

        Bpos = Buf("pos")
        for hh in range(2):
            P.dma("sp", DMA(posi, pos_d[:, hh * 1024:(hh + 1) * 1024].partition_broadcast(128)), writes=[Bpos])
            P.op("dve", CP(posf[:, hh * 1024:(hh + 1) * 1024], posi), reads=[Bpos], writes=[Bpos])
        BVD = Buf("VD")
        P.op("pool", MSET(VD[:, :, :, 128:129], 1.0), writes=[BVD])

        BwAV = Buf("wAV")
        BwF = [Buf("wF%d" % i) for i in range(3)]
        P.dma("pool", DMA(wAV, wAV_d.rearrange("(k p) c -> p k c", p=128)), writes=[BwAV])
        for i in range(3):
            P.dma("pool", DMA(wF[:, :, i * 768:(i + 1) * 768],
                              wF_d[:, i * 768:(i + 1) * 768].rearrange("(k p) c -> p k c", p=128)), writes=[BwF[i]])

        gA3 = cst[:, C_GA:C_GA + 8].rearrange("p (a b) -> p a b", b=1)
        gQ3 = cst[:, C_GQKV:C_GQKV + 5].rearrange("p (a b) -> p a b", b=1)
        psT, psT2, psA0, psA1, psV, psB, psF0, psF1 = [bank(i) for i in range(8)]
        psT_b = bank_bf(0)
        psT2_b = bank_bf(1)
        BpsT, BpsT2, BpsA0, BpsA1, BpsV, BpsB, BpsF0, BpsF1 = [Buf("ps%d" % i) for i in range(8)]
        Bxs = [Buf()] * 2
        Bxb = [Buf(), Buf()]
        BxT = [[Buf() for _ in range(4)] for _ in range(2)]
        Bcn = [Buf(), Buf()]
        Btab = Buf("tab")
        Btmp = Buf("tabtmp")
        Bt12 = [Buf()] * 2
        Bdiag = [Buf(), Buf()]
        BcnT = Buf("cnT")
        Bqk = Buf("qkT")
        def a_load(tt):
            sl = slice(tt * 128, (tt + 1) * 128)
            i2 = tt % 2
            P.dma("sp", DMA(xs[i2], x_d[sl, :]), writes=[Bxs[i2]])
            P.dma("pool", DMA(xb[i2], x_d[sl, :]), writes=[Bxb[i2]])

        def a_step1(tc, r):
            tt = 4 * tc + r
            sl = slice(tt * 128, (tt + 1) * 128)
            rs = slice(r * 128, (r + 1) * 128)
            i2 = tt % 2
            buf = tc % 2
            rx = rstdx[:, tt:tt + 1]
            P.op("act", ACTV(junk, xs[i2], AF.Square, accum_out=ssx[:, tt:tt + 1]),
                 reads=[Bxs[i2], Bst_t[tt]], writes=[Bst_t[tt]])
            if tt + 1 < 16:
                a_load(tt + 1)
            P.op("act", ACTV(sqx[:, tt:tt + 1], ssx[:, tt:tt + 1], AF.Sqrt, bias=epsb, scale=1.0 / D),
                 reads=[Bst_t[tt]], writes=[Bst_t[tt]])
            P.op("dve", RECIP(rstdx[:, tt:tt + 1], sqx[:, tt:tt + 1]), reads=[Bst_t[tt]], writes=[Bst_t[tt]])
            for k in range(8):
                P.op("pe", TR(psT_b[:, k * 128:(k + 1) * 128], xb[i2][:, k * 128:(k + 1) * 128], identb),
                     reads=[Bxb[i2], Bcst], writes=[BpsT])
            P.op("dve", TT(xT[buf][:, :, rs], psT_b.rearrange("p (k t) -> p k t", k=8), bc(gA3, [128, 8, 128]),
                           ALU.mult), reads=[BpsT, Bcst], writes=[BxT[buf][r]])

        def a_step2(tc, r):
            tt = 4 * tc + r
            sl = slice(tt * 128, (tt + 1) * 128)
            rs = slice(r * 128, (r + 1) * 128)
            i2 = tt % 2
            buf = tc % 2
            rx = rstdx[:, tt:tt + 1]
            for (c0, c1, pb, Bp) in ((0, 384, psA0, BpsA0), (384, 640, psA1, BpsA1), (640, 1152, psV, BpsV)):
                for k in range(8):
                    P.op("pe", MM(pb[:, 0:c1 - c0], xT[buf][:, k, rs], wAV[:, k, c0:c1], k == 0, k == 7),
                         reads=[BxT[buf][r], BwAV], writes=[Bp])
            P.op("act", ACTV(junk[:, 0:384], psA0[:, 0:384], AF.Square, accum_out=ssqkv[:, tt, 0:1]),
                 reads=[BpsA0, Bst_t[tt]], writes=[Bst_t[tt]])
            P.op("act", ACTV(junk[:, 0:256], psA1[:, 0:256], AF.Square, accum_out=ssqkv[:, tt, 1:2]),
                 reads=[BpsA1, Bst_t[tt]], writes=[Bst_t[tt]])
            P.op("dve", STT(t_a[:, tt, :], ssqkv[:, tt, :], rx, cst[:, C_DIVQ:C_DIVQ + 2], ALU.mult, ALU.mult),
                 reads=[Bst_t[tt], Bcst], writes=[Bst_t[tt]])
            P.op("dve", TS(t_a[:, tt, :], t_a[:, tt, :], rx, ALU.mult), reads=[Bst_t[tt]], writes=[Bst_t[tt]])
            P.op("act", ACTV(t_b[:, tt, :], t_a[:, tt, :], AF.Sqrt, bias=epsb, scale=1.0), reads=[Bst_t[tt]], writes=[Bst_t[tt]])
            P.op("dve", RECIP(t_c[:, tt, :], t_b[:, tt, :]), reads=[Bst_t[tt]], writes=[Bst_t[tt]])
            P.op("dve", TS(sqkv[:, tt, :], t_c[:, tt, :], rx, ALU.mult), reads=[Bst_t[tt]], writes=[Bst_t[tt]])
            P.op("dve", TS(cn[i2][:, 0:384], psA0[:, 0:384], sqkv[:, tt, 0:1], ALU.mult),
                 reads=[BpsA0, Bst_t[tt]], writes=[Bcn[i2]])
            P.op("dve", TS(cn[i2][:, 384:640], psA1[:, 0:256], sqkv[:, tt, 1:2], ALU.mult),
                 reads=[BpsA1, Bst_t[tt]], writes=[Bcn[i2]])

        def a_step3(tc, r):
            tt = 4 * tc + r
            sl = slice(tt * 128, (tt + 1) * 128)
            rs = slice(r * 128, (r + 1) * 128)
            i2 = tt % 2
            buf = tc % 2
            rx = rstdx[:, tt:tt + 1]
            for j in range(5):
                P.op("pe", TR(psT2_b[:, j * 128:(j + 1) * 128], cn[i2][:, j * 128:(j + 1) * 128], identb),
                     reads=[Bcn[i2], Bcst], writes=[BpsT2])
            P.op("dve", TT(cnT[:, :, sl], psT2_b[:, 0:640].rearrange("p (k t) -> p k t", k=5),
                           bc(gQ3, [128, 5, 128]), ALU.mult), reads=[BpsT2, Bcst], writes=[BcnT])
            P.op("act", ACTV(VD[:, tt, :, 0:128], psV.rearrange("p (h d) -> p h d", h=4), AF.Copy, scale=rx),
                 reads=[BpsV, Bst_t[tt]], writes=[BVD])
            P.op("dve", TS(diag[i2], identf, rx, ALU.mult), reads=[Bst_t[tt], Bcst], writes=[Bdiag[i2]])
            P.op("pe", MM(psB[:, rs], onesf, diag[i2], True, True), reads=[Bdiag[i2], Bst_t[tt]], writes=[BpsB])

        MAGIC = 12582912.0
        Bcs = [Buf(), Buf()]

        def a_tabprep(tc, si):
            chunk = slice(tc * 512, (tc + 1) * 512)
            invc = (C_INVD, C_INVM)[si]
            iv = cst[:, invc:invc + 1]
            cs_ = cs_set[si]
            P.op("dve", TS(u2[:, 0, :], posf[:, chunk], iv, ALU.mult), reads=[Bpos, Bcst], writes=[Btmp])
            P.op("dve", TS(u2[:, 1, :], posf[:, chunk], iv, ALU.mult, 0.25, ALU.add), reads=[Bpos, Bcst], writes=[Btmp])
            P.op("dve", TS(nn, u2, MAGIC, ALU.add, MAGIC, ALU.subtract), reads=[Btmp], writes=[Btmp])
            P.op("dve", TT(cs_, u2, nn, ALU.subtract), reads=[Btmp], writes=[Bcs[si]])
            P.op("act", ACTV(cs_, cs_, AF.Sin, scale=float(2 * np.pi)), reads=[Bcs[si]], writes=[Bcs[si]])

        def a_tables(tc):
            for si, (sgnc, Cout, Sout) in enumerate(((C_SGND, CD, SD), (C_SGNM, CMr, SMr))):
                cs_ = cs_set[si]
                P.op("dve", STT(Sout, cs_[:, 0, :], cst[:, sgnc:sgnc + 1], psB, ALU.mult, ALU.mult),
                     reads=[Bcs[si], BpsB, Bcst], writes=[Btab])
                P.op("dve", TT(Cout, cs_[:, 1, :], psB, ALU.mult), reads=[Bcs[si], BpsB], writes=[Btab])

        def a_feat(tc, i):
            buf = tc % 2
            chunk = slice(tc * 512, (tc + 1) * 512)
            for hf, (pb, Bp) in enumerate(((psF0, BpsF0), (psF1, BpsF1))):
                blk = 2 * i + hf
                for k in range(8):
                    P.op("pe", MM(pb, wF[:, k, blk * 128:(blk + 1) * 128], xT[buf][:, k, :], k == 0, k == 7),
                         reads=BxT[buf] + [BwF[blk // 6]], writes=[Bp])
            if i < 8:
                rows = slice(0, 128)
                Ct, St = CD, SD
                dest = (QTD if i < 4 else KTD)[:, i % 4, chunk]
            else:
                rows = slice(64, 96)
                Ct, St = CMr, SMr
                dest = kpeT[64:96, chunk]
            j2 = i % 2
            P.op("dve", TT(t1[j2][rows], psF0[rows], Ct[rows], ALU.mult), reads=[BpsF0, Btab], writes=[Bt12[j2]])
            P.op("dve", TT(t2[j2][rows], psF1[rows], St[rows], ALU.mult), reads=[BpsF1, Btab], writes=[Bt12[j2]])
            P.op("pool", TT(dest, t1[j2][rows], t2[j2][rows], ALU.add), reads=[Bt12[j2]], writes=[Bqk])


        a_load(0)
        for tc in range(4):
            for r in range(4):
                a_step1(tc, r)
                if tc > 0:
                    a_feat(tc - 1, 2 * r)
                a_step2(tc, r)
                if r < 2:
                    a_tabprep(tc, r)
                if tc > 0:
                    a_feat(tc - 1, 2 * r + 1)
                a_step3(tc, r)
            if tc > 0:
                a_feat(tc - 1, 8)
            if tc == 3:
                for h in range(4):
                    for half in range(2):
                        zrows = slice(64 * (1 - half), 64 * (1 - half) + 64)
                        P.op("dve", MSET(KTz[zrows, 2 * h + half, :], 0.0),
                             writes=[BKTz, BwAV, Bxs[0], Bxb[0], Bxb[1]] + BxT[0])
            a_tables(tc)
        for i in range(9):
            a_feat(3, i)
            if 4 <= i < 8:
                h = i - 4
                for half in range(2):
                    rows = slice(64 * half, 64 * half + 64)
                    P.op("act", ACTV(KTz[rows, 2 * h + half, :], KTD[rows, h, :], AF.Copy), reads=[Bqk], writes=[BKTz])

        dump("cnT", cnT, [128, 5, S], BF16, [BcnT])
        dump("QTD", QTD, [128, 4, S], BF16, [Bqk])
        dump("KTD", KTD, [128, 4, S], BF16, [Bqk])
        dump("kpeT", kpeT, [128, S], BF16, [Bqk])
        dump("VD", VD, [128, 16, 4, 129], BF16, [BVD])
        dump("rstdx", rstdx, [128, 16], F32, [Bst])
        P.fence()
        if "stopA" in dbg:
            P.emit(nc, block, sems, dsems, finals)
            return nc, dbg_d
        A.top = mark_after_KTz
        PT = [A.alloc([1024], BF16) for _ in range(3)]
        o_all = R1.rearrange("p a b -> p (a b)")[:, 0:16 * 1024].rearrange("p (t c) -> p t c", t=16)
        A1 = A.alloc([4, 128], F32)
        A2 = A.alloc([4, 128], F32)
        Dd = A.alloc([4, 128], F32)
        Dsq = A.alloc([4, 128], F32)
        rec4 = [A.alloc([4], F32) for _ in range(2)]
        BS = [Buf("S0"), Buf("S1"), Buf("S2")]
        BPT = [Buf(), Buf(), Buf()]
        Bacc = [Buf("acc0"), Buf("acc1")]
        Bo = Buf("o_all")
        BA1, BA2, BDd = Buf(), Buf(), Buf()
        Brec = [Buf(), Buf()]

        def run_attention(units, nS=2):
            groups = []
            for ui, u in enumerate(units):
                c = u["c"]
                gl = []
                for pr in range(2 * c):
                    gl.append(dict(ncols=1024, ents=[(2 * pr, 0, [(r, r * 128) for r in range(4)]),
                                                     (2 * pr + 1, 512, [(r, r * 128) for r in range(4)])]))
                gl.append(dict(ncols=1024, ents=[(4 * c, 0, [(r, r * 128) for r in range(4)]),
                                                 (4 * c + 1, 512, [(r, r * 128) for r in range(1, 4)])]))
                gl.append(dict(ncols=384, ents=[(4 * c + 2, 0, [(2, 0), (3, 128)]),
                                                (4 * c + 3, 0, [(3, 256)])]))
                for gi, g in enumerate(gl):
                    g["u"] = u
                    g["ui"] = ui
                    g["last"] = gi == len(gl) - 1
                    g["second_last"] = gi == len(gl) - 2
                    groups.append(g)

            def emit_qk(gidx):
                g = groups[gidx]
                u = g["u"]
                c = u["c"]
                Sg = bank(2 * (gidx % nS), 2)
                for (kb, base, rl) in g["ents"]:
                    r0 = rl[0][0]
                    c0 = base + rl[0][1]
                    n = len(rl)
                    isdiag = kb >= 4 * c
                    P.op("pe", MM(Sg[:, c0:c0 + n * 128], u["KT"][:, kb * 128:(kb + 1) * 128],
                                  u["QT"][:, (4 * c + r0) * 128:(4 * c + 4) * 128], True, not isdiag, skip=True),
                         writes=[BS[gidx % nS]])
                    if isdiag:
                        P.op("pe", MM(Sg[:, c0:c0 + 128], identb, mtrib, False, True, skip=True),
                             writes=[BS[gidx % nS]])

            def emit_exp_pv(gidx):
                g = groups[gidx]
                u = g["u"]
                c = u["c"]
                W = u["W"]
                Sg = bank(2 * (gidx % nS), 2)
                pt = PT[gidx % 3]
                P.op("act", ACTV(pt[:, 0:g["ncols"]], Sg[:, 0:g["ncols"]], AF.Exp, scale=u["scale"]),
                     reads=[BS[gidx % nS]], writes=[BPT[gidx % 3]])
                ci = g["ui"] % 2
                for (kb, base, rl) in g["ents"]:
                    for (r, col) in rl:
                        tok = u["acctok"](ci, r) if "acctok" in u else Bacc[ci]
                        P.op("pe", MM(u["acc"](ci, r), pt[:, base + col:base + col + 128], u["V"](kb),
                                      kb == 0 and (r % u["rper"]) == 0, kb == 4 * c + r, skip=True),
                             reads=[BPT[gidx % 3]], writes=[tok])
                if "post_half" in u:
                    if g["second_last"]:
                        u["post_half"](u, 0)
                    if g["last"]:
                        u["post_half"](u, 1)
                elif g["last"]:
                    u["post"](ci, u)

            G = len(groups)
            for g0 in range(min(nS - 1, G)):
                emit_qk(g0)
            for gidx in range(G):
                if gidx + nS - 1 < G:
                    emit_qk(gidx + nS - 1)
                emit_exp_pv(gidx)

        BaccD = [Buf("accD0"), Buf("accD1")]
        BrecD = [[Buf(), Buf()], [Buf(), Buf()]]
        BA1D, BA2D, BDdD, BDsqD = [[Buf(), Buf()] for _ in range(4)]

        def acc_diff(ci, r):
            return bank(6 + r // 2)[:, (r % 2) * 129:(r % 2) * 129 + 129]

        def acctok_diff(ci, r):
            return BaccD[r // 2]

        def post_diff_half(u, hb):
            h, half, c = u["h"], u["half"], u["c"]
            q0 = 2 * hb
            accv = bank(6 + hb)[:, 0:258].rearrange("p (s w) -> p s w", w=129)
            rc = rec4[half][:, q0:q0 + 2].rearrange("p (s o) -> p s o", o=1)
            P.op("dve", RECIP(rc, accv[:, :, 128:129]), reads=[BaccD[hb]], writes=[BrecD[half][hb]])
            dst = (A1 if half == 0 else A2)[:, q0:q0 + 2, :]
            P.op("dve", TT(dst, accv[:, :, 0:128], bc(rc, [128, 2, 128]), ALU.mult),
                 reads=[BaccD[hb], BrecD[half][hb]], writes=[BA1D[hb] if half == 0 else BA2D[hb]])
            if half == 1:
                Dd_, Dsq_ = Dd[:, q0:q0 + 2, :], Dsq[:, q0:q0 + 2, :]
                P.op("dve", STT(Dd_, A2[:, q0:q0 + 2, :], neglam[:, 0:1], A1[:, q0:q0 + 2, :], ALU.mult, ALU.add),
                     reads=[BA1D[hb], BA2D[hb], Bst], writes=[BDdD[hb]])
                P.op("pool", TT(Dsq_, Dd_, Dd_, ALU.mult), reads=[BDdD[hb]], writes=[BDsqD[hb]])
                P.op("dve", RSUM(dss[:, 4 * c + q0:4 * c + q0 + 2, h], Dsq_), reads=[BDsqD[hb]], writes=[Bst])
                P.op("pool", CP(o_all[:, 4 * c + q0:4 * c + q0 + 2, 512 + h * 128:512 + (h + 1) * 128], Dd_),
                     reads=[BDdD[hb]], writes=[Bo])

        units = []
        for h in range(4):
            for c in range(4):
                for half in range(2):
                    units.append(dict(KT=KTz[:, 2 * h + half, :], QT=QTD[:, h, :], W=129, rper=2, scale=64 ** -0.5, c=c, h=h,
                                      half=half, V=(lambda kb, h=h: VD[:, kb, h, :]), acc=acc_diff, acctok=acctok_diff,
                                      post_half=post_diff_half))
        run_attention(units, nS=3)
        dump("dss", dss, [128, 16, 4], F32, [Bst])
        dump("o_all", o_all, [128, 16, 1024], BF16, [Bo])
        P.fence()
        if "stopB" in dbg:
            P.emit(nc, block, sems, dsems, finals)
            return nc, dbg_d
        A.top = mark_VD
        QTM = A.alloc([8, S], BF16)
        KTM = A.alloc([8, S], BF16)
        VM = A.alloc([16, 8, 65], BF16)
        wUQ = A.alloc([3, 1536], BF16)
        wUKV = A.alloc([2, 1024], BF16)
        CM = A.alloc([512], F32)
        SM = A.alloc([512], F32)
        u2 = A.alloc([2, 512], F32)
        nf = A.alloc([2, 512], F32)
        cs = A.alloc([2, 512], F32)
        t1c = [A.alloc([512], F32)] * 2
        t2c = [A.alloc([512], F32)] * 2
        PT = [A.alloc([1024], BF16) for _ in range(3)]
        recm = [A.alloc([4], F32) for _ in range(2)]
        BwUQ, BwUKV, BVM, BQTM, BKTM, BQTMn, BKTMn = Buf(), Buf(), Buf(), Buf(), Buf(), Buf(), Buf()
        Btab, Btmp, Bt12c = Buf(), Buf(), [Buf()] * 2
        BpsC3 = [(Buf(), Buf(), Buf()), (Buf(), Buf(), Buf())]
        P.dma("pool", DMA(wUQ, wUQ_d.rearrange("(k p) c -> p k c", p=128)), writes=[BwUQ])
        P.dma("pool", DMA(wUKV, wUKV_d.rearrange("(k p) c -> p k c", p=128)), writes=[BwUKV])
        P.op("pool", MSET(VM[:, :, :, 64:65], 1.0), writes=[BVM])
        for h in range(8):
            P.op("pool", CP(KTM[64:96, h, :], kpeT[64:96, :]), writes=[BKTM])
        psVm = bank(3)
        BpsVm = Buf()
        for tt in range(16):
            sl = slice(tt * 128, (tt + 1) * 128)
            for k in range(2):
                P.op("pe", MM(psVm, cnT[:, 3 + k, sl], wUKV[:, k, 512:1024], k == 0, k == 1),
                     reads=[BwUKV], writes=[BpsVm])
            P.op("act", ACTV(VM[:, tt, :, 0:64], psVm.rearrange("p (h d) -> p h d", h=8), AF.Copy),
                 reads=[BpsVm], writes=[BVM])
        for tc in range(4):
            chunk = slice(tc * 512, (tc + 1) * 512)
            iv = cst[:, C_INVM:C_INVM + 1]
            P.op("dve", TS(u2[:, 0, :], posf[:, chunk], iv, ALU.mult), writes=[Btmp, Bt12c[1]])
            P.op("dve", TS(u2[:, 1, :], posf[:, chunk], iv, ALU.mult, 0.25, ALU.add), writes=[Btmp, Bt12c[1]])
            P.op("dve", TS(nf, u2, MAGIC, ALU.add, MAGIC, ALU.subtract), reads=[Btmp], writes=[Btmp])
            P.op("dve", TT(cs, u2, nf, ALU.subtract), reads=[Btmp], writes=[Btmp])
            P.op("act", ACTV(cs, cs, AF.Sin, scale=float(2 * np.pi)), reads=[Btmp], writes=[Btmp])
            P.op("dve", TS(SM, cs[:, 0, :], cst[:, C_SGNM:C_SGNM + 1], ALU.mult), reads=[Btmp], writes=[Btab])
            P.op("dve", CP(CM, cs[:, 1, :]), reads=[Btmp], writes=[Btab])
            for h in range(8):
                par = h % 2
                psQ0, psQ1, psK = bank(4 * par), bank(4 * par + 1), bank(4 * par + 2)
                BpsQ0, BpsQ1, BpsK = BpsC3[par]
                for hf, (pb, Bp) in enumerate(((psQ0, BpsQ0), (psQ1, BpsQ1))):
                    for k in range(3):
                        P.op("pe", MM(pb[0:96, :], wUQ[:, k, hf * 768 + h * 96:hf * 768 + (h + 1) * 96], cnT[:, k, chunk],
                                      k == 0, k == 2), reads=[BwUQ], writes=[Bp])
                for k in range(2):
                    P.op("pe", MM(psK[0:64, :], wUKV[:, k, h * 64:(h + 1) * 64], cnT[:, 3 + k, chunk], k == 0, k == 1),
                         reads=[BwUKV], writes=[BpsK])
                P.op("act", ACTV(QTM[0:64, h, chunk], psQ0[0:64, :], AF.Copy), reads=[BpsQ0], writes=[BQTMn])
                P.op("dve", TT(t1c[par][64:96], psQ0[64:96, :], CM[64:96], ALU.mult), reads=[BpsQ0, Btab], writes=[Bt12c[par]])
                P.op("dve", TT(t2c[par][64:96], psQ1[64:96, :], SM[64:96], ALU.mult), reads=[BpsQ1, Btab], writes=[Bt12c[par]])
                P.op("dve", TT(QTM[64:96, h, chunk], t1c[par][64:96], t2c[par][64:96], ALU.add), reads=[Bt12c[par]], writes=[BQTM])
                P.op("act", ACTV(KTM[0:64, h, chunk], psK[0:64, :], AF.Copy), reads=[BpsK], writes=[BKTMn])
        dump("QTM", QTM, [128, 8, S], BF16, [BQTM, BQTMn])
        dump("KTM", KTM, [128, 8, S], BF16, [BKTM, BKTMn])
        dump("VM", VM, [128, 16, 8, 65], BF16, [BVM])
        P.fence()

        def acc_mla(ci, r):
            return bank(6 + ci)[:, r * 65:(r + 1) * 65]

        def post_mla(ci, u):
            h, c = u["h"], u["c"]
            accv = bank(6 + ci)[:, 0:260].rearrange("p (r w) -> p r w", w=65)
            rc = recm[ci].rearrange("p (r o) -> p r o", o=1)
            P.op("dve", RECIP(rc, accv[:, :, 64:65]), reads=[Bacc[ci]], writes=[Brec[ci]])
            P.op("dve", TT(o_all[:, 4 * c:4 * c + 4, h * 64:(h + 1) * 64], accv[:, :, 0:64], bc(rc, [128, 4, 64]),
                           ALU.mult), reads=[Bacc[ci], Brec[ci]], writes=[Bo])

        units = []
        for h in range(8):
            for c in range(4):
                units.append(dict(KT=KTM[0:96, h, :], QT=QTM[0:96, h, :], W=65, rper=4, scale=96 ** -0.5, c=c, h=h,
                                  V=(lambda kb, h=h: VM[:, kb, h, :]), acc=acc_mla, post=post_mla))
        run_attention(units, nS=3)
        P.fence()

        A.top = mark_cnT
        hres = A.alloc([16, 1024], F32)
        ob = [A.alloc([1024], F32) for _ in range(2)]
        mark_after_hres = A.top
        wO = A.alloc([8, 1024], BF16)
        mixT = [A.alloc([8, 128], BF16) for _ in range(2)]
        xs2 = [A.alloc([1024], F32) for _ in range(2)]
        BwO, Bmix, Bxs2, Bob, Bh = Buf(), [Buf(), Buf()], [Buf(), Buf()], [Buf(), Buf()], [Buf() for _ in range(16)]
        P.dma("pool", DMA(wO, wO_d.rearrange("(k p) c -> p k c", p=128)), writes=[BwO])
        P.op("act", ACTV(dsq, dss, AF.Sqrt, bias=epsb, scale=1.0 / 128), reads=[Bst], writes=[Bst])
        P.op("dve", RECIP(drd, dsq), reads=[Bst], writes=[Bst])
        P.op("dve", TS(drd, drd, 1.0 - LAM_INIT, ALU.mult), reads=[Bst], writes=[Bst])
        subg = rowbc[:, R_SUBG:R_SUBG + 128].rearrange("p (a b) -> p a b", a=1)
        fing = rowbc[:, R_FING:R_FING + 1024]
        BpsTe, BpsO = Buf(), [Buf(), Buf()]
        psTe = bank_bf(0)
        Bo_t = [Buf() for _ in range(16)]
        for tt in range(16):
            od = o_all[:, tt, 512:1024].rearrange("p (h d) -> p h d", h=4)
            P.op("dve", TT(od, od, bc(drd[:, tt, :].rearrange("p (h o) -> p h o", o=1), [128, 4, 128]), ALU.mult),
                 reads=[Bo, Bst], writes=[Bo_t[tt]])
            P.op("dve", TT(od, od, bc(subg, [128, 4, 128]), ALU.mult), reads=[Bo_t[tt], Bcst], writes=[Bo_t[tt]])

        def e_transposes(tt):
            i2 = tt % 2
            for c8 in range(8):
                P.op("pe", TR(psTe[:, c8 * 128:(c8 + 1) * 128], o_all[:, tt, c8 * 128:(c8 + 1) * 128], identb),
                     reads=[Bo, Bo_t[tt]], writes=[BpsTe])
            P.op("act", ACTV(mixT[i2], psTe.rearrange("p (c t) -> p c t", c=8), AF.Copy), reads=[BpsTe], writes=[Bmix[i2]])

        def e_matmuls(tt):
            i2 = tt % 2
            sl = slice(tt * 128, (tt + 1) * 128)
            psO = bank(2 + 2 * i2, 2)
            for hf in range(2):
                for c8 in range(8):
                    P.op("pe", MM(psO[:, hf * 512:(hf + 1) * 512], mixT[i2][:, c8, :], wO[:, c8, hf * 512:(hf + 1) * 512],
                                  c8 == 0, c8 == 7), reads=[Bmix[i2], BwO], writes=[BpsO[i2]])
            P.dma("sp", DMA(xs2[i2], x_d[sl, :]), writes=[Bxs2[i2]])
            P.op("dve", TT(hres[:, tt, :], psO, xs2[i2], ALU.add), reads=[BpsO[i2], Bxs2[i2]], writes=[Bh[tt]])

        e_transposes(0)
        for tt in range(16):
            if tt + 1 < 16:
                e_transposes(tt + 1)
            e_matmuls(tt)
        dump("hres", hres, [128, 16, 1024], F32, Bh)
        FINAL_DONE = [False]

        def final_tile(tt):
            i2 = tt % 2
            sl = slice(tt * 128, (tt + 1) * 128)
            P.op("act", ACTV(junk, hres[:, tt, :], AF.Square, accum_out=ssf[:, tt:tt + 1]), reads=[Bh[tt], Bst],
                 writes=[Bst])
            P.op("act", ACTV(sqf[:, tt:tt + 1], ssf[:, tt:tt + 1], AF.Sqrt, bias=epsb, scale=1.0 / D), reads=[Bst], writes=[Bst])
            P.op("dve", RECIP(rf[:, tt:tt + 1], sqf[:, tt:tt + 1]), reads=[Bst], writes=[Bst])
            P.op("dve", STT(ob[i2], hres[:, tt, :], rf[:, tt:tt + 1], fing, ALU.mult, ALU.mult),
                 reads=[Bh[tt], Bst, Bcst], writes=[Bob[i2]])
            finals.append(P.dma("sp", DMA(out_d[sl, :], ob[i2]), reads=[Bob[i2]]))

        if "noF" not in dbg and "dense" not in dbg:
            P.fence()
            A.top = mark_after_hres
            BIG = float(1 << 16)
            NTL = 47
            NSL = NTL * 256
            hn_d = nc.dram_tensor("hn_scr", [S, D], BF16).ap()
            tos_d = nc.dram_tensor("tos_scr", [NSL, 16], I32).ap()
            Y_d = nc.dram_tensor("y_scr", [NSL, D], BF16).ap()
            BhnD, BtosD, BYd = Buf(), Buf(), Buf()
            R1f = R1.rearrange("p a b -> p (a b)")
            Wg = [R1f[:, i * 6144: i * 6144 + 2048].rearrange("p (k f) -> p k f", k=8) for i in range(3)]
            Wu = [R1f[:, i * 6144 + 2048: i * 6144 + 4096].rearrange("p (k f) -> p k f", k=8) for i in range(3)]
            Wd = [R1f[:, i * 6144 + 4096: i * 6144 + 6144].rearrange("p (c d) -> p c d", c=2) for i in range(3)]
            wR32 = A.alloc([8, 36], F32)
            Whi = A.alloc([8, 36], BF16)
            Wlo = A.alloc([8, 36], BF16)
            wRt = A.alloc([8, 36], F32)
            ssh = A.alloc([16], F32)
            sqh = A.alloc([16], F32)
            rh = A.alloc([16], F32)
            lg = A.alloc([16, 36], F32)
            utri = A.alloc([128], BF16)
            onesb = A.alloc([128], BF16)

            def f16(n):
                return A.alloc([16, n], F32)
            gmax, sume, pg, v0, v1, dlt, exd, w1, w2, gi8, i1, i2_, eid1, eid2, rank1, rank2 = [A.alloc([16], F32) for _ in range(16)]
            ohg, gsh = f16(4), f16(4)
            selg = A.alloc([64, 8], F32)
            sel, m1, m2, sel2, tm8 = f16(8), f16(8), f16(8), f16(8), f16(8)
            oh1, oh2, OH, incl, t32 = f16(32), f16(32), f16(32), f16(32), f16(32)
            OHb = A.alloc([16, 32], BF16)
            g12 = A.alloc([16, 2], F32)
            posf2 = A.alloc([16, 2], F32)
            posi2 = A.alloc([16, 2], I32)
            tokf = A.alloc([16], F32)
            tokrow = A.alloc([16, 16], I32)
            ne, nt, csum, csum2, excl, sbase = [A.alloc([32], F32) for _ in range(6)]
            eot, used, widxf = [A.alloc([NTL], F32) for _ in range(3)]
            widx = A.alloc([NTL], I32)
            tosT = A.alloc([2 * NTL, 16], I32)
            tosf, yvalid, yidxf = [A.alloc([2 * NTL], F32) for _ in range(3)]
            yidx = A.alloc([2 * NTL], I32)
            m_ = A.top
            hn32 = [A.alloc([1024], F32) for _ in range(2)]
            hnb = [A.alloc([1024], BF16) for _ in range(2)]
            hnl = [A.alloc([1024], BF16) for _ in range(2)]
            hiT = [A.alloc([8, 128], BF16) for _ in range(2)]
            loT = [A.alloc([8, 128], BF16) for _ in range(2)]
            Bje = A.alloc([NTL, 32], F32)
            Aje = A.alloc([NTL, 32], F32)
            bigt = A.alloc([NSL * 16 // 128], I32)
            top_r = A.top
            A.top = m_
            Xg = [[A.alloc([1024], BF16) for _ in range(2)] for _ in range(3)]
            sa = A.alloc([512], F32)
            hdnT = A.alloc([2, 256], BF16)
            xgT = [A.alloc([8, 256], BF16) for _ in range(2)]
            ysb = [A.alloc([1024], BF16) for _ in range(2)]
            yk = [A.alloc([1024], BF16) for _ in range(4)]
            A.top = max(A.top, top_r)
            iota = rowbc[:, R_IOTA:R_IOTA + 96]
            c8g = rowbc[:, R_C8G:R_C8G + 4]
            pcol = cst[:, C_PCOL:C_PCOL + 1]
            pmB = cst[:, C_PMB:C_PMB + 1]
            BwR, Bhn32, Bhnb, Brt, Bcnt = Buf(), [Buf(), Buf()], [Buf(), Buf()], Buf(), Buf()
            Bhnl, BhiT, BloT, BpsH, BpsLo = [[Buf(), Buf()] for _ in range(5)]
            BwR0 = Buf()
            P.dma("sp", DMA(wR32, wR_d.rearrange("(k p) c -> p k c", p=128)), writes=[BwR0])
            P.op("dve", CP(Whi, wR32), reads=[BwR0], writes=[BwR])
            P.op("dve", TT(wRt, wR32, Whi, ALU.subtract), reads=[BwR0, BwR], writes=[BwR])
            P.op("dve", CP(Wlo, wRt), reads=[BwR], writes=[BwR])
            P.dma("pool", DMA(utri, utri_d[:, :]), writes=[Bcnt])
            P.op("dve", MSET(ssh, 0.0), writes=[Brt])
            P.op("pool", MSET(onesb, 1.0), writes=[Bcnt])
            P.op("pool", MSET(bigt, 1 << 16), writes=[Bcnt])
            P.dma("sp", DMA(tos_d.rearrange("(p r) w -> p (r w)", p=128), bigt), reads=[Bcnt], writes=[BtosD])
            psX = bank(6, 2)
            psL = bank(1)
            BpsX, BpsL = Buf(), Buf()
            gFbc = rowbc[:, R_GF:R_GF + 1024]
            for tt in range(16):
                P.op("act", ACTV(junk, hres[:, tt, :], AF.Square, accum_out=ssh[:, tt:tt + 1]), reads=[Bh[tt], Brt],
                     writes=[Brt])
            P.op("act", ACTV(sqh, ssh, AF.Sqrt, bias=epsb, scale=1.0 / D), reads=[Brt], writes=[Brt])
            P.op("dve", RECIP(rh, sqh), reads=[Brt], writes=[Brt])
            Blg = Buf()
            BpsXs, BpsLs = [Buf(), Buf()], [Buf(), Buf()]
            def rt_front(tt):
                i2 = tt % 2
                sl = slice(tt * 128, (tt + 1) * 128)
                P.op("dve", STT(hn32[i2], hres[:, tt, :], rh[:, tt:tt + 1], gFbc, ALU.mult, ALU.mult),
                     reads=[Bh[tt], Brt, Bcst], writes=[Bhn32[i2]])
                P.op("act", ACTV(hnb[i2], hn32[i2], AF.Copy), reads=[Bhn32[i2]], writes=[Bhnb[i2]])
                P.dma("sp", DMA(hn_d[sl, :], hnb[i2]), reads=[Bhnb[i2]], writes=[BhnD])
                P.op("dve", TT(hnl[i2], hn32[i2], hnb[i2], ALU.subtract), reads=[Bhn32[i2], Bhnb[i2]], writes=[Bhnl[i2]])
                pH, pL = bank_bf(4 + 2 * i2), bank_bf(5 + 2 * i2)
                for k in range(8):
                    P.op("pe", TR(pH[:, k * 128:(k + 1) * 128], hnb[i2][:, k * 128:(k + 1) * 128], identb),
                         reads=[Bhnb[i2]], writes=[BpsH[i2]])
                for k in range(8):
                    P.op("pe", TR(pL[:, k * 128:(k + 1) * 128], hnl[i2][:, k * 128:(k + 1) * 128], identb),
                         reads=[Bhnl[i2]], writes=[BpsLo[i2]])
                P.op("act", ACTV(hiT[i2], pH.rearrange("p (k t) -> p k t", k=8), AF.Copy), reads=[BpsH[i2]], writes=[BhiT[i2]])
                P.op("act", ACTV(loT[i2], pL.rearrange("p (k t) -> p k t", k=8), AF.Copy), reads=[BpsLo[i2]], writes=[BloT[i2]])

            def rt_back(tt):
                i2 = tt % 2
                pl = bank(1 + i2)[:, 0:36]
                n_ = 0
                for (xT_, W_, Bx) in ((hiT[i2], Whi, BhiT[i2]), (loT[i2], Whi, BloT[i2]), (hiT[i2], Wlo, BhiT[i2])):
                    for k in range(8):
                        P.op("pe", MM(pl, xT_[:, k, :], W_[:, k, :], n_ == 0, n_ == 23), reads=[Bx, BwR], writes=[BpsLs[i2]])
                        n_ += 1
                P.op("act", ACTV(lg[:, tt, :], pl, AF.Copy), reads=[BpsLs[i2]], writes=[Blg])

            rt_front(0)
            for tt in range(16):
                if tt + 1 < 16:
                    rt_front(tt + 1)
                rt_back(tt)

            def R(eng, fn):
                return P.op(eng, fn, reads=[Brt, Blg, Bcst], writes=[Brt])

            def col(t):
                return t.rearrange("p (t o) -> p t o", o=1)
            R("dve", TT(lg, lg, bc(rowbc[:, R_BR:R_BR + 36].rearrange("p (o c) -> p o c", o=1), [128, 16, 36]), ALU.add))
            gl = lg[:, :, 0:4]
            R("dve", lambda e: e.tensor_reduce(out=gmax, in_=gl, axis=AX.X, op=ALU.max))
            R("dve", TT(ohg, gl, bc(col(gmax), [128, 16, 4]), ALU.is_equal))
            R("dve", TT(gsh, gl, bc(col(gmax), [128, 16, 4]), ALU.subtract))
            R("act", ACTV(gsh, gsh, AF.Exp))
            R("dve", RSUM(sume, gsh))
            R("dve", RECIP(pg, sume))
            R("dve", CP(t32, lg[:, :, 4:36]))
            el = t32.rearrange("p t (g e) -> p (t g) e", g=4)
            R("dve", TT(selg, el, bc(ohg.rearrange("p t g -> p (t g)").rearrange("p (x o) -> p x o", o=1), [128, 64, 8]), ALU.mult))
            R("dve", RSUM(sel, selg.rearrange("p (t g) e -> p t e g", g=4)))
            R("dve", lambda e: e.tensor_reduce(out=v0, in_=sel, axis=AX.X, op=ALU.max))
            R("dve", TT(m1, sel, bc(col(v0), [128, 16, 8]), ALU.is_equal))
            R("dve", STT(sel2, m1, -1e30, sel, ALU.mult, ALU.add))
            R("dve", lambda e: e.tensor_reduce(out=v1, in_=sel2, axis=AX.X, op=ALU.max))
            R("dve", TT(m2, sel2, bc(col(v1), [128, 16, 8]), ALU.is_equal))
            R("dve", TT(dlt, v1, v0, ALU.subtract))
            R("act", ACTV(exd, dlt, AF.Exp))
            R("dve", TS(w1, exd, 1.0, ALU.add))
            R("dve", RECIP(w1, w1))
            R("dve", TT(w2, exd, w1, ALU.mult))
            R("dve", TT(g12[:, :, 0], w1, pg, ALU.mult))
            R("dve", TT(g12[:, :, 1], w2, pg, ALU.mult))
            R("dve", TT(gsh, ohg, bc(c8g.rearrange("p (o g) -> p o g", o=1), [128, 16, 4]), ALU.mult))
            R("dve", RSUM(gi8, gsh))
            io8 = bc(iota[:, 0:8].rearrange("p (o e) -> p o e", o=1), [128, 16, 8])
            R("dve", TT(tm8, m1, io8, ALU.mult))
            R("dve", RSUM(i1, tm8))
            R("dve", TT(tm8, m2, io8, ALU.mult))
            R("dve", RSUM(i2_, tm8))
            R("dve", TT(eid1, gi8, i1, ALU.add))
            R("dve", TT(eid2, gi8, i2_, ALU.add))
            io32 = bc(iota[:, 0:32].rearrange("p (o e) -> p o e", o=1), [128, 16, 32])
            R("dve", TT(oh1, io32, bc(col(eid1), [128, 16, 32]), ALU.is_equal))
            R("dve", TT(oh2, io32, bc(col(eid2), [128, 16, 32]), ALU.is_equal))
            R("dve", TT(OH, oh1, oh2, ALU.add))
            R("dve", CP(OHb, OH))
            psC = bank(0)
            psN = bank(2)
            BpsC, BpsN = Buf(), Buf()
            for tt in range(16):
                for j in range(tt):
                    P.op("pe", MM(psC[:, tt * 32:(tt + 1) * 32], onesb, OHb[:, j, :], j == 0, False, skip=True),
                         reads=[Brt, Bcnt], writes=[BpsC])
                P.op("pe", MM(psC[:, tt * 32:(tt + 1) * 32], utri, OHb[:, tt, :], tt == 0, True, skip=True),
                     reads=[Brt, Bcnt], writes=[BpsC])
            for tt in range(16):
                P.op("pe", MM(psN[:, 0:32], onesb, OHb[:, tt, :], tt == 0, tt == 15), reads=[Brt, Bcnt], writes=[BpsN])
            P.op("dve", CP(incl, psC.rearrange("p (t e) -> p t e", t=16)), reads=[BpsC], writes=[Brt])
            P.op("dve", CP(ne, psN[:, 0:32]), reads=[BpsN], writes=[Brt])
            R("dve", TT(t32, oh1, incl, ALU.mult))
            R("dve", RSUM(rank1, t32))
            R("dve", TT(t32, oh2, incl, ALU.mult))
            R("dve", RSUM(rank2, t32))
            R("dve", TSS(nt, ne, 0.0, ALU.is_gt))
            for j in range(1, 8):
                R("dve", STT(nt, ne, 256.0 * j, nt, ALU.is_gt, ALU.add))
            R("dve", CP(csum, nt))
            cur, oth = csum, csum2
            for s_ in (1, 2, 4, 8, 16):
                R("dve", CP(oth[:, 0:s_], cur[:, 0:s_]))
                R("dve", TT(oth[:, s_:32], cur[:, s_:32], cur[:, 0:32 - s_], ALU.add))
                cur, oth = oth, cur
            cfin = cur
            R("dve", TT(excl, cfin, nt, ALU.subtract))
            R("dve", TS(sbase, excl, 256.0, ALU.mult))
            sb3 = bc(sbase.rearrange("p (o e) -> p o e", o=1), [128, 16, 32])
            R("dve", TT(t32, oh1, sb3, ALU.mult))
            R("dve", RSUM(posf2[:, :, 0], t32))
            R("dve", TT(t32, oh2, sb3, ALU.mult))
            R("dve", RSUM(posf2[:, :, 1], t32))
            R("dve", TT(posf2[:, :, 0], posf2[:, :, 0], rank1, ALU.add))
            R("dve", TT(posf2[:, :, 1], posf2[:, :, 1], rank2, ALU.add))
            R("dve", TS(posf2, posf2, -1.0, ALU.add))
            R("dve", CP(posi2, posf2))
            jt = bc(iota[:, 0:NTL].rearrange("p (j o) -> p j o", o=1), [128, NTL, 32])
            R("dve", TT(Aje, jt, bc(excl.rearrange("p (o e) -> p o e", o=1), [128, NTL, 32]), ALU.is_ge))
            R("dve", TT(Bje, jt, bc(cfin.rearrange("p (o e) -> p o e", o=1), [128, NTL, 32]), ALU.is_lt))
            R("dve", TT(Aje, Aje, Bje, ALU.mult))
            R("dve", RSUM(used, Aje))
            R("dve", TT(Bje, Aje, bc(iota[:, 0:32].rearrange("p (o e) -> p o e", o=1), [128, NTL, 32]), ALU.mult))
            R("dve", RSUM(eot, Bje))
            R("dve", TS(widxf, eot, 128.0, ALU.mult, pcol, ALU.add))
            R("dve", TS(used, used, -BIG, ALU.mult, BIG, ALU.add))
            R("dve", TT(widxf, widxf, used, ALU.add))
            R("dve", CP(widx, widxf))
            R("dve", TS(tokf, iota[:, 0:16], 128.0, ALU.mult, pcol, ALU.add))
            R("dve", CP(tokrow, bc(col(tokf), [128, 16, 16])))
            dump("posi2", posi2, [128, 16, 2], I32, [Brt])
            dump("widx", widx, [128, NTL], I32, [Brt])
            dump("g12", g12, [128, 16, 2], F32, [Brt])
            Btos_list = []
            for tt in range(16):
                for k in range(2):
                    bt = Buf()
                    Btos_list.append(bt)
                    P.dma("pool", IDMA(tos_d[:, :], tokrow[:, tt, :], out_off=posi2[:, tt, k:k + 1], bound=NSL - 1),
                          reads=[Brt, BtosD], writes=[bt])
            BtosT = Buf()
            P.dma("sp", DMA(tosT, tos_d.rearrange("(js p) w -> p js w", p=128)), reads=[BtosD] + Btos_list, writes=[BtosT])
            dump("tosT", tosT, [128, 2 * NTL, 16], I32, [BtosT])
            P.fence()
            BXg0 = Buf()
            for a_ in range(3):
                for b_ in range(2):
                    P.op("pool", MSET(Xg[a_][b_], 0.0), writes=[BXg0])
            BWg, BWu, BWd = [Buf(), Buf(), Buf()], [Buf(), Buf(), Buf()], [Buf(), Buf(), Buf()]
            BYd_list = []
            BXg = [[Buf(), Buf()], [Buf(), Buf()], [Buf(), Buf()]]
            BxgT = [Buf(), Buf()]
            Bsa, Bhd, Bpa, Bpu, Bpy, Bysb = Buf(), Buf(), Buf(), Buf(), [Buf(), Buf()], [Buf(), Buf()]
            BpsXg = [Buf(), Buf()]
            psa, psu = bank(0), bank(1)
            NT_RUN = NTL
            for nm in dbg:
                if nm.startswith("ntl"):
                    NT_RUN = int(nm[3:])
            def ffn_xgathers(j):
                wb = j % 3
                for s_ in range(2):
                    js = 2 * j + s_
                    P.dma("pool", IDMA(Xg[wb][s_], hn_d[:, :], in_off=tosT[:, js, 0:1], bound=S - 1),
                          reads=[BtosT, BhnD, BXg0], writes=[BXg[wb][s_]])

            def ffn_gathers(j):
                wb = j % 3
                P.dma("pool", IDMA(Wg[wb].rearrange("p k f -> p (k f)"), wGt_d[:, :], in_off=widx[:, j:j + 1], bound=4095),
                      reads=[Brt], writes=[BWg[wb]])
                P.dma("pool", IDMA(Wu[wb].rearrange("p k f -> p (k f)"), wUt_d[:, :], in_off=widx[:, j:j + 1], bound=4095),
                      reads=[Brt], writes=[BWu[wb]])
                P.dma("pool", IDMA(Wd[wb].rearrange("p c d -> p (c d)"), wDt_d[:, :], in_off=widx[:, j:j + 1], bound=4095),
                      reads=[Brt], writes=[BWd[wb]])

            def ffn_transposes(j):
                wb = j % 3
                xb_ = j % 2
                for s_ in range(2):
                    pX = bank_bf(6 + s_)
                    for k in range(8):
                        P.op("pe", TR(pX[:, k * 128:(k + 1) * 128], Xg[wb][s_][:, k * 128:(k + 1) * 128], identb),
                             reads=[BXg[wb][s_]], writes=[BpsXg[s_]])
                    P.op("act" if s_ == 0 else "dve",
                         (ACTV(xgT[xb_][:, :, s_ * 128:(s_ + 1) * 128], pX.rearrange("p (k t) -> p k t", k=8), AF.Copy) if s_ == 0
                          else CP(xgT[xb_][:, :, s_ * 128:(s_ + 1) * 128], pX.rearrange("p (k t) -> p k t", k=8))),
                         reads=[BpsXg[s_]], writes=[BxgT[xb_]])

            def ffn_compute(j):
                wb = j % 3
                xb_ = j % 2
                for fc in range(4):
                    pb, Bp = (psa, Bpa) if fc < 2 else (psu, Bpu)
                    Wsrc = Wg[wb] if fc < 2 else Wu[wb]
                    BWs = BWg[wb] if fc < 2 else BWu[wb]
                    for k in range(8):
                        P.op("pe", MM(pb[:, (fc % 2) * 256:(fc % 2) * 256 + 256], Wsrc[:, k, (fc % 2) * 128:(fc % 2) * 128 + 128],
                                      xgT[xb_][:, k, :], k == 0, k == 7), reads=[BWs, BxgT[xb_]], writes=[Bp])
                P.op("act", ACTV(sa, psa, AF.Silu), reads=[Bpa], writes=[Bsa])
                P.op("dve", TT(hdnT.rearrange("p c t -> p (c t)"), sa, psu, ALU.mult), reads=[Bsa, Bpu], writes=[Bhd])

            def ffn_down(j):
                wb = j % 3
                for s_ in range(2):
                    js = 2 * j + s_
                    ys = js % 2
                    py = bank(2 + 2 * ys, 2)
                    for hf in range(2):
                        for c2 in range(2):
                            P.op("pe", MM(py[:, hf * 512:(hf + 1) * 512], hdnT[:, c2, s_ * 128:(s_ + 1) * 128],
                                          Wd[wb][:, c2, hf * 512:(hf + 1) * 512], c2 == 0, c2 == 1),
                                 reads=[Bhd, BWd[wb]], writes=[Bpy[ys]])
                    P.op("act" if ys == 0 else "dve",
                         (ACTV(ysb[ys], py, AF.Copy) if ys == 0 else CP(ysb[ys], py)), reads=[Bpy[ys]], writes=[Bysb[ys]])
                    byd = Buf()
                    BYd_list.append(byd)
                    P.dma("sp", DMA(Y_d[js * 128:(js + 1) * 128, :], ysb[ys]), reads=[Bysb[ys]], writes=[byd])

            for j0 in range(min(3, NT_RUN)):
                ffn_xgathers(j0)
                ffn_gathers(j0)
            ffn_transposes(0)
            if NT_RUN > 3:
                ffn_xgathers(3)
            for j in range(NT_RUN):
                ffn_compute(j)
                if j + 1 < NT_RUN:
                    ffn_transposes(j + 1)
                    if j + 4 < NT_RUN:
                        ffn_xgathers(j + 4)
                ffn_down(j)
                if j + 3 < NT_RUN:
                    ffn_gathers(j + 3)
            P.fence()
            yk_all = list(yk) + list(ysb)
            Byk = [Buf() for _ in range(len(yk_all))]
            cnt_ = 0
            for tt in range(16):
                for k in range(2):
                    bi = cnt_ % len(yk_all)
                    cnt_ += 1
                    P.dma("pool", IDMA(yk_all[bi], Y_d[:, :], in_off=posi2[:, tt, k:k + 1], bound=NSL - 1),
                          reads=BYd_list + [Brt], writes=[Byk[bi]])
                    P.op("dve", STT(hres[:, tt, :], yk_all[bi], g12[:, tt, k:k + 1], hres[:, tt, :], ALU.mult, ALU.add),
                         reads=[Byk[bi], Brt, Bh[tt]], writes=[Bh[tt]])
                if tt >= 1:
                    final_tile(tt - 1)
            final_tile(15)
            FINAL_DONE[0] = True
        elif "noF" not in dbg:
            P.fence()
            A.top = mark_after_hres
            gF_off = R_GF
            hn32 = [A.alloc([1024], F32) for _ in range(2)]
            hnT32 = A.alloc([8, 128], F32)
            wR32 = A.alloc([8, 36], F32)
            hnT = R1.rearrange("p a b -> p (a b)")[:, 0:8 * S].rearrange("p (k t) -> p k t", k=8)
            ssh = A.alloc([16], F32)
            sqh = A.alloc([16], F32)
            rh = A.alloc([16], F32)
            lg = A.alloc([36], F32)
            g8 = A.alloc([8], F32)
            m8 = A.alloc([8], F32)
            m8b = A.alloc([8], F32)
            ohg = A.alloc([4], F32)
            negm = A.alloc([1], F32)
            ejunk = A.alloc([4], F32)
            sume = A.alloc([1], F32)
            pg = A.alloc([1], F32)
            selg = A.alloc([4, 8], F32)
            sel = A.alloc([8], F32)
            m1 = A.alloc([8], F32)
            m2 = A.alloc([8], F32)
            dlt = A.alloc([1], F32)
            exd = A.alloc([1], F32)
            w12 = A.alloc([2], F32)
            g12 = A.alloc([16, 2], F32)
            me = A.alloc([8], F32)
            gates = A.alloc([16, 32], F32)
            BwR, Bhn32, BhnT32, BhnT, Brt, Bgates = Buf(), [Buf(), Buf()], Buf(), Buf(), Buf(), Buf()
            P.dma("sp", DMA(wR32, wR_d.rearrange("(k p) c -> p k c", p=128)), writes=[BwR])
            P.op("dve", MSET(ssh, 0.0), writes=[Brt])
            P.op("dve", MSET(g8, -1e30), writes=[Brt])
            psX = bank(6, 2)
            psL = bank(1)
            BpsX, BpsL = Buf(), Buf()
            gFbc = rowbc[:, gF_off:gF_off + 1024]
            lvl = 9
            for nm in dbg:
                if nm.startswith("stoprt"):
                    lvl = int(nm[6:])
            for tt in range(16):
                i2 = tt % 2
                sl = slice(tt * 128, (tt + 1) * 128)
                P.op("act", ACTV(junk, hres[:, tt, :], AF.Square, accum_out=ssh[:, tt:tt + 1]), reads=[Bh[tt], Brt],
                     writes=[Brt])
                P.op("act", ACTV(sqh[:, tt:tt + 1], ssh[:, tt:tt + 1], AF.Sqrt, bias=epsb, scale=1.0 / D), reads=[Brt], writes=[Brt])
                P.op("dve", RECIP(rh[:, tt:tt + 1], sqh[:, tt:tt + 1]), reads=[Brt], writes=[Brt])
                P.op("dve", STT(hn32[i2], hres[:, tt, :], rh[:, tt:tt + 1], gFbc, ALU.mult, ALU.mult),
                     reads=[Bh[tt], Brt, Bcst], writes=[Bhn32[i2]])
                if lvl < 2:
                    continue
                for k in range(8):
                    P.op("pe", MM(psX[:, k * 128:(k + 1) * 128], hn32[i2][:, k * 128:(k + 1) * 128], identf, True, True),
                         reads=[Bhn32[i2]], writes=[BpsX])
                P.op("act", ACTV(hnT32, psX.rearrange("p (k t) -> p k t", k=8), AF.Copy), reads=[BpsX], writes=[BhnT32])
                P.op("pool", CP(hnT[:, :, sl], hnT32), reads=[BhnT32], writes=[BhnT])
                if lvl < 3:
                    continue
                for k in range(8):
                    P.op("pe", MM(psL[:, 0:36], hnT32[:, k, :], wR32[:, k, :], k == 0, k == 7), reads=[BhnT32, BwR], writes=[BpsL])
                P.op("dve", TT(lg, psL[:, 0:36], rowbc[:, R_BR:R_BR + 36], ALU.add), reads=[BpsL, Bcst], writes=[Brt])
                if lvl < 4:
                    continue
                P.op("dve", CP(g8[:, 0:4], lg[:, 0:4]), reads=[Brt], writes=[Brt])
                P.op("dve", lambda e: e.max(out=m8, in_=g8), reads=[Brt], writes=[Brt])
                P.op("dve", TS(ohg, lg[:, 0:4], m8[:, 0:1], ALU.is_equal), reads=[Brt], writes=[Brt])
                P.op("dve", TS(negm, m8[:, 0:1], -1.0, ALU.mult), reads=[Brt], writes=[Brt])
                P.op("act", ACTV(ejunk, lg[:, 0:4], AF.Exp, bias=negm, accum_out=sume), reads=[Brt], writes=[Brt])
                P.op("dve", RECIP(pg, sume), reads=[Brt], writes=[Brt])
                el = lg[:, 4:36].rearrange("p (g e) -> p g e", g=4)
                P.op("dve", TT(selg, el, bc(ohg.rearrange("p (g o) -> p g o", o=1), [128, 4, 8]), ALU.mult), reads=[Brt], writes=[Brt])
                P.op("dve", RSUM(sel, selg.rearrange("p g e -> p e g")), reads=[Brt], writes=[Brt])
                P.op("dve", lambda e: e.max(out=m8b, in_=sel), reads=[Brt], writes=[Brt])
                P.op("dve", TS(m1, sel, m8b[:, 0:1], ALU.is_equal), reads=[Brt], writes=[Brt])
                P.op("dve", TS(m2, sel, m8b[:, 1:2], ALU.is_equal), reads=[Brt], writes=[Brt])
                P.op("dve", TT(dlt, m8b[:, 1:2], m8b[:, 0:1], ALU.subtract), reads=[Brt], writes=[Brt])
                P.op("act", ACTV(exd, dlt, AF.Exp), reads=[Brt], writes=[Brt])
                P.op("dve", TS(w12[:, 0:1], exd, 1.0, ALU.add), reads=[Brt], writes=[Brt])
                P.op("dve", RECIP(w12[:, 0:1], w12[:, 0:1]), reads=[Brt], writes=[Brt])
                P.op("dve", TT(w12[:, 1:2], exd, w12[:, 0:1], ALU.mult), reads=[Brt], writes=[Brt])
                P.op("dve", TS(g12[:, tt, :], w12, pg[:, 0:1], ALU.mult), reads=[Brt], writes=[Brt])
                P.op("dve", TS(me, m1, g12[:, tt, 0:1], ALU.mult), reads=[Brt], writes=[Brt])
                P.op("dve", STT(me, m2, g12[:, tt, 1:2], me, ALU.mult, ALU.add), reads=[Brt], writes=[Brt])
                P.op("dve", TT(gates[:, tt, :].rearrange("p (g e) -> p g e", g=4),
                               bc(me.rearrange("p (o e) -> p o e", o=1), [128, 4, 8]),
                               bc(ohg.rearrange("p (g o) -> p g o", o=1), [128, 4, 8]), ALU.mult), reads=[Brt], writes=[Bgates])
            dump("gates", gates, [128, 16, 32], F32, [Bgates])
            dump("hnT", hnT, [128, 8, S], BF16, [BhnT])
            P.fence()
            DENSE_EXPERTS = N_EXP
            for nm in dbg:
                if nm.startswith("nexp"):
                    DENSE_EXPERTS = int(nm[4:])
            Wgu = [A.alloc([8, 512], BF16) for _ in range(2)]
            Wd = [A.alloc([2, 1024], BF16) for _ in range(2)]
            sa = A.alloc([512], F32)
            hdnT = A.alloc([2, 256], BF16)
            BW = [Buf(), Buf()]
            Bsa, Bhd, Bpa, Bpu, Bpy = Buf(), Buf(), Buf(), Buf(), [Buf(), Buf()]
            psa, psu = bank(0), bank(1)
            ycnt = 0
            for e_ in range(DENSE_EXPERTS):
                wb = e_ % 2
                er = slice(e_ * 128, (e_ + 1) * 128)
                P.dma("pool", DMA(Wgu[wb][:, :, 0:256], wGt_d[er, :].rearrange("p (k f) -> p k f", k=8)), writes=[BW[wb]])
                P.dma("pool", DMA(Wgu[wb][:, :, 256:512], wUt_d[er, :].rearrange("p (k f) -> p k f", k=8)), writes=[BW[wb]])
                P.dma("pool", DMA(Wd[wb], wDt_d[er, :].rearrange("p (c d) -> p c d", c=2)), writes=[BW[wb]])
                for pr in range(8):
                    tok = slice(pr * 256, (pr + 1) * 256)
                    for fc in range(4):
                        pb, Bp = (psa, Bpa) if fc < 2 else (psu, Bpu)
                        for k in range(8):
                            P.op("pe", MM(pb[:, (fc % 2) * 256:(fc % 2) * 256 + 256], Wgu[wb][:, k, fc * 128:(fc + 1) * 128],
                                          hnT[:, k, tok], k == 0, k == 7), reads=[BW[wb], BhnT], writes=[Bp])
                    P.op("act", ACTV(sa, psa, AF.Silu), reads=[Bpa], writes=[Bsa])
                    P.op("dve", TT(hdnT.rearrange("p c t -> p (c t)"), sa, psu, ALU.mult), reads=[Bsa, Bpu], writes=[Bhd])
                    for sub in range(2):
                        tt = 2 * pr + sub
                        ys = ycnt % 2
                        ycnt += 1
                        py = bank(2 + 2 * ys, 2)
                        for hf in range(2):
                            for c2 in range(2):
                                P.op("pe", MM(py[:, hf * 512:(hf + 1) * 512], hdnT[:, c2, sub * 128:(sub + 1) * 128],
                                              Wd[wb][:, c2, hf * 512:(hf + 1) * 512], c2 == 0, c2 == 1),
                                     reads=[Bhd, BW[wb]], writes=[Bpy[ys]])
                        P.op("dve", STT(hres[:, tt, :], py, gates[:, tt, e_:e_ + 1], hres[:, tt, :], ALU.mult, ALU.add),
                             reads=[Bpy[ys], Bgates, Bh[tt]], writes=[Bh[tt]])
        if not FINAL_DONE[0]:
            for tt in range(16):
                final_tile(tt)
        print('SBUF arena peak bytes', A.peak, 'cap', A.cap)
        P.emit(nc, block, sems, dsems, finals)
    return nc, dbg_d


_NC_CACHE = {}


def kernel(**inputs):
    inp = {k: np.asarray(v) for k, v in inputs.items()}
    if "nc" not in _NC_CACHE:
        _NC_CACHE["nc"] = build_nc()[0]
    nc = _NC_CACHE["nc"]
    sh = prep_shared(inp)
    in_maps = []
    for b in range(8):
        m = dict(sh)
        m["x"] = np.ascontiguousarray(inp["x"][b], dtype=np.float32)
        m["pos"] = np.ascontiguousarray(inp["positions"][b:b + 1]).astype(np.int32)
        in_maps.append(m)
    res = run_bass_kernel_spmd(nc, in_maps, core_ids=list(range(8)))
    out = np.stack([np.asarray(r["out"], dtype=np.float32) for r in res.results], axis=0)
    return out
```
